# Optimizing a Trainium2 kernel written in Bass

```python
import math
import jax, jax.numpy as jnp
from jax import lax
import numpy as np

D_MODEL = 1024
BATCH = 2
SEQ = 8192
DEPTH = 1

MEM_LEN = 256
D_FF = 2816
EPS = 1e-6
GLA_HEADS = 4
GLA_DK = 128
GLA_DV = 256
GLA_RANK = 16
GLA_TAU = 16.0
GLA_CHUNK = 64
DSA_HEADS = 8
DSA_DH = 64
IDX_HEADS = 8
IDX_DH = 64
TOPK_MAX = 256
Q_BLOCK = 128
REL_BUCKETS = 32
REL_MAX_DIST = 128
X_HEADS = 4
X_DH = 128

GLA_QK_W = GLA_HEADS * GLA_DK
GLA_V_W = GLA_HEADS * GLA_DV
DSA_W = DSA_HEADS * DSA_DH
IDX_QW = IDX_HEADS * IDX_DH
IN_SPLITS = (GLA_QK_W, GLA_QK_W, GLA_V_W, GLA_V_W, GLA_RANK,
             DSA_W, DSA_W, DSA_W, IDX_QW, IDX_DH, IDX_HEADS, D_MODEL, D_MODEL)
IN_WIDTH = GLA_QK_W * 2 + GLA_V_W * 2 + GLA_RANK + DSA_W * 3 + IDX_QW + IDX_DH + IDX_HEADS + 2 * D_MODEL

kernel_name = 'hybrid_gla_dsa_macaron_block'


def rmsnorm(x, g):
    xf = x.astype(jnp.float32)
    y = xf * lax.rsqrt(jnp.mean(xf * xf, axis=-1, keepdims=True) + EPS)
    return (y * g.astype(jnp.float32)).astype(x.dtype)


def swiglu(x, w_in, w_out):
    a, b = jnp.split(x @ w_in, 2, axis=-1)
    return (jax.nn.silu(a) * b) @ w_out


def gla_chunked(q, k, v, log_a):
    B, T, H, dk = q.shape
    dv = v.shape[-1]
    C = GLA_CHUNK
    N = T // C

    def to_chunks(t):
        t = t.astype(jnp.float32).reshape(B, N, C, H, t.shape[-1])
        return t.transpose(1, 0, 3, 2, 4)

    qc = to_chunks(q) * (dk ** -0.5)
    kc, vc, ac = to_chunks(k), to_chunks(v), to_chunks(log_a)
    causal = jnp.tril(jnp.ones((C, C), dtype=bool))[:, :, None]

    def step(S, inp):
        qi, ki, vi, ai = inp
        b = jnp.cumsum(ai, axis=2)
        diff = b[:, :, :, None, :] - b[:, :, None, :, :]
        decay = jnp.exp(jnp.where(causal, diff, -jnp.inf))
        scores = jnp.einsum('bhid,bhjd,bhijd->bhij', qi, ki, decay)
        o = scores @ vi + jnp.einsum('bhid,bhde->bhie', qi * jnp.exp(b), S)
        b_last = b[:, :, -1:, :]
        S = jnp.exp(b_last[:, :, 0, :])[..., None] * S + jnp.einsum('bhjd,bhje->bhde', ki * jnp.exp(b_last - b), vi)
        return S, o

    S0 = jnp.zeros((B, H, dk, dv), jnp.float32)
    _, o = lax.scan(step, S0, (qc, kc, vc, ac))
    return o.transpose(1, 0, 3, 2, 4).reshape(B, T, H, dv)


def t5_bucket(rel):
    max_exact = REL_BUCKETS // 2
    relf = jnp.maximum(rel, 1).astype(jnp.float32)
    large = max_exact + (jnp.log(relf / max_exact) / math.log(REL_MAX_DIST / max_exact)
                         * (REL_BUCKETS - max_exact)).astype(jnp.int32)
    large = jnp.minimum(large, REL_BUCKETS - 1)
    return jnp.where(rel < max_exact, rel, large)


def dsa_sparse_attention(q, k, v, iq, ik, iw, rel_table):
    B, T, H, dh = q.shape
    nb = T // Q_BLOCK
    topk = min(TOPK_MAX, T // 4)
    key_pos = jnp.arange(T)
    ikf = ik.astype(jnp.float32)

    def blocks(t):
        return t.reshape((B, nb, Q_BLOCK) + t.shape[2:]).swapaxes(0, 1)

    def one_block(inp):
        blk, qb, iqb, iwb = inp
        q_pos = blk * Q_BLOCK + jnp.arange(Q_BLOCK)
        causal = key_pos[None, :] <= q_pos[:, None]
        idx_dot = jnp.einsum('bqhd,bsd->bqhs', iqb.astype(jnp.float32), ikf) * (IDX_DH ** -0.5)
        idx_w = iwb.astype(jnp.float32) * (IDX_HEADS ** -0.5)
        idx_score = jnp.einsum('bqhs,bqh->bqs', jax.nn.relu(idx_dot), idx_w)
        idx_score = jnp.where(causal[None], idx_score, -jnp.inf)
        _, sel = lax.top_k(idx_score, topk)
        valid = sel <= q_pos[None, :, None]
        k_sel = jax.vmap(lambda kb, ib: kb[ib])(k, sel)
        v_sel = jax.vmap(lambda vb, ib: vb[ib])(v, sel)
        logits = jnp.einsum('bqhd,bqkhd->bhqk', qb, k_sel).astype(jnp.float32) * (dh ** -0.5)
        rel = jnp.maximum(q_pos[None, :, None] - sel, 0)
        bias = rel_table[t5_bucket(rel)].astype(jnp.float32)
        logits = logits + bias.transpose(0, 3, 1, 2)
        logits = jnp.where(valid[:, None], logits, -jnp.inf)
        p = jax.nn.softmax(logits, axis=-1).astype(v.dtype)
        return jnp.einsum('bhqk,bqkhd->bqhd', p, v_sel)

    out = lax.map(one_block, (jnp.arange(nb), blocks(q), blocks(iq), blocks(iw)))
    return out.swapaxes(0, 1).reshape(B, T, H, dh)


def token_mixer(xn, w_in, w_alpha_up, b_alpha, gla_norm, w_gla_proj, w_dsa_proj, rel_table, w_out):
    B, T, _ = xn.shape
    offsets = []
    acc = 0
    for w in IN_SPLITS[:-1]:
        acc += w
        offsets.append(acc)
    (gq, gk, gv, gr, ga, dq, dk, dv, iq, ik, iw, gate_a, gate_b) = jnp.split(xn @ w_in, offsets, axis=-1)
    log_a = jax.nn.log_sigmoid((ga @ w_alpha_up + b_alpha).astype(jnp.float32)) / GLA_TAU
    o_a = gla_chunked(gq.reshape(B, T, GLA_HEADS, GLA_DK), gk.reshape(B, T, GLA_HEADS, GLA_DK),
                      gv.reshape(B, T, GLA_HEADS, GLA_DV), log_a.reshape(B, T, GLA_HEADS, GLA_DK))
    o_a = rmsnorm(o_a, gla_norm).astype(xn.dtype).reshape(B, T, GLA_V_W) * jax.nn.silu(gr)
    y_a = o_a @ w_gla_proj
    o_b = dsa_sparse_attention(dq.reshape(B, T, DSA_HEADS, DSA_DH), dk.reshape(B, T, DSA_HEADS, DSA_DH),
                               dv.reshape(B, T, DSA_HEADS, DSA_DH), iq.reshape(B, T, IDX_HEADS, IDX_DH),
                               ik, iw, rel_table)
    y_b = o_b.reshape(B, T, DSA_W) @ w_dsa_proj
    merged = jax.nn.sigmoid(gate_a) * y_a + jax.nn.sigmoid(gate_b) * y_b
    return merged @ w_out


def cross_attention(xn, memn, w_cq, w_ckv, w_co):
    B, T, _ = xn.shape
    M = memn.shape[1]
    q = (xn @ w_cq).reshape(B, T, X_HEADS, X_DH)
    k, v = jnp.split(memn @ w_ckv, 2, axis=-1)
    k = k.reshape(B, M, X_HEADS, X_DH)
    v = v.reshape(B, M, X_HEADS, X_DH)
    logits = jnp.einsum('bthd,bmhd->bhtm', q, k).astype(jnp.float32) * (X_DH ** -0.5)
    p = jax.nn.softmax(logits, axis=-1).astype(v.dtype)
    o = jnp.einsum('bhtm,bmhd->bthd', p, v).reshape(B, T, X_HEADS * X_DH)
    return o @ w_co


def setup_inputs(seed: int = 0) -> dict:
    key = jax.random.key(seed)
    ks = jax.random.split(key, 32)
    L = DEPTH

    def w(k, shape, fan_in):
        return jax.random.normal(k, shape, jnp.float32) * (fan_in ** -0.5)

    def gain(k, shape):
        return 1.0 + 0.05 * jax.random.normal(k, shape, jnp.float32)

    return {
        'x': jax.random.normal(ks[0], (BATCH, SEQ, D_MODEL), jnp.float32),
        'mem': jax.random.normal(ks[1], (BATCH, MEM_LEN, D_MODEL), jnp.float32),
        'ffn1_pre': gain(ks[2], (L, D_MODEL)),
        'ffn1_post': gain(ks[3], (L, D_MODEL)),
        'ffn1_w_in': w(ks[4], (L, D_MODEL, 2 * D_FF), D_MODEL),
        'ffn1_w_out': w(ks[5], (L, D_FF, D_MODEL), D_FF),
        'mix_pre': gain(ks[6], (L, D_MODEL)),
        'mix_post': gain(ks[7], (L, D_MODEL)),
        'w_in': w(ks[8], (L, D_MODEL, IN_WIDTH), D_MODEL),
        'w_alpha_up': w(ks[9], (L, GLA_RANK, GLA_QK_W), GLA_RANK),
        'b_alpha': 0.1 * jax.random.normal(ks[10], (L, GLA_QK_W), jnp.float32),
        'gla_norm': gain(ks[11], (L, GLA_HEADS, GLA_DV)),
        'w_gla_proj': w(ks[12], (L, GLA_V_W, D_MODEL), GLA_V_W),
        'w_dsa_proj': w(ks[13], (L, DSA_W, D_MODEL), DSA_W),
        'rel_bias_table': 0.5 * jax.random.normal(ks[14], (REL_BUCKETS, DSA_HEADS), jnp.float32),
        'w_out': w(ks[15], (L, D_MODEL, D_MODEL), D_MODEL),
        'mem_norm': gain(ks[16], (L, D_MODEL)),
        'cross_pre': gain(ks[17], (L, D_MODEL)),
        'cross_post': gain(ks[18], (L, D_MODEL)),
        'w_cq': w(ks[19], (L, D_MODEL, X_HEADS * X_DH), D_MODEL),
        'w_ckv': w(ks[20], (L, D_MODEL, 2 * X_HEADS * X_DH), D_MODEL),
        'w_co': w(ks[21], (L, X_HEADS * X_DH, D_MODEL), X_HEADS * X_DH),
        'ffn2_pre': gain(ks[22], (L, D_MODEL)),
        'ffn2_post': gain(ks[23], (L, D_MODEL)),
        'ffn2_w_in': w(ks[24], (L, D_MODEL, 2 * D_FF), D_MODEL),
        'ffn2_w_out': w(ks[25], (L, D_FF, D_MODEL), D_FF),
    }


def reference(x, mem, ffn1_pre, ffn1_post, ffn1_w_in, ffn1_w_out, mix_pre, mix_post, w_in,
              w_alpha_up, b_alpha, gla_norm, w_gla_proj, w_dsa_proj, rel_bias_table, w_out,
              mem_norm, cross_pre, cross_post, w_cq, w_ckv, w_co, ffn2_pre, ffn2_post,
              ffn2_w_in, ffn2_w_out):
    h = x
    for l in range(DEPTH):
        h = h + 0.5 * rmsnorm(swiglu(rmsnorm(h, ffn1_pre[l]), ffn1_w_in[l], ffn1_w_out[l]), ffn1_post[l])
        mix = token_mixer(rmsnorm(h, mix_pre[l]), w_in[l], w_alpha_up[l], b_alpha[l], gla_norm[l],
                          w_gla_proj[l], w_dsa_proj[l], rel_bias_table, w_out[l])
        h = h + rmsnorm(mix, mix_post[l])
        xa = cross_attention(rmsnorm(h, cross_pre[l]), rmsnorm(mem, mem_norm[l]), w_cq[l], w_ckv[l], w_co[l])
        h = h + rmsnorm(xa, cross_post[l])
        h = h + 0.5 * rmsnorm(swiglu(rmsnorm(h, ffn2_pre[l]), ffn2_w_in[l], ffn2_w_out[l]), ffn2_post[l])
    return h
```

```python
import numpy as np
from contextlib import ExitStack
import concourse.bass as bass
import concourse.mybir as mybir
from concourse.bass_utils import run_bass_kernel_spmd

F32 = mybir.dt.float32
BF16 = mybir.dt.bfloat16
AF = mybir.ActivationFunctionType
ALU = mybir.AluOpType
AX = mybir.AxisListType

D = 1024
DFF = 2816
T = 8192
EPS = 1e-6
NEG = -30000.0

ENGS = ("pe", "act", "dve", "pool", "sp")


class Op:
    __slots__ = ("eng", "fn", "deps", "odeps", "needs_sig", "sig", "ndma", "idx", "dsem", "cost", "epoch", "start", "finish", "ready", "npend", "done")

    def __init__(self, eng, fn, ndma, cost):
        self.eng = eng
        self.fn = fn
        self.deps = set()
        self.odeps = set()
        self.needs_sig = False
        self.sig = None
        self.ndma = ndma
        self.dsem = None
        self.cost = cost
        self.epoch = 0
        self.start = 0.0
        self.finish = 0.0
        self.ready = 0.0
        self.npend = 0
        self.done = False


DEF_COST = {"pe": 0.35, "act": 0.7, "dve": 0.7, "pool": 0.9, "sp": 2.5}
SCHED = True
WINDOW = 48


class Prog:
    def __init__(self, nc, n_dma_sems=40):
        self.nc = nc
        self.ops = {e: [] for e in ENGS}
        self.lastw = {}
        self.readers = {}
        self.n_dma_sems = n_dma_sems
        self.all_ops = []
        self.epoch = 0

    def op(self, eng, fn, reads=(), writes=(), ndma=0, cost=None):
        if cost is None:
            cost = 2.5 if ndma > 0 else DEF_COST[eng]
        o = Op(eng, fn, ndma, cost)
        o.idx = len(self.all_ops)
        o.epoch = self.epoch
        self.all_ops.append(o)
        is_dma = ndma > 0
        for k in reads:
            w = self.lastw.get(k)
            if w is not None:
                self._dep(o, w, True, is_dma)
        for k in writes:
            w = self.lastw.get(k)
            if w is not None:
                self._dep(o, w, False, is_dma)
            for r in self.readers.get(k, ()):
                self._dep(o, r, False, is_dma)
        for k in reads:
            self.readers.setdefault(k, []).append(o)
        for k in writes:
            self.lastw[k] = o
            self.readers[k] = []
        self.ops[eng].append(o)
        return o

    def _dep(self, o, d, raw, is_dma):
        if d is o:
            return
        if d.eng == o.eng and d.ndma == 0 and not is_dma:
            if o.eng == "pe" or not raw:
                o.odeps.add(d)
                return
        o.deps.add(d)
        d.needs_sig = True

    def barrier(self):
        for e in ENGS:
            o = Op(e, None, 0, 0.0)
            o.idx = len(self.all_ops)
            o.epoch = self.epoch
            self.all_ops.append(o)
            self.ops[e].append(o)
        self.epoch += 1
        self.lastw = {}
        self.readers = {}

    def schedule(self):
        LAT = 0.2
        succs = {}
        for o in self.all_ops:
            o.npend = 0
            o.ready = 0.0
            o.done = False
        for o in self.all_ops:
            if o.fn is None:
                continue
            for d in (o.deps | o.odeps):
                succs.setdefault(d.idx, []).append(o)
                o.npend += 1
        nep = self.epoch + 1
        ep_remaining = [0] * (nep + 1)
        ep_finish = [0.0] * (nep + 1)
        for o in self.all_ops:
            if o.fn is not None:
                ep_remaining[o.epoch] += 1
        head = {e: 0 for e in ENGS}
        eng_free = {e: 0.0 for e in ENGS}
        order = {e: [] for e in ENGS}
        remaining = len(self.all_ops)
        if not SCHED:
            t = 0.0
            for o in self.all_ops:
                o.start = t
                t += 1.0
                order[o.eng].append(o)
            self.ops = order
            return
        while remaining > 0:
            best = None
            bstart = None
            for e in ENGS:
                q = self.ops[e]
                i = head[e]
                cnt = 0
                n = len(q)
                while i < n and cnt < WINDOW:
                    o = q[i]
                    if not o.done:
                        if o.fn is None:
                            if cnt == 0 and ep_remaining[o.epoch] == 0:
                                st_ = max(eng_free[e], ep_finish[o.epoch] + LAT)
                                if best is None or st_ < bstart or (st_ == bstart and o.idx < best.idx):
                                    best, bstart = o, st_
                            break
                        if o.npend == 0:
                            st_ = max(eng_free[e], o.ready)
                            if best is None or st_ < bstart or (st_ == bstart and o.idx < best.idx):
                                best, bstart = o, st_
                        cnt += 1
                    i += 1
            assert best is not None, "scheduler deadlock"
            o = best
            e = o.eng
            o.done = True
            o.start = bstart
            remaining -= 1
            if o.fn is None:
                o.finish = bstart
                eng_free[e] = max(eng_free[e], bstart)
            else:
                o.finish = bstart + o.cost
                eng_free[e] = bstart + (0.08 * o.ndma if o.ndma > 0 else o.cost)
                ep_remaining[o.epoch] -= 1
                if o.finish > ep_finish[o.epoch]:
                    ep_finish[o.epoch] = o.finish
                for s_ in succs.get(o.idx, ()):
                    s_.npend -= 1
                    if o.finish + LAT > s_.ready:
                        s_.ready = o.finish + LAT
            order[e].append(o)
            q = self.ops[e]
            while head[e] < len(q) and q[head[e]].done:
                head[e] += 1
        self.ops = order

    def emit(self, stack):
        nc = self.nc
        self.schedule()
        last_in_epoch = {}
        dmas_in_epoch = {}
        for e in ENGS:
            for o in self.ops[e]:
                if o.fn is None:
                    continue
                if o.ndma > 0:
                    dmas_in_epoch.setdefault(o.epoch, []).append(o)
                else:
                    last_in_epoch[(o.epoch, e)] = o
        for e in ENGS:
            for o in self.ops[e]:
                if o.fn is None:
                    for e2 in ENGS:
                        d = last_in_epoch.get((o.epoch, e2))
                        if d is not None and e2 != e:
                            o.deps.add(d)
                            d.needs_sig = True
                    for d in dmas_in_epoch.get(o.epoch, ()):
                        o.deps.add(d)
        esem = {e: stack.enter_context(nc.semaphore("s_" + e)) for e in ENGS}
        dsems = [stack.enter_context(nc.semaphore("d%d" % i)) for i in range(self.n_dma_sems)]
        dcount = [0] * self.n_dma_sems
        dlast = [None] * self.n_dma_sems
        ecount = {e: 0 for e in ENGS}
        rr = 0
        glob = sorted(self.all_ops, key=lambda x: (x.start, x.idx))
        pos = {}
        for e in ENGS:
            for n_, o in enumerate(self.ops[e]):
                pos[o.idx] = n_
        for o in glob:
            if o.ndma > 0:
                s = rr % self.n_dma_sems
                rr += 1
                if dlast[s] is not None:
                    o.deps.add(dlast[s])
                dcount[s] += 16 * o.ndma
                o.sig = (dsems[s], dcount[s])
                o.dsem = dsems[s]
                dlast[s] = o
        for e in ENGS:
            for o in self.ops[e]:
                if o.ndma == 0 and o.needs_sig and o.fn is not None:
                    ecount[e] += 1
                    o.sig = (esem[e], ecount[e])
        engobj = {"pe": nc.tensor, "act": nc.scalar, "dve": nc.vector, "pool": nc.gpsimd, "sp": nc.sync}
        block = stack.enter_context(nc.Block())

        def run(e):
            eng = engobj[e]
            waited = {}
            for o in self.ops[e]:
                for d in sorted(o.deps, key=lambda x: x.idx):
                    if d.sig is None:
                        continue
                    sem, val = d.sig
                    key = id(sem)
                    if waited.get(key, 0) < val:
                        eng.wait_ge(sem, val)
                        waited[key] = val
                if o.fn is None:
                    continue
                r = o.fn(eng)
                if o.ndma > 0:
                    rs = r if isinstance(r, (list, tuple)) else [r]
                    assert len(rs) == o.ndma, (len(rs), o.ndma)
                    for i in rs:
                        i.then_inc(o.dsem, 16)
                elif o.sig is not None:
                    last = r[-1] if isinstance(r, (list, tuple)) else r
                    last.then_inc(o.sig[0], 1)

        @block.tensor
        def _(e):
            run("pe")

        @block.scalar
        def _(e):
            run("act")

        @block.vector
        def _(e):
            run("dve")

        @block.gpsimd
        def _(e):
            run("pool")

        @block.sync
        def _(e):
            run("sp")


class KB:
    def __init__(self, upto):
        self.upto = upto
        self.nc = bass.Bass("TRN2", target_bir_lowering=False)
        self.P = Prog(self.nc)
        self.uid = 0

    def din(self, name, shape, dt=F32):
        return self.nc.dram_tensor(name, list(shape), dt, kind="ExternalInput").ap()

    def dout(self, name, shape, dt=F32):
        return self.nc.dram_tensor(name, list(shape), dt, kind="ExternalOutput").ap()

    def dscr(self, name, shape, dt):
        return self.nc.dram_tensor(name, list(shape), dt, kind="Internal").ap()

    def sb(self, st, name, shape, dt):
        return st.enter_context(self.nc.sbuf_tensor(name, list(shape), dt))

    def ps(self, st, name, shape, dt=F32):
        return st.enter_context(self.nc.psum_tensor(name, list(shape), dt))

    def consts(self, st):
        P = self.P
        self.idn_d = self.I["idn"]
        idf = self.sb(st, "idf", [128, 128], F32)
        self.idf = idf
        self.idb = self.sb(st, "idb", [128, 128], BF16)
        self.epsc = self.sb(st, "epsc", [128, 1], F32)
        P.op("sp", lambda e: e.dma_start(out=idf[:], in_=self.idn_d), writes=["idf"], ndma=1)
        P.op("dve", lambda e: e.tensor_copy(out=self.idb[:], in_=idf[:]), reads=["idf"], writes=["idb"])
        P.op("dve", lambda e: e.memset(self.epsc[:], EPS), writes=["epsc"])

    def rstd_of(self, src, width, stat, col, key_src, tag, junk):
        P = self.P
        c0 = stat[:, col:col + 1]
        c1 = stat[:, col + 1:col + 2]
        k0 = ("stat", tag, col)
        P.op("dve", lambda e: e.memset(c0, 0.0), writes=[k0])
        P.op("act", lambda e: e.activation(out=junk, in_=src, func=AF.Square, accum_out=c0),
             reads=[key_src, k0], writes=[k0, ("junk", tag)])
        P.op("act", lambda e: e.activation(out=c1, in_=c0, func=AF.Sqrt, bias=self.epsc[:, 0:1], scale=1.0 / width),
             reads=[k0, "epsc"], writes=[(k0, 1)])
        P.op("dve", lambda e: e.reciprocal(out=c0, in_=c1), reads=[(k0, 1)], writes=[k0])
        return c0, k0

    def ffn_phase(self, name, src_d, ntok, w_in_d, w_out_d, pre_d, post_d, dst_d):
        nc, P = self.nc, self.P
        with ExitStack() as st:
            wi = self.sb(st, name + "wi", [128, 8, 2 * DFF], BF16)
            wo = self.sb(st, name + "wo", [128, 22, D], BF16)
            gpre = self.sb(st, name + "gpre", [128, D], F32)
            gpost = self.sb(st, name + "gpost", [128, D], F32)
            xt = [self.sb(st, name + "xt%d" % i, [128, D], F32) for i in range(2)]
            xr = [self.sb(st, name + "xr%d" % i, [128, D], F32) for i in range(2)]
            xn = self.sb(st, name + "xn", [128, D], BF16)
            xT = self.sb(st, name + "xT", [128, 8, 512], BF16)
            h1T = self.sb(st, name + "h1T", [128, 22, 512], BF16)
            sa = [self.sb(st, name + "sa%d" % i, [128, 512], F32) for i in range(2)]
            tmp = self.sb(st, name + "tmp", [128, D], F32)
            junk = self.sb(st, name + "junk", [128, D], BF16)
            stat = self.sb(st, name + "stat", [128, 64], F32)
            psA = [self.ps(st, name + "psA%d" % i, [128, 512]) for i in range(2)]
            psB = [self.ps(st, name + "psB%d" % i, [128, 512]) for i in range(2)]
            psY = self.ps(st, name + "psY", [128, 1024])
            psT = [self.ps(st, name + "psT%d" % i, [128, 512], BF16) for i in range(2)]

            wi_src = w_in_d.rearrange("(k p) n -> p k n", p=128)
            for k in range(8):
                P.op("pool", lambda e, k=k: e.dma_start(out=wi[:, k, :], in_=wi_src[:, k, :]), writes=[("wi", k)], ndma=1)
            wo_src = w_out_d.rearrange("(f p) n -> p f n", p=128)
            for f0 in range(0, 22, 6):
                f1 = min(22, f0 + 6)
                P.op("pool", lambda e, f0=f0, f1=f1: e.dma_start(out=wo[:, f0:f1, :], in_=wo_src[:, f0:f1, :]),
                     writes=[("wo", f) for f in range(f0, f1)], ndma=1)
            P.op("sp", lambda e: e.dma_start(out=gpre[:], in_=pre_d.to_broadcast([128, D])), writes=["gpre"], ndma=1)
            P.op("sp", lambda e: e.dma_start(out=gpost[:], in_=post_d.to_broadcast([128, D])), writes=["gpost"], ndma=1)
            wi_keys = [("wi", k) for k in range(8)]
            wo_keys = [("wo", f) for f in range(22)]

            nblk = ntok // 512
            tcount = 0
            for blk in range(nblk):
                for tt in range(4):
                    r0 = blk * 512 + tt * 128
                    xb = xt[tcount % 2]
                    kx = ("xt", tcount % 2)
                    tcount += 1
                    P.op("sp", lambda e, xb=xb, r0=r0: e.dma_start(out=xb[:], in_=src_d[r0:r0 + 128, :]), writes=[kx], ndma=1)
                    c0, k0 = self.rstd_of(xb[:], D, stat, 2 * (tcount % 8), kx, name, junk[:])
                    P.op("dve", lambda e, xb=xb, c0=c0: e.scalar_tensor_tensor(out=xn[:], in0=xb[:], scalar=c0, in1=gpre[:], op0=ALU.mult, op1=ALU.mult),
                         reads=[kx, k0, "gpre"], writes=["xn"])
                    for g in range(2):
                        def tr(e, g=g):
                            r = None
                            for kk in range(4):
                                k = 4 * g + kk
                                r = e.transpose(out=psT[g][:, kk * 128:(kk + 1) * 128], in_=xn[:, k * 128:(k + 1) * 128], identity=self.idb[:])
                            return r
                        P.op("pe", tr, reads=["xn", "idb"], writes=[("psT", g)])
                        eng = "act" if g == 0 else "dve"
                        if eng == "act":
                            P.op("act", lambda e, g=g, tt=tt: e.copy(out=xT[:, 4 * g:4 * g + 4, tt * 128:(tt + 1) * 128],
                                                                     in_=psT[g][:].rearrange("p (c t) -> p c t", c=4)),
                                 reads=[("psT", g)], writes=[("xT", tt, g)])
                        else:
                            P.op("dve", lambda e, g=g, tt=tt: e.tensor_copy(out=xT[:, 4 * g:4 * g + 4, tt * 128:(tt + 1) * 128],
                                                                            in_=psT[g][:].rearrange("p (c t) -> p c t", c=4)),
                                 reads=[("psT", g)], writes=[("xT", tt, g)])
                xT_keys = [("xT", tt, g) for tt in range(4) for g in range(2)]
                for f in range(22):
                    pa, pb = psA[f % 2], psB[f % 2]

                    def mmab(e, f=f, pa=pa, pb=pb):
                        for k in range(8):
                            e.matmul(pa[:], lhsT=wi[:, k, f * 128:(f + 1) * 128], rhs=xT[:, k, :], start=(k == 0), stop=(k == 7))
                        r = None
                        for k in range(8):
                            r = e.matmul(pb[:], lhsT=wi[:, k, DFF + f * 128:DFF + (f + 1) * 128], rhs=xT[:, k, :], start=(k == 0), stop=(k == 7))
                        return r
                    P.op("pe", mmab, reads=wi_keys + xT_keys, writes=[("psA", f % 2), ("psB", f % 2)])
                    s = sa[f % 2]
                    P.op("act", lambda e, s=s, pa=pa: e.activation(out=s[:], in_=pa[:], func=AF.Silu), reads=[("psA", f % 2)], writes=[("sa", f % 2)])
                    P.op("dve", lambda e, s=s, pb=pb, f=f: e.tensor_tensor(out=h1T[:, f, :], in0=s[:], in1=pb[:], op=ALU.mult),
                         reads=[("sa", f % 2), ("psB", f % 2)], writes=[("h1T", f)])
                h1T_keys = [("h1T", f) for f in range(22)]
                for tt in range(4):
                    r0 = blk * 512 + tt * 128

                    def mmy(e, tt=tt):
                        r = None
                        for nh in range(2):
                            for f in range(22):
                                r = e.matmul(psY[:, nh * 512:(nh + 1) * 512], lhsT=h1T[:, f, tt * 128:(tt + 1) * 128],
                                             rhs=wo[:, f, nh * 512:(nh + 1) * 512], start=(f == 0), stop=(f == 21))
                        return r
                    P.op("pe", mmy, reads=h1T_keys + wo_keys, writes=["psY"])
                    xres = xr[tt % 2]
                    kr = ("xr", tt % 2)
                    P.op("sp", lambda e, xres=xres, r0=r0: e.dma_start(out=xres[:], in_=src_d[r0:r0 + 128, :]), writes=[kr], ndma=1)
                    c0, k0 = self.rstd_of(psY[:], D, stat, 16 + 2 * (tt % 4), "psY", name + "y", junk[:])
                    P.op("dve", lambda e, c0=c0: e.scalar_tensor_tensor(out=tmp[:], in0=psY[:], scalar=c0, in1=gpost[:], op0=ALU.mult, op1=ALU.mult),
                         reads=["psY", k0, "gpost"], writes=["tmp"])
                    P.op("dve", lambda e, xres=xres: e.scalar_tensor_tensor(out=xres[:], in0=tmp[:], scalar=0.5, in1=xres[:], op0=ALU.mult, op1=ALU.add),
                         reads=["tmp", kr], writes=[kr])
                    P.op("sp", lambda e, xres=xres, r0=r0: e.dma_start(out=dst_d[r0:r0 + 128, :], in_=xres[:]), reads=[kr], writes=[("dst", name, r0)], ndma=1)
            P.barrier()

    def transposes8(self, xn, psT, xT_out_fn, keys_in, key_out_fn, nchunks=8):
        P = self.P
        ng = (nchunks + 3) // 4
        for g in range(ng):
            n = min(4, nchunks - 4 * g)

            def tr(e, g=g, n=n):
                r = None
                for kk in range(n):
                    k = 4 * g + kk
                    r = e.transpose(out=psT[g % 2][:, kk * 128:(kk + 1) * 128], in_=xn[:, k * 128:(k + 1) * 128], identity=self.idb[:])
                return r
            P.op("pe", tr, reads=list(keys_in) + ["idb"], writes=[("psT", g % 2)])
            dst = xT_out_fn(g, n)
            src = psT[g % 2][:, 0:n * 128].rearrange("p (c t) -> p c t", c=n)
            if g % 2 == 0:
                P.op("act", lambda e, dst=dst, src=src: e.copy(out=dst, in_=src), reads=[("psT", g % 2)], writes=[key_out_fn(g)])
            else:
                P.op("dve", lambda e, dst=dst, src=src: e.tensor_copy(out=dst, in_=src), reads=[("psT", g % 2)], writes=[key_out_fn(g)])

    def load_w(self, dst, src_d, c0, c1, key, kchunks=8):
        src = src_d.rearrange("(k p) n -> p k n", p=128)
        self.P.op("pool", lambda e: e.dma_start(out=dst, in_=src[:, :, c0:c1]), writes=[key], ndma=1)

    def phase_a2(self, I, S, nblk):
        nc, P = self.nc, self.P
        w_in = I["w_in"]
        with ExitStack() as st:
            def dbl(name, shape, dt):
                return [self.sb(st, "a2%s%d" % (name, i), shape, dt) for i in range(2)]
            w_gk = self.sb(st, "a2w_gk", [128, 8, 512], BF16)
            w_gv = self.sb(st, "a2w_gv", [128, 8, 1024], BF16)
            w_dv = self.sb(st, "a2w_dv", [128, 8, 512], BF16)
            w_dk = self.sb(st, "a2w_dk", [128, 8, 512], BF16)
            w_ik = self.sb(st, "a2w_ik", [128, 8, 64], BF16)
            w_ga = self.sb(st, "a2w_ga", [128, 8, 16], BF16)
            w_au = self.sb(st, "a2w_au", [16, 512], BF16)
            balpha = self.sb(st, "a2balpha", [128, 512], F32)
            gpre = self.sb(st, "a2gpre", [128, D], F32)
            tri = self.sb(st, "a2tri", [128, 128], F32)
            negs = self.sb(st, "a2negs", [128, 1], F32)
            ej = self.sb(st, "a2ej", [128, 4], F32)
            ht = dbl("ht", [128, D], F32)
            hown = dbl("hown", [128, D], F32)
            xn = dbl("xn", [128, D], BF16)
            xT = dbl("xT", [128, 8, 512], BF16)
            xTown = dbl("xTown", [128, 8, 128], BF16)
            junk = self.sb(st, "a2junk", [128, D], BF16)
            stat = self.sb(st, "a2stat", [128, 64], F32)
            kTs = dbl("kTs", [128, 4, 512], BF16)
            ikTs = dbl("ikTs", [64, 512], BF16)
            gaT = dbl("gaT", [16, 512], BF16)
            zb = dbl("zb", [128, 512], F32)
            lt = dbl("lt", [128, 512], F32)
            enb = dbl("enb", [128, 512], F32)
            ktil = dbl("ktil", [128, 512], BF16)
            gvb = dbl("gvb", [128, 1024], BF16)
            dvb = dbl("dvb", [128, 512], BF16)
            decay = dbl("decay", [128, 4], F32)
            Sst = self.sb(st, "a2S", [128, 4, 256], F32)
            Stmp = dbl("Stmp", [128, 4, 256], F32)
            Ssel = dbl("Ssel", [128, 4, 256], F32)
            psT = [self.ps(st, "a2psT%d" % i, [128, 512], BF16) for i in range(2)]
            psW = self.ps(st, "a2psW", [128, 1024])
            psX = [self.ps(st, "a2psX%d" % i, [128, 512]) for i in range(2)]
            psZ = [self.ps(st, "a2psZ%d" % i, [128, 512]) for i in range(2)]

            self.load_w(w_gk[:], w_in, 512, 1024, "w_gk")
            self.load_w(w_gv[:], w_in, 1024, 2048, "w_gv")
            self.load_w(w_dv[:], w_in, 4112, 4624, "w_dv")
            self.load_w(w_dk[:], w_in, 3600, 4112, "w_dk")
            self.load_w(w_ik[:], w_in, 5136, 5200, "w_ik")
            self.load_w(w_ga[:], w_in, 3072, 3088, "w_ga")
            P.op("pool", lambda e: e.dma_start(out=w_au[:], in_=I["w_alpha_up"]), writes=["w_au"], ndma=1)
            P.op("sp", lambda e: e.dma_start(out=balpha[:], in_=I["b_alpha"].to_broadcast([128, 512])), writes=["balpha"], ndma=1)
            P.op("sp", lambda e: e.dma_start(out=gpre[:], in_=I["mix_pre"].to_broadcast([128, D])), writes=["gpre"], ndma=1)
            P.op("sp", lambda e: e.dma_start(out=tri[:], in_=I["tri"]), writes=["tri"], ndma=1)
            P.op("sp", lambda e: e.dma_start(out=ej[:], in_=I["ej"]), writes=["ej"], ndma=1)
            P.op("dve", lambda e: e.memset(negs[:], -1.0 / 16.0), writes=["negs"])
            P.op("dve", lambda e: e.memset(Sst[:], 0.0), writes=[("S", h) for h in range(4)])
            kab = self.sb(st, "a2kab", [128, 4], F32)
            KA = self.sb(st, "a2KA", [128, 4], F32)
            P.op("dve", lambda e: e.memset(KA[:], 0.0), writes=["KA"])

            tcount = 0
            for blk in range(nblk):
                bp = blk % 2
                xTb, hownb, xTownb, kTsb, ikTsb, gaTb, Sselb = xT[bp], hown[bp], xTown[bp], kTs[bp], ikTs[bp], gaT[bp], Ssel[bp]
                for u in range(4):
                    r0 = blk * 512 + u * 128
                    tp = tcount % 2
                    hb = ht[tp]
                    xnb = xn[tp]
                    kh = ("ht", tp)
                    kxn = ("xn", tp)
                    tcount += 1
                    P.op("sp", lambda e, hb=hb, r0=r0: e.dma_start(out=hb[:], in_=S["h1"][r0:r0 + 128, :]), writes=[kh], ndma=1)
                    c0, k0 = self.rstd_of(hb[:], D, stat, 2 * (tcount % 8), kh, "a2", junk[:])
                    P.op("dve", lambda e, hb=hb, c0=c0, xnb=xnb: e.scalar_tensor_tensor(out=xnb[:], in0=hb[:], scalar=c0, in1=gpre[:], op0=ALU.mult, op1=ALU.mult),
                         reads=[kh, k0, "gpre"], writes=[kxn], cost=1.2)
                    if u == 0:
                        P.op("pool", lambda e, hb=hb, hownb=hownb: e.tensor_scalar(out=hownb[:], in0=hb[:], scalar1=ej[:, 0:1], scalar2=None, op0=ALU.mult),
                             reads=[kh, "ej"], writes=[("hown", bp)], cost=2.0)
                    else:
                        P.op("dve", lambda e, hb=hb, u=u, hownb=hownb: e.scalar_tensor_tensor(out=hownb[:], in0=hb[:], scalar=ej[:, u:u + 1], in1=hownb[:], op0=ALU.mult, op1=ALU.add),
                             reads=[kh, "ej", ("hown", bp)], writes=[("hown", bp)], cost=1.2)
                    self.transposes8(xnb, psT, lambda g, n, u=u, xTb=xTb: xTb[:, 4 * g:4 * g + n, u * 128:(u + 1) * 128], [kxn], lambda g, u=u, bp=bp: ("xT", bp, u, g))
                P.op("sp", lambda e, blk=blk, hownb=hownb: e.dma_start(out=S["h1own"][blk * 128:(blk + 1) * 128, :], in_=hownb[:]), reads=[("hown", bp)], writes=[("h1own_d", blk)], ndma=1)
                xT_keys = [("xT", bp, u, g) for u in range(4) for g in range(2)]
                for u in range(4):
                    if u == 0:
                        P.op("pool", lambda e, xTb=xTb, xTownb=xTownb: e.tensor_scalar(out=xTownb[:], in0=xTb[:, :, 0:128], scalar1=ej[:, 0:1], scalar2=None, op0=ALU.mult),
                             reads=xT_keys + ["ej"], writes=[("xTown", bp)], cost=1.5)
                    else:
                        P.op("dve", lambda e, u=u, xTb=xTb, xTownb=xTownb: e.scalar_tensor_tensor(out=xTownb[:], in0=xTb[:, :, u * 128:(u + 1) * 128], scalar=ej[:, u:u + 1], in1=xTownb[:],
                                                                                                  op0=ALU.mult, op1=ALU.add),
                             reads=xT_keys + ["ej", ("xTown", bp)], writes=[("xTown", bp)], cost=1.5)
                P.op("sp", lambda e, blk=blk, xTownb=xTownb: e.dma_start(out=S["xTown"][blk], in_=xTownb[:]), reads=[("xTown", bp)], writes=[("xTown_d", blk)], ndma=1)
                for c in range(4):
                    pf = psX[c % 2]

                    def mmf(e, c=c, pf=pf, xTb=xTb):
                        r = None
                        for k in range(8):
                            r = e.matmul(pf[:], lhsT=w_dk[:, k, c * 128:(c + 1) * 128], rhs=xTb[:, k, :], start=(k == 0), stop=(k == 7))
                        return r
                    P.op("pe", mmf, reads=xT_keys + ["w_dk"], writes=[("psX", c % 2)], cost=1.9)
                    if c % 2 == 0:
                        P.op("act", lambda e, c=c, pf=pf, kTsb=kTsb: e.copy(out=kTsb[:, c, :], in_=pf[:]), reads=[("psX", c % 2)], writes=[("kTs", bp, c)])
                    else:
                        P.op("dve", lambda e, c=c, pf=pf, kTsb=kTsb: e.tensor_copy(out=kTsb[:, c, :], in_=pf[:]), reads=[("psX", c % 2)], writes=[("kTs", bp, c)])
                P.op("sp", lambda e, blk=blk, kTsb=kTsb: e.dma_start(out=S["kT"][:, :, blk * 512:(blk + 1) * 512], in_=kTsb[:]),
                     reads=[("kTs", bp, c) for c in range(4)], writes=[("kT_d", blk)], ndma=1)
                P.op("dve", lambda e, kTsb=kTsb: e.tensor_reduce(out=kab[:], in_=kTsb[:], axis=AX.X, op=ALU.max, apply_absolute_value=True),
                     reads=[("kTs", bp, c) for c in range(4)], writes=["kab"], cost=2.3)
                P.op("dve", lambda e: e.tensor_tensor(out=KA[:], in0=KA[:], in1=kab[:], op=ALU.max), reads=["kab", "KA"], writes=["KA"], cost=0.1)

                def mmik(e, xTb=xTb):
                    r = None
                    for k in range(8):
                        r = e.matmul(psZ[0][0:64, :], lhsT=w_ik[:, k, :], rhs=xTb[:, k, :], start=(k == 0), stop=(k == 7))
                    return r
                P.op("pe", mmik, reads=xT_keys + ["w_ik"], writes=[("psZ", 0)], cost=1.9)
                P.op("act", lambda e, ikTsb=ikTsb: e.copy(out=ikTsb[:], in_=psZ[0][0:64, :]), reads=[("psZ", 0)], writes=[("ikTs", bp)])
                P.op("sp", lambda e, blk=blk, ikTsb=ikTsb: e.dma_start(out=S["ikT"][:, blk * 512:(blk + 1) * 512], in_=ikTsb[:]), reads=[("ikTs", bp)], writes=[("ikT_d", blk)], ndma=1)

                def mmga(e, xTb=xTb):
                    r = None
                    for k in range(8):
                        r = e.matmul(psZ[1][0:16, :], lhsT=w_ga[:, k, :], rhs=xTb[:, k, :], start=(k == 0), stop=(k == 7))
                    return r
                P.op("pe", mmga, reads=xT_keys + ["w_ga"], writes=[("psZ", 1)], cost=1.9)
                P.op("dve", lambda e, gaTb=gaTb: e.tensor_copy(out=gaTb[:], in_=psZ[1][0:16, :]), reads=[("psZ", 1)], writes=[("gaT", bp)])
                for u in range(4):
                    tile = blk * 4 + u
                    tp = tile % 2
                    ucols = slice(u * 128, (u + 1) * 128)
                    pX, pZ = psX[tp], psZ[tp]
                    kX, kZ = ("psX", tp), ("psZ", tp)
                    zb_, lt_, enb_, ktil_, gvb_, dvb_, decay_, Stmp_ = zb[tp], lt[tp], enb[tp], ktil[tp], gvb[tp], dvb[tp], decay[tp], Stmp[tp]

                    def mmgk(e, ucols=ucols, pX=pX, xTb=xTb):
                        r = None
                        for k in range(8):
                            r = e.matmul(pX[:], lhsT=xTb[:, k, ucols], rhs=w_gk[:, k, :], start=(k == 0), stop=(k == 7))
                        return r
                    P.op("pe", mmgk, reads=xT_keys + ["w_gk"], writes=[kX], cost=1.9)
                    P.op("pe", lambda e, ucols=ucols, pZ=pZ, gaTb=gaTb: e.matmul(pZ[:], lhsT=gaTb[:, ucols], rhs=w_au[:], start=True, stop=True), reads=[("gaT", bp), "w_au"], writes=[kZ])
                    P.op("dve", lambda e, pZ=pZ, zb_=zb_: e.tensor_tensor(out=zb_[:], in0=pZ[:], in1=balpha[:], op=ALU.add), reads=[kZ, "balpha"], writes=[("zb", tp)])
                    P.op("act", lambda e, zb_=zb_, lt_=lt_: e.activation(out=lt_[:], in_=zb_[:], func=AF.Exp, scale=-1.0), reads=[("zb", tp)], writes=[("lt", tp)])
                    P.op("act", lambda e, lt_=lt_: e.activation(out=lt_[:], in_=lt_[:], func=AF.Ln, bias=1.0, scale=1.0), reads=[("lt", tp)], writes=[("lt", tp)])
                    P.op("pe", lambda e, pZ=pZ, lt_=lt_: e.matmul(pZ[:], lhsT=tri[:], rhs=lt_[:], start=True, stop=True), reads=["tri", ("lt", tp)], writes=[kZ], cost=1.0)
                    P.op("act", lambda e, pZ=pZ, enb_=enb_: e.activation(out=enb_[:], in_=pZ[:], func=AF.Exp, scale=-1.0), reads=[kZ], writes=[("enb", tp)])
                    P.op("dve", lambda e, pX=pX, enb_=enb_, ktil_=ktil_: e.tensor_tensor(out=ktil_[:], in0=pX[:], in1=enb_[:], op=ALU.mult), reads=[kX, ("enb", tp)], writes=[("ktil", tp)])

                    def mmbl(e, pZ=pZ, lt_=lt_):
                        r = None
                        for h in range(4):
                            r = e.matmul(pZ[:, h:h + 1], lhsT=lt_[:, h * 128:(h + 1) * 128], rhs=negs[:], start=True, stop=True)
                        return r
                    P.op("pe", mmbl, reads=[("lt", tp), "negs"], writes=[kZ], cost=0.8)
                    P.op("act", lambda e, pZ=pZ, decay_=decay_: e.activation(out=decay_[:], in_=pZ[:, 0:4], func=AF.Exp), reads=[kZ], writes=[("decay", tp)], cost=0.3)

                    def mmdv(e, ucols=ucols, pX=pX, xTb=xTb):
                        r = None
                        for k in range(8):
                            r = e.matmul(pX[:], lhsT=xTb[:, k, ucols], rhs=w_dv[:, k, :], start=(k == 0), stop=(k == 7))
                        return r
                    P.op("pe", mmdv, reads=xT_keys + ["w_dv"], writes=[kX], cost=1.9)
                    P.op("act", lambda e, pX=pX, dvb_=dvb_: e.copy(out=dvb_[:], in_=pX[:]), reads=[kX], writes=[("dvb", tp)])
                    P.op("sp", lambda e, tile=tile, dvb_=dvb_: e.dma_start(out=S["v2"][:, :, tile, :], in_=dvb_[:].rearrange("p (a c) -> p a c", a=4)),
                         reads=[("dvb", tp)], writes=[("v2_d", tile)], ndma=1)

                    def mmgv(e, ucols=ucols, xTb=xTb):
                        r = None
                        for nh in range(2):
                            for k in range(8):
                                r = e.matmul(psW[:, nh * 512:(nh + 1) * 512], lhsT=xTb[:, k, ucols], rhs=w_gv[:, k, nh * 512:(nh + 1) * 512], start=(k == 0), stop=(k == 7))
                        return r
                    P.op("pe", mmgv, reads=xT_keys + ["w_gv"], writes=["psW"], cost=3.8)
                    P.op("act", lambda e, gvb_=gvb_: e.copy(out=gvb_[:], in_=psW[:]), reads=["psW"], writes=[("gvb", tp)], cost=1.2)

                    def mmkv(e, ktil_=ktil_, gvb_=gvb_):
                        r = None
                        for h in range(4):
                            r = e.matmul(psW[:, h * 256:(h + 1) * 256], lhsT=ktil_[:, h * 128:(h + 1) * 128], rhs=gvb_[:, h * 256:(h + 1) * 256], start=True, stop=True)
                        return r
                    P.op("pe", mmkv, reads=[("ktil", tp), ("gvb", tp)], writes=["psW"], cost=0.8)
                    for h in range(4):
                        if u == 0:
                            P.op("pool", lambda e, h=h, Sselb=Sselb: e.tensor_scalar(out=Sselb[:, h, :], in0=Sst[:, h, :], scalar1=ej[:, 0:1], scalar2=None, op0=ALU.mult),
                                 reads=[("S", h), "ej"], writes=[("Ssel", bp, h)], cost=0.6)
                        else:
                            P.op("dve", lambda e, h=h, u=u, Sselb=Sselb: e.scalar_tensor_tensor(out=Sselb[:, h, :], in0=Sst[:, h, :], scalar=ej[:, u:u + 1], in1=Sselb[:, h, :],
                                                                                                 op0=ALU.mult, op1=ALU.add),
                                 reads=[("S", h), "ej", ("Ssel", bp, h)], writes=[("Ssel", bp, h)], cost=0.6)
                        P.op("dve", lambda e, h=h, Stmp_=Stmp_: e.tensor_tensor(out=Stmp_[:, h, :], in0=Sst[:, h, :], in1=psW[:, h * 256:(h + 1) * 256], op=ALU.add),
                             reads=[("S", h), "psW"], writes=[("Stmp", tp, h)], cost=0.4)
                        P.op("dve", lambda e, h=h, Stmp_=Stmp_, decay_=decay_: e.tensor_scalar(out=Sst[:, h, :], in0=Stmp_[:, h, :], scalar1=decay_[:, h:h + 1], scalar2=None, op0=ALU.mult),
                             reads=[("Stmp", tp, h), ("decay", tp)], writes=[("S", h)], cost=0.4)
                P.op("sp", lambda e, blk=blk, Sselb=Sselb: e.dma_start(out=S["Ssel"][blk], in_=Sselb[:]), reads=[("Ssel", bp, h) for h in range(4)], writes=[("Ssel_d", blk)], ndma=1)
            P.op("sp", lambda e: e.dma_start(out=S["ka"], in_=KA[:]), reads=["KA"], writes=["ka_d"], ndma=1)
            P.barrier()

    def phase_b1(self, I, S, nblk):
        nc, P = self.nc, self.P
        w_in = I["w_in"]
        with ExitStack() as st:
            W = {}
            specs = [("gq", 0, 512), ("gk", 512, 1024), ("gv", 1024, 2048), ("gr", 2048, 3072), ("ga", 3072, 3088), ("dq", 3088, 3600),
                     ("iq", 4624, 5136), ("iw", 5200, 5208), ("gta", 5208, 6232), ("gtb", 6232, 7256)]
            for nm, c0, c1 in specs:
                W[nm] = self.sb(st, "b1w_" + nm, [128, 8, c1 - c0], BF16)
                self.load_w(W[nm][:], w_in, c0, c1, "w_" + nm)
            w_au = self.sb(st, "b1w_au", [16, 512], BF16)
            balpha = self.sb(st, "b1balpha", [128, 512], F32)
            tri = self.sb(st, "b1tri", [128, 128], F32)
            gnorm = self.sb(st, "b1gnorm", [128, D], F32)
            maskT4 = self.sb(st, "b1maskT4", [128, 512], F32)
            xTo = [self.sb(st, "b1xTo%d" % i, [128, 8, 128], BF16) for i in range(2)]
            Sf = self.sb(st, "b1Sf", [128, 4, 256], F32)
            Sb = self.sb(st, "b1Sb", [128, 4, 256], BF16)
            gaTs = self.sb(st, "b1gaTs", [16, 128], BF16)
            zb = self.sb(st, "b1zb", [128, 512], F32)
            lt = self.sb(st, "b1lt", [128, 512], F32)
            eb = self.sb(st, "b1eb", [128, 512], F32)
            enb = self.sb(st, "b1enb", [128, 512], F32)
            qtil = self.sb(st, "b1qtil", [128, 512], BF16)
            ktil = self.sb(st, "b1ktil", [128, 512], BF16)
            gvb = self.sb(st, "b1gvb", [128, 1024], BF16)
            qT = self.sb(st, "b1qT", [128, 4, 128], BF16)
            kTl = self.sb(st, "b1kTl", [128, 4, 128], BF16)
            PT = self.sb(st, "b1PT", [128, 512], BF16)
            oaf = self.sb(st, "b1oaf", [128, D], F32)
            sgr = self.sb(st, "b1sgr", [128, D], F32)
            oanb = self.sb(st, "b1oanb", [128, D], BF16)
            sgab = self.sb(st, "b1sgab", [128, D], BF16)
            sgbb = self.sb(st, "b1sgbb", [128, D], BF16)
            dqTs = self.sb(st, "b1dqTs", [128, 4, 128], BF16)
            iqTs = self.sb(st, "b1iqTs", [128, 4, 128], BF16)
            iws = self.sb(st, "b1iws", [128, 8], F32)
            junk = self.sb(st, "b1junk", [128, D], BF16)
            stat = self.sb(st, "b1stat", [128, 64], F32)
            psT = [self.ps(st, "b1psT%d" % i, [128, 512], BF16) for i in range(2)]
            pA = self.ps(st, "b1pA", [128, 1024])
            pB = self.ps(st, "b1pB", [128, 1024])
            pX = self.ps(st, "b1pX", [128, 512])
            pZ = self.ps(st, "b1pZ", [128, 512])

            P.op("pool", lambda e: e.dma_start(out=w_au[:], in_=I["w_alpha_up"]), writes=["w_au"], ndma=1)
            P.op("sp", lambda e: e.dma_start(out=balpha[:], in_=I["b_alpha"].to_broadcast([128, 512])), writes=["balpha"], ndma=1)
            P.op("sp", lambda e: e.dma_start(out=tri[:], in_=I["tri"]), writes=["tri"], ndma=1)
            P.op("sp", lambda e: e.dma_start(out=gnorm[:], in_=I["gla_norm"].to_broadcast([128, D])), writes=["gnorm"], ndma=1)
            P.op("sp", lambda e: e.dma_start(out=maskT4[:], in_=I["maskT4"]), writes=["maskT4"], ndma=1)

            def tok_major(ps, wt, ncols, xk, xb, wkey):
                def mm(e):
                    r = None
                    for n0 in range(0, ncols, 512):
                        n1 = min(ncols, n0 + 512)
                        for k in range(8):
                            r = e.matmul(ps[:, n0:n1], lhsT=xb[:, k, :], rhs=wt[:, k, n0:n1], start=(k == 0), stop=(k == 7))
                    return r
                return mm

            for i in range(nblk):
                xb = xTo[i % 2]
                xk = ("xTo", i % 2)
                P.op("sp", lambda e, xb=xb, i=i: e.dma_start(out=xb[:], in_=S["xTown"][i]), writes=[xk], ndma=1)
                P.op("sp", lambda e, i=i: e.dma_start(out=Sf[:], in_=S["Ssel"][i]), writes=["Sf"], ndma=1)
                P.op("pool", lambda e: e.tensor_copy(out=Sb[:], in_=Sf[:]), reads=["Sf"], writes=["Sb"])
                def mmga(e, xb=xb):
                    r = None
                    for k in range(8):
                        r = e.matmul(pZ[0:16, 0:128], lhsT=W["ga"][:, k, :], rhs=xb[:, k, :], start=(k == 0), stop=(k == 7))
                    return r
                P.op("pe", mmga, reads=[xk, "w_ga"], writes=["pZ"])
                P.op("dve", lambda e: e.tensor_copy(out=gaTs[:], in_=pZ[0:16, 0:128]), reads=["pZ"], writes=["gaTs"])
                P.op("pe", lambda e: e.matmul(pZ[:], lhsT=gaTs[:], rhs=w_au[:], start=True, stop=True), reads=["gaTs", "w_au"], writes=["pZ"])
                P.op("dve", lambda e: e.tensor_tensor(out=zb[:], in0=pZ[:], in1=balpha[:], op=ALU.add), reads=["pZ", "balpha"], writes=["zb"])
                P.op("act", lambda e: e.activation(out=lt[:], in_=zb[:], func=AF.Exp, scale=-1.0), reads=["zb"], writes=["lt"])
                P.op("act", lambda e: e.activation(out=lt[:], in_=lt[:], func=AF.Ln, bias=1.0, scale=1.0), reads=["lt"], writes=["lt"])
                P.op("pe", lambda e: e.matmul(pZ[:], lhsT=tri[:], rhs=lt[:], start=True, stop=True), reads=["tri", "lt"], writes=["pZ"])
                P.op("act", lambda e: e.activation(out=eb[:], in_=pZ[:], func=AF.Exp), reads=["pZ"], writes=["eb"])
                P.op("act", lambda e: e.activation(out=enb[:], in_=pZ[:], func=AF.Exp, scale=-1.0), reads=["pZ"], writes=["enb"])
                P.op("pe", tok_major(pX, W["gq"], 512, xk, xb, "w_gq"), reads=[xk, "w_gq"], writes=["pX"])
                P.op("dve", lambda e: e.scalar_tensor_tensor(out=qtil[:], in0=pX[:], scalar=128.0 ** -0.5, in1=eb[:], op0=ALU.mult, op1=ALU.mult),
                     reads=["pX", "eb"], writes=["qtil"])
                P.op("pe", tok_major(pX, W["gk"], 512, xk, xb, "w_gk"), reads=[xk, "w_gk"], writes=["pX"])
                P.op("dve", lambda e: e.tensor_tensor(out=ktil[:], in0=pX[:], in1=enb[:], op=ALU.mult), reads=["pX", "enb"], writes=["ktil"])
                P.op("pe", tok_major(pA, W["gv"], 1024, xk, xb, "w_gv"), reads=[xk, "w_gv"], writes=["pA"])
                P.op("act", lambda e: e.copy(out=gvb[:], in_=pA[:]), reads=["pA"], writes=["gvb"])
                self.transposes8(qtil, psT, lambda g, n: qT[:, 0:n, :], ["qtil"], lambda g: "qT", nchunks=4)
                self.transposes8(ktil, [psT[1], psT[0]], lambda g, n: kTl[:, 0:n, :], ["ktil"], lambda g: "kTl", nchunks=4)

                def mmsc(e):
                    r = None
                    for h in range(4):
                        r = e.matmul(pX[:, h * 128:(h + 1) * 128], lhsT=kTl[:, h, :], rhs=qT[:, h, :], start=True, stop=True)
                    return r
                P.op("pe", mmsc, reads=["qT", "kTl"], writes=["pX"])
                P.op("dve", lambda e: e.tensor_tensor(out=PT[:], in0=pX[:], in1=maskT4[:], op=ALU.mult), reads=["pX", "maskT4"], writes=["PT"])

                def mmo(e):
                    r = None
                    for h in range(4):
                        e.matmul(pA[:, h * 256:(h + 1) * 256], lhsT=PT[:, h * 128:(h + 1) * 128], rhs=gvb[:, h * 256:(h + 1) * 256], start=True, stop=False)
                        r = e.matmul(pA[:, h * 256:(h + 1) * 256], lhsT=qT[:, h, :], rhs=Sb[:, h, :], start=False, stop=True)
                    return r
                P.op("pe", mmo, reads=["PT", "gvb", "qT", "Sb"], writes=["pA"])
                for h in range(4):
                    c0, k0 = self.rstd_of(pA[:, h * 256:(h + 1) * 256], 256, stat, 2 * h, "pA", "b1", junk[:, 0:256])
                    P.op("dve", lambda e, h=h, c0=c0: e.scalar_tensor_tensor(out=oaf[:, h * 256:(h + 1) * 256], in0=pA[:, h * 256:(h + 1) * 256], scalar=c0,
                                                                            in1=gnorm[:, h * 256:(h + 1) * 256], op0=ALU.mult, op1=ALU.mult),
                         reads=["pA", k0, "gnorm"], writes=[("oaf", h)])
                P.op("pe", tok_major(pB, W["gr"], 1024, xk, xb, "w_gr"), reads=[xk, "w_gr"], writes=["pB"])
                P.op("act", lambda e: e.activation(out=sgr[:], in_=pB[:], func=AF.Silu), reads=["pB"], writes=["sgr"])
                P.op("pool", lambda e: e.tensor_tensor(out=oanb[:], in0=oaf[:], in1=sgr[:], op=ALU.mult), reads=[("oaf", h) for h in range(4)] + ["sgr"], writes=["oanb"])
                P.op("sp", lambda e, i=i: e.dma_start(out=S["oan"][i * 128:(i + 1) * 128, :], in_=oanb[:]), reads=["oanb"], writes=[("oan_d", i)], ndma=1)
                P.op("pe", tok_major(pB, W["gta"], 1024, xk, xb, "w_gta"), reads=[xk, "w_gta"], writes=["pB"])
                P.op("act", lambda e: e.activation(out=sgab[:], in_=pB[:], func=AF.Sigmoid), reads=["pB"], writes=["sgab"])
                P.op("sp", lambda e, i=i: e.dma_start(out=S["sga"][i * 128:(i + 1) * 128, :], in_=sgab[:]), reads=["sgab"], writes=[("sga_d", i)], ndma=1)
                P.op("pe", tok_major(pA, W["gtb"], 1024, xk, xb, "w_gtb"), reads=[xk, "w_gtb"], writes=["pA"])
                P.op("act", lambda e: e.activation(out=sgbb[:], in_=pA[:], func=AF.Sigmoid), reads=["pA"], writes=["sgbb"])
                P.op("sp", lambda e, i=i: e.dma_start(out=S["sgb"][i * 128:(i + 1) * 128, :], in_=sgbb[:]), reads=["sgbb"], writes=[("sgb_d", i)], ndma=1)
                for nm, dstT, dkey in (("dq", dqTs, "dqT"), ("iq", iqTs, "iqT")):
                    def mmf(e, nm=nm, xb=xb):
                        r = None
                        for c in range(4):
                            for k in range(8):
                                r = e.matmul(pX[:, c * 128:(c + 1) * 128], lhsT=W[nm][:, k, c * 128:(c + 1) * 128], rhs=xb[:, k, :], start=(k == 0), stop=(k == 7))
                        return r
                    P.op("pe", mmf, reads=[xk, "w_" + nm], writes=["pX"])
                    P.op("dve", lambda e, dstT=dstT: e.tensor_scalar(out=dstT[:].rearrange("p c t -> p (c t)"), in0=pX[:], scalar1=0.125, scalar2=None, op0=ALU.mult),
                         reads=["pX"], writes=[dkey + "s"])
                    P.op("sp", lambda e, dstT=dstT, dkey=dkey, i=i: e.dma_start(out=S[dkey][i], in_=dstT[:]), reads=[dkey + "s"], writes=[(dkey + "_d", i)], ndma=1)

                def mmiw(e, xb=xb):
                    r = None
                    for k in range(8):
                        r = e.matmul(pZ[:, 0:8], lhsT=xb[:, k, :], rhs=W["iw"][:, k, :], start=(k == 0), stop=(k == 7))
                    return r
                P.op("pe", mmiw, reads=[xk, "w_iw"], writes=["pZ"])
                P.op("dve", lambda e: e.tensor_scalar(out=iws[:], in0=pZ[:, 0:8], scalar1=8.0 ** -0.5, scalar2=None, op0=ALU.mult), reads=["pZ"], writes=["iws"])
                P.op("sp", lambda e, i=i: e.dma_start(out=S["iw"][i * 128:(i + 1) * 128, :], in_=iws[:]), reads=["iws"], writes=[("iw_d", i)], ndma=1)
            P.barrier()

    def phase_b2(self, I, S, nblk, NIT=14):
        nc, P = self.nc, self.P
        SM = 512 * nblk
        with ExitStack() as st:
            tab = self.sb(st, "b2tab", [32, 8], F32)
            ohrev = self.sb(st, "b2ohrev", [32, 768], F32)
            Vs = self.sb(st, "b2Vs", [8, 768], F32)
            Biasf = self.sb(st, "b2Biasf", [128, 8, 640], F32)
            Biasb = self.sb(st, "b2Biasb", [128, 8, 640], BF16)
            cm = self.sb(st, "b2cm", [128, 512], F32)
            Dg = self.sb(st, "b2Dg", [128, 8, 128], BF16)
            dqT = self.sb(st, "b2dqT", [128, 4, 128], BF16)
            iqT = self.sb(st, "b2iqT", [128, 4, 128], BF16)
            iw = self.sb(st, "b2iw", [128, 8], F32)
            ik2 = [self.sb(st, "b2ik2_%d" % i, [128, 512], BF16) for i in range(2)]
            Rl = [self.sb(st, "b2R%d" % i, [128, 512], BF16) for i in range(2)]
            wk = self.sb(st, "b2wk", [128, SM], F32)
            madd = self.sb(st, "b2madd", [128, SM], BF16)
            madd_b = self.sb(st, "b2madd_b", [128, SM], BF16)
            dqT_b = self.sb(st, "b2dqT_b", [128, 4, 128], BF16)
            jk = self.sb(st, "b2jk", [128, SM], BF16)
            kTp = [self.sb(st, "b2kTp%d" % i, [128, SM], BF16) for i in range(2)]
            vp = [self.sb(st, "b2vp%d" % i, [128, SM // 128, 128], BF16) for i in range(2)]
            tmn = self.sb(st, "b2tmn", [128, 512], F32)
            Pg = [self.sb(st, "b2Pg%d" % i, [128, 512], BF16) for i in range(3)]
            PTs = [self.sb(st, "b2PT%d" % i, [128, 4, 128], BF16) for i in range(2)]
            ob = self.sb(st, "b2ob", [128, 512], BF16)
            sc = self.sb(st, "b2sc", [128, 16], F32)
            pw = self.sb(st, "b2pw", [128, NIT], F32)
            steps = self.sb(st, "b2steps", [128, NIT], F32)
            mids = self.sb(st, "b2mids", [128, NIT + 1], F32)
            cntd = self.sb(st, "b2cntd", [128, NIT], F32)
            cnta = self.sb(st, "b2cnta", [128, NIT], F32)
            mg = [self.sb(st, "b2mg%d" % i, [128, 16], F32) for i in range(2)]
            lcol = [self.sb(st, "b2lcol%d" % i, [128, 16], F32) for i in range(2)]
            psD = [self.ps(st, "b2psD%d" % i, [128, 512]) for i in range(2)]
            psI = self.ps(st, "b2psI", [128, 512])
            psQ = [self.ps(st, "b2psQ%d" % i, [128, 512]) for i in range(2)]
            psT = [self.ps(st, "b2psT%d" % i, [128, 512], BF16) for i in range(2)]
            po = self.ps(st, "b2po", [128, 512])
            psM = psD[0]

            P.op("sp", lambda e: e.dma_start(out=tab[:], in_=I["rel_bias_table"]), writes=["tab"], ndma=1)
            P.op("sp", lambda e: e.dma_start(out=ohrev[:], in_=I["ohrev"]), writes=["ohrev"], ndma=1)
            P.op("sp", lambda e: e.dma_start(out=cm[:], in_=I["cm"]), writes=["cm"], ndma=1)
            for k in range(NIT):
                P.op("pool", lambda e, k=k: e.memset(pw[:, k:k + 1], 2.0 ** -(k + 1)), writes=[("pw", k)])
            pw_keys = [("pw", k) for k in range(NIT)]

            def mmv(e):
                e.matmul(psI[0:8, 0:384], lhsT=tab[:], rhs=ohrev[:, 0:384], start=True, stop=True)
                return e.matmul(psQ[0][0:8, 0:384], lhsT=tab[:], rhs=ohrev[:, 384:768], start=True, stop=True)
            P.op("pe", mmv, reads=["tab", "ohrev"], writes=["psI", ("psQ", 0)])
            P.op("dve", lambda e: e.tensor_copy(out=Vs[:, 0:384], in_=psI[0:8, 0:384]), reads=["psI"], writes=["Vs0"])
            P.op("dve", lambda e: e.tensor_copy(out=Vs[:, 384:768], in_=psQ[0][0:8, 0:384]), reads=[("psQ", 0)], writes=["Vs1"])
            P.op("sp", lambda e: e.dma_start(out=S["vbias"], in_=Vs[:]), reads=["Vs0", "Vs1"], writes=["vbias_d"], ndma=1)
            for r0 in range(0, 128, 16):
                def ld(e, r0=r0):
                    out = []
                    for r in range(r0, r0 + 16):
                        out.append(e.dma_start(out=Biasf[r:r + 1, :, :], in_=S["vbias"][:, 127 - r:127 - r + 640].unsqueeze(0)))
                    return out
                P.op("sp" if (r0 // 16) % 2 == 0 else "pool", ld, reads=["vbias_d"], writes=[("Biasf", r0)], ndma=16)
            P.op("dve", lambda e: e.tensor_copy(out=Biasb[:], in_=Biasf[:]), reads=[("Biasf", r0) for r0 in range(0, 128, 16)], writes=["Biasb"])

            st_ = {"ikc": 0, "pgc": 0}
            KAf = self.sb(st, "b2KAf", [128, 4], F32)
            KAblk = self.sb(st, "b2KAblk", [128, 4, 2], BF16)
            absq = self.sb(st, "b2absq", [128, 4, 128], BF16)
            negb2 = [self.sb(st, "b2negb%d" % i_, [128, 8], F32) for i_ in range(2)]
            P.op("sp", lambda e: e.dma_start(out=KAf[:], in_=S["ka"]), writes=["KAf"], ndma=1)
            P.op("dve", lambda e: e.memset(KAblk[:], 0.0), writes=["KAblk"])
            P.op("dve", lambda e: e.tensor_copy(out=KAblk[0:64, :, 0], in_=KAf[0:64, :]), reads=["KAf", "KAblk"], writes=["KAblk"])
            P.op("dve", lambda e: e.tensor_copy(out=KAblk[64:128, :, 1], in_=KAf[64:128, :]), reads=["KAf", "KAblk"], writes=["KAblk"])
            madd2 = [madd, madd_b]
            dqT2 = [dqT, dqT_b]

            def stageX(i):
                Si = 512 * (i + 1)
                ng = i + 1
                par = i % 2
                maddc = madd2[par]
                dq_ = dqT2[par]
                P.op("sp", lambda e, i=i, dq_=dq_: e.dma_start(out=dq_[:], in_=S["dqT"][i]), writes=[("dqT", par)], ndma=1)
                P.op("sp", lambda e, i=i: e.dma_start(out=iqT[:], in_=S["iqT"][i]), writes=["iqT"], ndma=1)
                P.op("sp", lambda e, i=i: e.dma_start(out=iw[:], in_=S["iw"][i * 128:(i + 1) * 128, :]), writes=["iw"], ndma=1)
                for h in range(8):
                    P.op("pool", lambda e, h=h: e.tensor_scalar(out=Dg[:, h, :], in0=self.idf[:], scalar1=iw[:, h:h + 1], scalar2=None, op0=ALU.mult),
                         reads=["idf", "iw"], writes=[("Dg", h)], cost=0.4)
                P.op("act", lambda e, dq_=dq_: e.activation(out=absq[:].rearrange("p c t -> p (c t)"), in_=dq_[:].rearrange("p c t -> p (c t)"), func=AF.Abs),
                     reads=[("dqT", par)], writes=["absq"], cost=0.6)

                def mmb(e):
                    r = None
                    for p in range(4):
                        r = e.matmul(psI[:, 2 * p:2 * p + 2], lhsT=absq[:, p, :], rhs=KAblk[:, p, :], start=True, stop=True)
                    return r
                P.op("pe", mmb, reads=["absq", "KAblk"], writes=["psI"], cost=0.4)
                nb_ = negb2[par]
                P.op("dve", lambda e, nb_=nb_: e.tensor_scalar(out=nb_[:], in0=psI[:, 0:8], scalar1=-1.0, scalar2=None, op0=ALU.mult), reads=["psI"], writes=[("negb", par)], cost=0.1)
                yield
                for g in range(ng):
                    ikb = ik2[st_["ikc"] % 2]
                    kik = ("ik2", st_["ikc"] % 2)
                    st_["ikc"] += 1

                    def ldik(e, ikb=ikb, g=g):
                        a_ = e.dma_start(out=ikb[0:64, :], in_=S["ikT"][:, g * 512:(g + 1) * 512])
                        b_ = e.dma_start(out=ikb[64:128, :], in_=S["ikT"][:, g * 512:(g + 1) * 512])
                        return [a_, b_]
                    P.op("sp", ldik, writes=[kik], ndma=2)
                    for h in range(8):
                        hp = h % 2
                        pd = psD[h % 2]
                        P.op("pe", lambda e, h=h, hp=hp, pd=pd, ikb=ikb: e.matmul(pd[:], lhsT=iqT[hp * 64:(hp + 1) * 64, h // 2, :], rhs=ikb[hp * 64:(hp + 1) * 64, :],
                                                                                  start=True, stop=True),
                             reads=["iqT", kik], writes=[("psD", h % 2)], cost=0.25)
                        rl_ = Rl[h % 2]
                        if h % 2 == 0:
                            P.op("act", lambda e, rl_=rl_, pd=pd: e.activation(out=rl_[:], in_=pd[:], func=AF.Relu), reads=[("psD", h % 2)], writes=[("R", h % 2)], cost=0.6)
                        else:
                            P.op("dve", lambda e, rl_=rl_, pd=pd: e.tensor_scalar(out=rl_[:], in0=pd[:], scalar1=0.0, scalar2=None, op0=ALU.max),
                                 reads=[("psD", h % 2)], writes=[("R", h % 2)], cost=0.6)
                        P.op("pe", lambda e, h=h, rl_=rl_: e.matmul(psI[:], lhsT=Dg[:, h, :], rhs=rl_[:], start=(h == 0), stop=(h == 7)),
                             reads=[("Dg", h), ("R", h % 2)], writes=["psI"], cost=0.25)
                    gs = slice(g * 512, (g + 1) * 512)
                    if g < ng - 1:
                        P.op("act", lambda e, gs=gs: e.copy(out=wk[:, gs], in_=psI[:]), reads=["psI"], writes=[("wk", g)], cost=0.6)
                    else:
                        P.op("dve", lambda e, gs=gs: e.tensor_tensor(out=wk[:, gs], in0=psI[:], in1=cm[:], op=ALU.add), reads=["psI", "cm"], writes=[("wk", g)], cost=0.6)
                        P.op("dve", lambda e: e.tensor_tensor(out=tmn[:], in0=psI[:], in1=cm[:], op=ALU.subtract), reads=["psI", "cm"], writes=["tmn"], cost=0.6)
                    yield
                wk_keys = [("wk", g) for g in range(ng)]
                LO, W_, TT, SG, HI, MN1, MN2, THR = [sc[:, c:c + 1] for c in range(8)]
                big = Si / 960.0 + 0.1
                P.op("dve", lambda e, Si=Si: e.tensor_reduce(out=HI, in_=wk[:, 0:Si], axis=AX.X, op=ALU.max), reads=wk_keys, writes=["hi"], cost=big)
                P.op("dve", lambda e: e.tensor_reduce(out=MN1, in_=tmn[:], axis=AX.X, op=ALU.min), reads=["tmn"], writes=["mn1"], cost=0.6)
                if i > 0:
                    P.op("dve", lambda e, Si=Si: e.tensor_reduce(out=MN2, in_=wk[:, 0:Si - 512], axis=AX.X, op=ALU.min), reads=wk_keys, writes=["mn2"], cost=big)
                    P.op("dve", lambda e: e.tensor_tensor(out=MN1, in0=MN1, in1=MN2, op=ALU.min), reads=["mn1", "mn2"], writes=["mn1"], cost=0.1)
                P.op("dve", lambda e: e.tensor_scalar(out=LO, in0=MN1, scalar1=-1.0, scalar2=None, op0=ALU.add), reads=["mn1"], writes=["lo"], cost=0.1)
                P.op("dve", lambda e: e.scalar_tensor_tensor(out=W_, in0=HI, scalar=1.0, in1=LO, op0=ALU.add, op1=ALU.subtract), reads=["hi", "lo"], writes=["w"], cost=0.1)
                P.op("dve", lambda e: e.tensor_scalar(out=steps[:], in0=pw[:], scalar1=W_, scalar2=None, op0=ALU.mult), reads=pw_keys + ["w"], writes=["steps"], cost=0.1)
                P.op("dve", lambda e: e.tensor_tensor(out=mids[:, 0:1], in0=LO, in1=steps[:, 0:1], op=ALU.add), reads=["lo", "steps"], writes=[("mid", 0)], cost=0.1)
                P.op("pool", lambda e: e.memset(cntd[:], 0.0), writes=["cntd"] + [("cntd", it) for it in range(NIT)], cost=0.2)
                P.op("pool", lambda e: e.memset(cnta[:], 0.0), writes=["cnta"] + [("cnta", it) for it in range(NIT)], cost=0.2)
                yield
                Sh = (Si // 2 + 127) // 128 * 128
                n2 = Si - Sh
                half = Sh / 960.0 + 0.1
                for it in range(NIT):
                    MID = mids[:, it:it + 1]
                    P.op("act", lambda e, it=it, Sh=Sh, Si=Si, MID=MID: e.activation(out=jk[:, Sh:Si], in_=wk[:, Sh:Si], func=AF.Sign, bias=MID, scale=-1.0, accum_out=cnta[:, it:it + 1]),
                         reads=wk_keys + [("mid", it), "cnta"], writes=["jka", ("cnta", it)], cost=half)
                    P.op("dve", lambda e, it=it, Sh=Sh, MID=MID: e.tensor_scalar(out=jk[:, 0:Sh], in0=wk[:, 0:Sh], scalar1=MID, scalar2=0.0, op0=ALU.is_ge, op1=ALU.add,
                                                                                accum_out=cntd[:, it:it + 1]),
                         reads=wk_keys + [("mid", it), "cntd"], writes=["jkd", ("cntd", it)], cost=half)
                    P.op("dve", lambda e, it=it: e.scalar_tensor_tensor(out=TT, in0=cnta[:, it:it + 1], scalar=-0.5, in1=cntd[:, it:it + 1], op0=ALU.mult, op1=ALU.add),
                         reads=[("cnta", it), ("cntd", it)], writes=["tt"], cost=0.1)
                    P.op("dve", lambda e, n2=n2: e.tensor_scalar(out=SG, in0=TT, scalar1=255.5 - n2 / 2.0, scalar2=-0.5, op0=ALU.is_ge, op1=ALU.add), reads=["tt"], writes=["sg"], cost=0.1)
                    P.op("dve", lambda e, it=it, MID=MID: e.scalar_tensor_tensor(out=mids[:, it + 1:it + 2], in0=steps[:, it:it + 1], scalar=SG, in1=MID, op0=ALU.mult, op1=ALU.add),
                         reads=["steps", "sg", ("mid", it)], writes=[("mid", it + 1)], cost=0.1)
                    yield
                P.op("dve", lambda e: e.scalar_tensor_tensor(out=THR, in0=steps[:, NIT - 1:NIT], scalar=-0.5, in1=mids[:, NIT:NIT + 1], op0=ALU.mult, op1=ALU.add),
                     reads=["steps", ("mid", NIT)], writes=["thr"], cost=0.1)
                P.op("dve", lambda e, Si=Si, maddc=maddc: e.tensor_scalar(out=maddc[:, 0:Si], in0=wk[:, 0:Si], scalar1=THR, scalar2=NEG, op0=ALU.is_lt, op1=ALU.mult),
                     reads=wk_keys + ["thr"], writes=[("madd", par)], cost=big)
                yield

            def stageY(i):
                Si = 512 * (i + 1)
                ng = i + 1
                par = i % 2
                maddc = madd2[par]
                dq_ = dqT2[par]
                kdq = ("dqT", par)
                kmadd = ("madd", par)
                nkb = Si // 128

                def pass1(h, kb_, kk, p, rows):
                    mgb = mg[h % 2]
                    for g in range(ng):
                        pq = psM
                        gs = slice(g * 512, (g + 1) * 512)
                        P.op("pe", lambda e, pq=pq, rows=rows, p=p, kb_=kb_, gs=gs: e.matmul(pq[:], lhsT=dq_[rows, p, :], rhs=kb_[rows, gs], start=True, stop=True),
                             reads=[kdq, kk], writes=[("psD", 0)], cost=0.25)
                        P.op("dve", lambda e, pq=pq, g=g, mgb=mgb: e.tensor_reduce(out=mgb[:, g:g + 1], in_=pq[:], axis=AX.X, op=ALU.max),
                             reads=[("psD", 0)], writes=[("mg", h % 2, g)], cost=0.6)
                    if ng > 1:
                        P.op("dve", lambda e, mgb=mgb, ng=ng: e.tensor_reduce(out=mgb[:, 15:16], in_=mgb[:, 0:ng], axis=AX.X, op=ALU.max),
                             reads=[("mg", h % 2, g) for g in range(ng)], writes=[("m", h % 2)], cost=0.1)
                    else:
                        P.op("dve", lambda e, mgb=mgb: e.tensor_copy(out=mgb[:, 15:16], in_=mgb[:, 0:1]),
                             reads=[("mg", h % 2, 0)], writes=[("m", h % 2)], cost=0.1)
                    P.op("dve", lambda e, mgb=mgb: e.tensor_scalar(out=mgb[:, 14:15], in0=mgb[:, 15:16], scalar1=-1.0, scalar2=None, op0=ALU.mult),
                         reads=[("m", h % 2)], writes=[("negm", h % 2)], cost=0.1)

                loaded = {}

                def load_pair(p):
                    kb_ = kTp[p % 2]
                    vb_ = vp[p % 2]
                    kk = ("kTp", p % 2)
                    kv = ("vp", p % 2)
                    dc = Si * 256 * 128 / 150e3 + 2.0
                    P.op("sp", lambda e, kb_=kb_, p=p, Si=Si: e.dma_start(out=kb_[:, 0:Si], in_=S["kT"][:, p, 0:Si]), writes=[kk], ndma=1, cost=dc)
                    P.op("pool", lambda e, vb_=vb_, p=p, nkb=nkb: e.dma_start(out=vb_[:, 0:nkb, :], in_=S["v2"][:, p, 0:nkb, :]), writes=[kv], ndma=1, cost=dc)
                    loaded[p] = (kb_, vb_, kk, kv)

                load_pair(0)
                nb_ = negb2[par]
                for h in range(8):
                    p, hh = h // 2, h % 2
                    rows = slice(hh * 64, (hh + 1) * 64)
                    kb_, vb_, kk, kv = loaded[p]
                    if hh == 0 and p + 1 < 4:
                        load_pair(p + 1)
                    mgb = mg[h % 2]
                    lc = lcol[h % 2]
                    P.op("pool", lambda e, lc=lc: e.memset(lc[:], 0.0), writes=[("lcol", h % 2)] + [("lc", h % 2, g) for g in range(ng)], cost=0.2)
                    for g in range(ng):
                        pq = psQ[g % 2]
                        gs = slice(g * 512, (g + 1) * 512)

                        def qk2(e, pq=pq, rows=rows, p=p, kb_=kb_, gs=gs, g=g, h=h, ng=ng):
                            e.matmul(pq[:], lhsT=dq_[rows, p, :], rhs=kb_[rows, gs], start=True, stop=False)
                            if g == ng - 1:
                                e.matmul(pq[:], lhsT=self.idb[:], rhs=Biasb[:, h, 128:640], start=False, stop=False)
                            elif g == ng - 2:
                                e.matmul(pq[:, 384:512], lhsT=self.idb[:], rhs=Biasb[:, h, 0:128], start=False, stop=False)
                            return e.matmul(pq[:], lhsT=self.idb[:], rhs=maddc[:, gs], start=False, stop=True)
                        P.op("pe", qk2, reads=[kdq, kk, "idb", "Biasb", kmadd], writes=[("psQ", g % 2)], cost=0.5)
                        pgb = Pg[st_["pgc"] % 3]
                        kpg = ("Pg", st_["pgc"] % 3)
                        st_["pgc"] += 1
                        P.op("act", lambda e, pq=pq, pgb=pgb, nb_=nb_, lc=lc, g=g, h=h: e.activation(out=pgb[:], in_=pq[:], func=AF.Exp, bias=nb_[:, h:h + 1], scale=1.0,
                                                                                                     accum_out=lc[:, g:g + 1]),
                             reads=[("psQ", g % 2), ("negb", par), ("lcol", h % 2)], writes=[kpg, ("lc", h % 2, g)], cost=0.65)
                        qi = g % 2

                        def tr(e, pgb=pgb, qi=qi):
                            r = None
                            for c in range(4):
                                r = e.transpose(out=psT[qi][:, c * 128:(c + 1) * 128], in_=pgb[:, c * 128:(c + 1) * 128], identity=self.idb[:])
                            return r
                        P.op("pe", tr, reads=[kpg, "idb"], writes=[("psT", qi)], cost=0.3)
                        if qi == 0:
                            P.op("dve", lambda e, qi=qi: e.tensor_copy(out=PTs[qi][:].rearrange("p c t -> p (c t)"), in_=psT[qi][:]), reads=[("psT", qi)], writes=[("PT", qi)], cost=0.6)
                        else:
                            P.op("act", lambda e, qi=qi: e.copy(out=PTs[qi][:].rearrange("p c t -> p (c t)"), in_=psT[qi][:]), reads=[("psT", qi)], writes=[("PT", qi)], cost=0.6)

                        def pv(e, g=g, qi=qi, hh=hh, vb_=vb_, nkb=nkb):
                            r = None
                            for c in range(4):
                                kb = 4 * g + c
                                r = e.matmul(po[:, 0:64], lhsT=PTs[qi][:, c, :], rhs=vb_[:, kb, hh * 64:(hh + 1) * 64], start=(kb == 0), stop=(kb == nkb - 1))
                            return r
                        P.op("pe", pv, reads=[("PT", qi), kv], writes=["po"], cost=0.45)
                    LS, RL_ = sc[:, 8:9], sc[:, 9:10]
                    P.op("dve", lambda e, lc=lc: e.tensor_reduce(out=LS, in_=lc[:, 0:16], axis=AX.X, op=ALU.add), reads=[("lc", h % 2, g) for g in range(ng)] + [("lcol", h % 2)], writes=["ls"], cost=0.1)
                    P.op("dve", lambda e: e.reciprocal(out=RL_, in_=LS), reads=["ls"], writes=["rl"], cost=0.1)
                    P.op("dve", lambda e, h=h: e.tensor_scalar(out=ob[:, h * 64:(h + 1) * 64], in0=po[:, 0:64], scalar1=RL_, scalar2=None, op0=ALU.mult),
                         reads=["po", "rl"], writes=[("ob", h)], cost=0.15)
                    yield
                P.op("sp", lambda e, i=i: e.dma_start(out=S["ob"][i * 128:(i + 1) * 128, :], in_=ob[:]), reads=[("ob", h) for h in range(8)], writes=[("ob_d", i)], ndma=1)

            for _ in stageX(0):
                pass
            for i in range(nblk):
                gx = stageX(i + 1) if i + 1 < nblk else None
                nchunks = (2 + (i + 2) + 1 + NIT + 1) if gx is not None else 0
                per = (nchunks + 7) // 8
                for _ in stageY(i):
                    if gx is not None:
                        for _k in range(per):
                            try:
                                next(gx)
                            except StopIteration:
                                gx = None
                                break
                if gx is not None:
                    for _ in gx:
                        pass
            P.barrier()

    def phase_b3(self, I, S, nblk):
        nc, P = self.nc, self.P
        with ExitStack() as st:
            wgp = self.sb(st, "b3wgp", [128, 8, D], BF16)
            wdp = self.sb(st, "b3wdp", [128, 4, D], BF16)
            wout = self.sb(st, "b3wout", [128, 8, D], BF16)
            wcq = self.sb(st, "b3wcq", [128, 8, 512], BF16)
            wckv = self.sb(st, "b3wckv", [128, 8, D], BF16)
            wco = self.sb(st, "b3wco", [128, 4, D], BF16)
            G = {}
            for nm in ("mix_post", "cross_pre", "cross_post", "mem_norm"):
                G[nm] = self.sb(st, "b3g_" + nm, [128, D], F32)
                P.op("sp", lambda e, nm=nm: e.dma_start(out=G[nm][:], in_=I[nm].to_broadcast([128, D])), writes=["g_" + nm], ndma=1)
            self.load_w(wgp[:], I["w_gla_proj"], 0, D, "wgp")
            self.load_w(wdp[:], I["w_dsa_proj"], 0, D, "wdp", kchunks=4)
            self.load_w(wout[:], I["w_out"], 0, D, "wout")
            self.load_w(wcq[:], I["w_cq"], 0, 512, "wcq")
            self.load_w(wckv[:], I["w_ckv"], 0, D, "wckv")
            self.load_w(wco[:], I["w_co"], 0, D, "wco", kchunks=4)
            memf = self.sb(st, "b3memf", [128, D], F32)
            xnb = self.sb(st, "b3xnb", [128, D], BF16)
            memT = self.sb(st, "b3memT", [128, 8, 256], BF16)
            kmT = self.sb(st, "b3kmT", [128, 4, 256], BF16)
            vm = self.sb(st, "b3vm", [128, 2, 512], BF16)
            oanb = self.sb(st, "b3oanb", [128, D], BF16)
            obb = self.sb(st, "b3obb", [128, 512], BF16)
            sga = self.sb(st, "b3sga", [128, D], BF16)
            sgb = self.sb(st, "b3sgb", [128, D], BF16)
            h1o = self.sb(st, "b3h1o", [128, D], F32)
            oT = self.sb(st, "b3oT", [128, 8, 128], BF16)
            obT = self.sb(st, "b3obT", [128, 4, 128], BF16)
            gay = self.sb(st, "b3gay", [128, D], F32)
            t1 = self.sb(st, "b3t1", [128, D], F32)
            mrg = self.sb(st, "b3mrg", [128, D], BF16)
            mT = self.sb(st, "b3mT", [128, 8, 128], BF16)
            tmp = self.sb(st, "b3tmp", [128, D], F32)
            h2t = self.sb(st, "b3h2t", [128, D], F32)
            h3t = self.sb(st, "b3h3t", [128, D], F32)
            xcT = self.sb(st, "b3xcT", [128, 8, 128], BF16)
            qcT = self.sb(st, "b3qcT", [128, 4, 128], BF16)
            Pc = self.sb(st, "b3Pc", [128, D], BF16)
            PcT = self.sb(st, "b3PcT", [128, 8, 128], BF16)
            ox = self.sb(st, "b3ox", [128, 512], BF16)
            oxT = self.sb(st, "b3oxT", [128, 4, 128], BF16)
            junk = self.sb(st, "b3junk", [128, D], BF16)
            stat = self.sb(st, "b3stat", [128, 64], F32)
            sc = self.sb(st, "b3sc", [128, 16], F32)
            psT = [self.ps(st, "b3psT%d" % i, [128, 512], BF16) for i in range(2)]
            pA = self.ps(st, "b3pA", [128, 1024])
            pB = self.ps(st, "b3pB", [128, 1024])
            pX = self.ps(st, "b3pX", [128, 512])

            def proj(ps, xT_, wt, nk, keys):
                def mm(e):
                    r = None
                    for nh in range(2):
                        for k in range(nk):
                            r = e.matmul(ps[:, nh * 512:(nh + 1) * 512], lhsT=xT_[:, k, :], rhs=wt[:, k, nh * 512:(nh + 1) * 512], start=(k == 0), stop=(k == nk - 1))
                    return r
                return mm

            for mc in range(2):
                P.op("sp", lambda e, mc=mc: e.dma_start(out=memf[:], in_=I["mem"][mc * 128:(mc + 1) * 128, :]), writes=["memf"], ndma=1)
                c0, k0 = self.rstd_of(memf[:], D, stat, 2 * mc, "memf", "b3m", junk[:])
                P.op("dve", lambda e, c0=c0: e.scalar_tensor_tensor(out=xnb[:], in0=memf[:], scalar=c0, in1=G["mem_norm"][:], op0=ALU.mult, op1=ALU.mult),
                     reads=["memf", k0, "g_mem_norm"], writes=["xnb"])
                self.transposes8(xnb, psT, lambda g, n, mc=mc: memT[:, 4 * g:4 * g + n, mc * 128:(mc + 1) * 128], ["xnb"], lambda g, mc=mc: ("memT", mc, g))
            memT_keys = [("memT", mc, g) for mc in range(2) for g in range(2)]

            def mmk(e):
                r = None
                for h in range(4):
                    for k in range(8):
                        r = e.matmul(pA[:, h * 256:(h + 1) * 256], lhsT=wckv[:, k, h * 128:(h + 1) * 128], rhs=memT[:, k, :], start=(k == 0), stop=(k == 7))
                return r
            P.op("pe", mmk, reads=memT_keys + ["wckv"], writes=["pA"])
            P.op("act", lambda e: e.copy(out=kmT[:].rearrange("p h m -> p (h m)"), in_=pA[:]), reads=["pA"], writes=["kmT"])
            for mc in range(2):
                def mmvm(e, mc=mc):
                    r = None
                    for k in range(8):
                        r = e.matmul(pB[:, 0:512], lhsT=memT[:, k, mc * 128:(mc + 1) * 128], rhs=wckv[:, k, 512:1024], start=(k == 0), stop=(k == 7))
                    return r
                P.op("pe", mmvm, reads=memT_keys + ["wckv"], writes=["pB"])
                P.op("act", lambda e, mc=mc: e.copy(out=vm[:, mc, :], in_=pB[:, 0:512]), reads=["pB"], writes=[("vm", mc)])
            vm_keys = [("vm", 0), ("vm", 1)]

            for i in range(nblk):
                rs = slice(i * 128, (i + 1) * 128)
                P.op("sp", lambda e, rs=rs: e.dma_start(out=oanb[:], in_=S["oan"][rs, :]), writes=["oanb"], ndma=1)
                P.op("sp", lambda e, rs=rs: e.dma_start(out=obb[:], in_=S["ob"][rs, :]), writes=["obb"], ndma=1)
                P.op("sp", lambda e, rs=rs: e.dma_start(out=sga[:], in_=S["sga"][rs, :]), writes=["sga"], ndma=1)
                P.op("sp", lambda e, rs=rs: e.dma_start(out=sgb[:], in_=S["sgb"][rs, :]), writes=["sgb"], ndma=1)
                P.op("sp", lambda e, rs=rs: e.dma_start(out=h1o[:], in_=S["h1own"][rs, :]), writes=["h1o"], ndma=1)
                self.transposes8(oanb, psT, lambda g, n: oT[:, 4 * g:4 * g + n, :], ["oanb"], lambda g: ("oT", g))
                P.op("pe", proj(pA, oT, wgp, 8, None), reads=[("oT", 0), ("oT", 1), "wgp"], writes=["pA"])
                P.op("dve", lambda e: e.tensor_tensor(out=gay[:], in0=pA[:], in1=sga[:], op=ALU.mult), reads=["pA", "sga"], writes=["gay"])
                self.transposes8(obb, psT, lambda g, n: obT[:, 0:n, :], ["obb"], lambda g: "obT", nchunks=4)
                P.op("pe", proj(pB, obT, wdp, 4, None), reads=["obT", "wdp"], writes=["pB"])
                P.op("dve", lambda e: e.tensor_tensor(out=t1[:], in0=pB[:], in1=sgb[:], op=ALU.mult), reads=["pB", "sgb"], writes=["t1"])
                P.op("pool", lambda e: e.tensor_tensor(out=mrg[:], in0=t1[:], in1=gay[:], op=ALU.add), reads=["t1", "gay"], writes=["mrg"])
                self.transposes8(mrg, psT, lambda g, n: mT[:, 4 * g:4 * g + n, :], ["mrg"], lambda g: ("mT", g))
                P.op("pe", proj(pA, mT, wout, 8, None), reads=[("mT", 0), ("mT", 1), "wout"], writes=["pA"])
                c0, k0 = self.rstd_of(pA[:], D, stat, 8, "pA", "b3a", junk[:])
                P.op("dve", lambda e, c0=c0: e.scalar_tensor_tensor(out=tmp[:], in0=pA[:], scalar=c0, in1=G["mix_post"][:], op0=ALU.mult, op1=ALU.mult),
                     reads=["pA", k0, "g_mix_post"], writes=["tmp"])
                P.op("pool", lambda e: e.tensor_tensor(out=h2t[:], in0=tmp[:], in1=h1o[:], op=ALU.add), reads=["tmp", "h1o"], writes=["h2t"])
                c0, k0 = self.rstd_of(h2t[:], D, stat, 10, "h2t", "b3b", junk[:])
                P.op("dve", lambda e, c0=c0: e.scalar_tensor_tensor(out=xnb[:], in0=h2t[:], scalar=c0, in1=G["cross_pre"][:], op0=ALU.mult, op1=ALU.mult),
                     reads=["h2t", k0, "g_cross_pre"], writes=["xnb"])
                self.transposes8(xnb, psT, lambda g, n: xcT[:, 4 * g:4 * g + n, :], ["xnb"], lambda g: ("xcT", g))

                def mmq(e):
                    r = None
                    for h in range(4):
                        for k in range(8):
                            r = e.matmul(pX[:, h * 128:(h + 1) * 128], lhsT=wcq[:, k, h * 128:(h + 1) * 128], rhs=xcT[:, k, :], start=(k == 0), stop=(k == 7))
                    return r
                P.op("pe", mmq, reads=[("xcT", 0), ("xcT", 1), "wcq"], writes=["pX"])
                P.op("dve", lambda e: e.tensor_scalar(out=qcT[:].rearrange("p h t -> p (h t)"), in0=pX[:], scalar1=128.0 ** -0.5, scalar2=None, op0=ALU.mult),
                     reads=["pX"], writes=["qcT"])

                def mml(e):
                    r = None
                    for h in range(4):
                        r = e.matmul(pB[:, h * 256:(h + 1) * 256], lhsT=qcT[:, h, :], rhs=kmT[:, h, :], start=True, stop=True)
                    return r
                P.op("pe", mml, reads=["qcT", "kmT"], writes=["pB"])
                MX, NMX, LC, RLC = sc[:, 0:4], sc[:, 4:8], sc[:, 8:12], sc[:, 12:16]
                P.op("dve", lambda e: e.tensor_reduce(out=MX, in_=pB[:].rearrange("p (h m) -> p h m", h=4), axis=AX.X, op=ALU.max), reads=["pB"], writes=["mx"])
                P.op("dve", lambda e: e.tensor_scalar(out=NMX, in0=MX, scalar1=-1.0, scalar2=None, op0=ALU.mult), reads=["mx"], writes=["nmx"])
                P.op("dve", lambda e: e.memset(LC, 0.0), writes=["lc"])
                for h in range(4):
                    P.op("act", lambda e, h=h: e.activation(out=Pc[:, h * 256:(h + 1) * 256], in_=pB[:, h * 256:(h + 1) * 256], func=AF.Exp, bias=sc[:, 4 + h:5 + h], scale=1.0,
                                                            accum_out=sc[:, 8 + h:9 + h]),
                         reads=["pB", "nmx", "lc"], writes=[("Pc", h), ("lc", h)])
                self.transposes8(Pc, psT, lambda g, n: PcT[:, 4 * g:4 * g + n, :], [("Pc", h) for h in range(4)], lambda g: ("PcT", g))

                def mmov(e):
                    r = None
                    for h in range(4):
                        for mc in range(2):
                            r = e.matmul(pX[:, h * 128:(h + 1) * 128], lhsT=PcT[:, h * 2 + mc, :], rhs=vm[:, mc, h * 128:(h + 1) * 128], start=(mc == 0), stop=(mc == 1))
                    return r
                P.op("pe", mmov, reads=[("PcT", 0), ("PcT", 1)] + vm_keys, writes=["pX"])
                P.op("dve", lambda e: e.reciprocal(out=RLC, in_=LC), reads=[("lc", h) for h in range(4)], writes=["rlc"])
                for h in range(4):
                    P.op("dve", lambda e, h=h: e.tensor_scalar(out=ox[:, h * 128:(h + 1) * 128], in0=pX[:, h * 128:(h + 1) * 128], scalar1=sc[:, 12 + h:13 + h], scalar2=None, op0=ALU.mult),
                         reads=["pX", "rlc"], writes=[("ox", h)])
                self.transposes8(ox, psT, lambda g, n: oxT[:, 0:n, :], [("ox", h) for h in range(4)], lambda g: "oxT", nchunks=4)
                P.op("pe", proj(pA, oxT, wco, 4, None), reads=["oxT", "wco"], writes=["pA"])
                c0, k0 = self.rstd_of(pA[:], D, stat, 12, "pA", "b3c", junk[:])
                P.op("dve", lambda e, c0=c0: e.scalar_tensor_tensor(out=tmp[:], in0=pA[:], scalar=c0, in1=G["cross_post"][:], op0=ALU.mult, op1=ALU.mult),
                     reads=["pA", k0, "g_cross_post"], writes=["tmp"])
                P.op("pool", lambda e: e.tensor_tensor(out=h3t[:], in0=tmp[:], in1=h2t[:], op=ALU.add), reads=["tmp", "h2t"], writes=["h3t"])
                P.op("sp", lambda e, rs=rs: e.dma_start(out=S["h3"][rs, :], in_=h3t[:]), reads=["h3t"], writes=[("h3_d", i)], ndma=1)
            P.barrier()

    def build(self):
        nc, P = self.nc, self.P
        upto = self.upto
        nblk = self.nblk
        I = {}
        for nm, shp in INPUT_SPECS:
            I[nm] = self.din(nm, shp)
        self.I = I
        dbg = self.debug
        S = {}

        def scr(name, shape, dt):
            if dbg:
                S[name] = self.dout(name, shape, dt)
            else:
                S[name] = self.dscr(name, shape, dt)
        scr("h1", [nblk * 512, D], F32)
        scr("h1own", [nblk * 128, D], F32)
        scr("xTown", [nblk, 128, 8, 128], BF16)
        scr("kT", [128, 4, nblk * 512], BF16)
        scr("ikT", [64, nblk * 512], BF16)
        scr("v2", [128, 4, nblk * 4, 128], BF16)
        scr("Ssel", [nblk, 128, 4, 256], F32)
        scr("oan", [nblk * 128, D], BF16)
        scr("sga", [nblk * 128, D], BF16)
        scr("sgb", [nblk * 128, D], BF16)
        scr("dqT", [nblk, 128, 4, 128], BF16)
        scr("iqT", [nblk, 128, 4, 128], BF16)
        scr("iw", [nblk * 128, 8], F32)
        scr("vbias", [8, 768], F32)
        scr("ka", [128, 4], F32)
        scr("ob", [nblk * 128, 512], BF16)
        scr("h3", [nblk * 128, D], F32)
        S["out"] = self.dout("out", [nblk * 128, D], F32)
        with ExitStack() as st:
            self.consts(st)
            self.ffn_phase("f1", I["xall"], nblk * 512, I["ffn1_w_in"], I["ffn1_w_out"], I["ffn1_pre"], I["ffn1_post"], S["h1"])
            if upto != "A1":
                self.phase_a2(I, S, nblk)
            if upto not in ("A1", "A2"):
                self.phase_b1(I, S, nblk)
            if upto not in ("A1", "A2", "B1"):
                self.phase_b2(I, S, nblk)
            if upto not in ("A1", "A2", "B1", "B2"):
                self.phase_b3(I, S, nblk)
            if upto not in ("A1", "A2", "B1", "B2", "B3"):
                self.ffn_phase("f2", S["h3"], nblk * 128, I["ffn2_w_in"], I["ffn2_w_out"], I["ffn2_pre"], I["ffn2_post"], S["out"])
            P.emit(st)
        return nc


INPUT_SPECS = [
    ("xall", [T, D]), ("idn", [128, 128]), ("tri", [128, 128]), ("ej", [128, 4]),
    ("ffn1_pre", [1, D]), ("ffn1_post", [1, D]), ("ffn1_w_in", [D, 2 * DFF]), ("ffn1_w_out", [DFF, D]),
    ("mix_pre", [1, D]), ("w_in", [D, 7256]), ("w_alpha_up", [16, 512]), ("b_alpha", [1, 512]),
    ("gla_norm", [1, D]), ("maskT4", [128, 512]),
    ("rel_bias_table", [32, 8]), ("ohrev", [32, 768]), ("cm", [128, 512]),
    ("mem", [256, D]), ("mix_post", [1, D]), ("cross_pre", [1, D]), ("cross_post", [1, D]), ("mem_norm", [1, D]),
    ("w_gla_proj", [D, D]), ("w_dsa_proj", [512, D]), ("w_out", [D, D]), ("w_cq", [D, 512]), ("w_ckv", [D, D]), ("w_co", [512, D]),
    ("ffn2_pre", [1, D]), ("ffn2_post", [1, D]), ("ffn2_w_in", [D, 2 * DFF]), ("ffn2_w_out", [DFF, D]),
]


def t5_bucket_np(rel):
    rel = np.asarray(rel, dtype=np.int64)
    relf = np.maximum(rel, 1).astype(np.float32)
    large = 16 + (np.log(relf / np.float32(16)) / np.float32(np.log(8.0)) * np.float32(16)).astype(np.int32)
    large = np.minimum(large, 31)
    return np.where(rel < 16, rel, large)


def make_in_maps(inputs, ncores=8):
    in_maps = []
    idn = np.eye(128, dtype=np.float32)
    tri = np.triu(np.ones((128, 128), dtype=np.float32)) * (-1.0 / 16.0)
    for c in range(ncores):
        b, j = c // 4, c % 4
        ej = np.zeros((128, 4), dtype=np.float32)
        ej[:, j] = 1.0
        maskT = np.triu(np.ones((128, 128), dtype=np.float32))
        m = {"xall": np.ascontiguousarray(inputs["x"][b]), "idn": idn, "tri": tri, "ej": ej, "maskT4": np.ascontiguousarray(np.tile(maskT, (1, 4)))}
        mp = np.arange(768)
        rel = 128 * j + 255 - mp
        oh = np.zeros((32, 768), dtype=np.float32)
        ok = rel >= 0
        bk = t5_bucket_np(np.maximum(rel, 0))
        oh[bk[ok], mp[ok]] += 1.0
        oh[31, mp[ok]] -= 1.0
        m["ohrev"] = oh
        cc = np.arange(512)[None, :]
        rr = np.arange(128)[:, None]
        m["cm"] = np.where(cc <= 128 * j + rr, 0.0, NEG).astype(np.float32)
        m["rel_bias_table"] = np.ascontiguousarray(inputs["rel_bias_table"]).astype(np.float32)
        m["mem"] = np.ascontiguousarray(inputs["mem"][b])
        for nm, shp in INPUT_SPECS:
            if nm in m:
                continue
            m[nm] = np.ascontiguousarray(inputs[nm][0]).reshape(shp)
        in_maps.append(m)
    return in_maps


def run(inputs, upto, nblk=16, debug=True, ncores=8):
    kb = KB(upto)
    kb.nblk = nblk
    kb.debug = debug
    nc = kb.build()
    in_maps = make_in_maps(inputs, ncores)
    res = run_bass_kernel_spmd(nc, in_maps, core_ids=list(range(ncores)))
    return res


def kernel(**inputs):
    inputs = {k: np.asarray(v) for k, v in inputs.items()}
    res = run(inputs, "ALL", nblk=16, debug=False)
    out = np.zeros((2, T, D), dtype=np.float32)
    for c in range(8):
        b, j = c // 4, c % 4
        o = np.asarray(res.results[c]["out"]).reshape(16, 128, D)
        ov = out[b].reshape(16, 4, 128, D)
        ov[:, j] = o
    return out
```

```python
import numpy as np
from contextlib import ExitStack
import concourse.bass as bass
import concourse.mybir as mybir
from concourse.bass_utils import run_bass_kernel_spmd

F32 = mybir.dt.float32
BF16 = mybir.dt.bfloat16
AF = mybir.ActivationFunctionType
ALU = mybir.AluOpType
AX = mybir.AxisListType

D = 1024
DFF = 2816
T = 8192
EPS = 1e-6
NEG = -30000.0

ENGS = ("pe", "act", "dve", "pool", "sp")


class Op:
    __slots__ = ("eng", "fn", "deps", "odeps", "needs_sig", "sig", "ndma", "idx", "dsem", "cost", "epoch", "start", "finish", "ready", "npend", "done")

    def __init__(self, eng, fn, ndma, cost):
        self.eng = eng
        self.fn = fn
        self.deps = set()
        self.odeps = set()
        self.needs_sig = False
        self.sig = None
        self.ndma = ndma
        self.dsem = None
        self.cost = cost
        self.epoch = 0
        self.start = 0.0
        self.finish = 0.0
        self.ready = 0.0
        self.npend = 0
        self.done = False


DEF_COST = {"pe": 0.35, "act": 0.7, "dve": 0.7, "pool": 0.9, "sp": 2.5}
SCHED = True
WINDOW = 24


class Prog:
    def __init__(self, nc, n_dma_sems=40):
        self.nc = nc
        self.ops = {e: [] for e in ENGS}
        self.lastw = {}
        self.readers = {}
        self.n_dma_sems = n_dma_sems
        self.all_ops = []
        self.epoch = 0

    def op(self, eng, fn, reads=(), writes=(), ndma=0, cost=None):
        if cost is None:
            cost = 2.5 if ndma > 0 else DEF_COST[eng]
        o = Op(eng, fn, ndma, cost)
        o.idx = len(self.all_ops)
        o.epoch = self.epoch
        self.all_ops.append(o)
        is_dma = ndma > 0
        for k in reads:
            w = self.lastw.get(k)
            if w is not None:
                self._dep(o, w, True, is_dma)
        for k in writes:
            w = self.lastw.get(k)
            if w is not None:
                self._dep(o, w, False, is_dma)
            for r in self.readers.get(k, ()):
                self._dep(o, r, False, is_dma)
        for k in reads:
            self.readers.setdefault(k, []).append(o)
        for k in writes:
            self.lastw[k] = o
            self.readers[k] = []
        self.ops[eng].append(o)
        return o

    def _dep(self, o, d, raw, is_dma):
        if d is o:
            return
        if d.eng == o.eng and d.ndma == 0 and not is_dma:
            if o.eng == "pe" or not raw:
                o.odeps.add(d)
                return
        o.deps.add(d)
        d.needs_sig = True

    def barrier(self):
        for e in ENGS:
            o = Op(e, None, 0, 0.0)
            o.idx = len(self.all_ops)
            o.epoch = self.epoch
            self.all_ops.append(o)
            self.ops[e].append(o)
        self.epoch += 1
        self.lastw = {}
        self.readers = {}

    def schedule(self):
        LAT = 0.2
        succs = {}
        for o in self.all_ops:
            o.npend = 0
            o.ready = 0.0
            o.done = False
        for o in self.all_ops:
            if o.fn is None:
                continue
            for d in (o.deps | o.odeps):
                succs.setdefault(d.idx, []).append(o)
                o.npend += 1
        nep = self.epoch + 1
        ep_remaining = [0] * (nep + 1)
        ep_finish = [0.0] * (nep + 1)
        for o in self.all_ops:
            if o.fn is not None:
                ep_remaining[o.epoch] += 1
        head = {e: 0 for e in ENGS}
        eng_free = {e: 0.0 for e in ENGS}
        order = {e: [] for e in ENGS}
        remaining = len(self.all_ops)
        if not SCHED:
            t = 0.0
            for o in self.all_ops:
                o.start = t
                t += 1.0
                order[o.eng].append(o)
            self.ops = order
            return
        while remaining > 0:
            best = None
            bstart = None
            for e in ENGS:
                q = self.ops[e]
                i = head[e]
                cnt = 0
                n = len(q)
                while i < n and cnt < WINDOW:
                    o = q[i]
                    if not o.done:
                        if o.fn is None:
                            if cnt == 0 and ep_remaining[o.epoch] == 0:
                                st_ = max(eng_free[e], ep_finish[o.epoch] + LAT)
                                if best is None or st_ < bstart or (st_ == bstart and o.idx < best.idx):
                                    best, bstart = o, st_
                            break
                        if o.npend == 0:
                            st_ = max(eng_free[e], o.ready)
                            if best is None or st_ < bstart or (st_ == bstart and o.idx < best.idx):
                                best, bstart = o, st_
                        cnt += 1
                    i += 1
            assert best is not None, "scheduler deadlock"
            o = best
            e = o.eng
            o.done = True
            o.start = bstart
            remaining -= 1
            if o.fn is None:
                o.finish = bstart
                eng_free[e] = max(eng_free[e], bstart)
            else:
                o.finish = bstart + o.cost
                eng_free[e] = bstart + (0.08 * o.ndma if o.ndma > 0 else o.cost)
                ep_remaining[o.epoch] -= 1
                if o.finish > ep_finish[o.epoch]:
                    ep_finish[o.epoch] = o.finish
                for s_ in succs.get(o.idx, ()):
                    s_.npend -= 1
                    if o.finish + LAT > s_.ready:
                        s_.ready = o.finish + LAT
            order[e].append(o)
            q = self.ops[e]
            while head[e] < len(q) and q[head[e]].done:
                head[e] += 1
        self.ops = order

    def emit(self, stack):
        nc = self.nc
        self.schedule()
        last_in_epoch = {}
        dmas_in_epoch = {}
        for e in ENGS:
            for o in self.ops[e]:
                if o.fn is None:
                    continue
                if o.ndma > 0:
                    dmas_in_epoch.setdefault(o.epoch, []).append(o)
                else:
                    last_in_epoch[(o.epoch, e)] = o
        for e in ENGS:
            for o in self.ops[e]:
                if o.fn is None:
                    for e2 in ENGS:
                        d = last_in_epoch.get((o.epoch, e2))
                        if d is not None and e2 != e:
                            o.deps.add(d)
                            d.needs_sig = True
                    for d in dmas_in_epoch.get(o.epoch, ()):
                        o.deps.add(d)
        esem = {e: stack.enter_context(nc.semaphore("s_" + e)) for e in ENGS}
        dsems = [stack.enter_context(nc.semaphore("d%d" % i)) for i in range(self.n_dma_sems)]
        dcount = [0] * self.n_dma_sems
        dlast = [None] * self.n_dma_sems
        ecount = {e: 0 for e in ENGS}
        rr = 0
        glob = sorted(self.all_ops, key=lambda x: (x.start, x.idx))
        pos = {}
        for e in ENGS:
            for n_, o in enumerate(self.ops[e]):
                pos[o.idx] = n_
        for o in glob:
            if o.ndma > 0:
                s = rr % self.n_dma_sems
                rr += 1
                if dlast[s] is not None:
                    o.deps.add(dlast[s])
                dcount[s] += 16 * o.ndma
                o.sig = (dsems[s], dcount[s])
                o.dsem = dsems[s]
                dlast[s] = o
        for e in ENGS:
            for o in self.ops[e]:
                if o.ndma == 0 and o.needs_sig and o.fn is not None:
                    ecount[e] += 1
                    o.sig = (esem[e], ecount[e])
        engobj = {"pe": nc.tensor, "act": nc.scalar, "dve": nc.vector, "pool": nc.gpsimd, "sp": nc.sync}
        block = stack.enter_context(nc.Block())

        def run(e):
            eng = engobj[e]
            waited = {}
            for o in self.ops[e]:
                for d in sorted(o.deps, key=lambda x: x.idx):
                    if d.sig is None:
                        continue
                    sem, val = d.sig
                    key = id(sem)
                    if waited.get(key, 0) < val:
                        eng.wait_ge(sem, val)
                        waited[key] = val
                if o.fn is None:
                    continue
                r = o.fn(eng)
                if o.ndma > 0:
                    rs = r if isinstance(r, (list, tuple)) else [r]
                    assert len(rs) == o.ndma, (len(rs), o.ndma)
                    for i in rs:
                        i.then_inc(o.dsem, 16)
                elif o.sig is not None:
                    last = r[-1] if isinstance(r, (list, tuple)) else r
                    last.then_inc(o.sig[0], 1)

        @block.tensor
        def _(e):
            run("pe")

        @block.scalar
        def _(e):
            run("act")

        @block.vector
        def _(e):
            run("dve")

        @block.gpsimd
        def _(e):
            run("pool")

        @block.sync
        def _(e):
            run("sp")


class KB:
    def __init__(self, upto):
        self.upto = upto
        self.nc = bass.Bass("TRN2", target_bir_lowering=False)
        self.P = Prog(self.nc)
        self.uid = 0

    def din(self, name, shape, dt=F32):
        return self.nc.dram_tensor(name, list(shape), dt, kind="ExternalInput").ap()

    def dout(self, name, shape, dt=F32):
        return self.nc.dram_tensor(name, list(shape), dt, kind="ExternalOutput").ap()

    def dscr(self, name, shape, dt):
        return self.nc.dram_tensor(name, list(shape), dt, kind="Internal").ap()

    def sb(self, st, name, shape, dt):
        return st.enter_context(self.nc.sbuf_tensor(name, list(shape), dt))

    def ps(self, st, name, shape, dt=F32):
        return st.enter_context(self.nc.psum_tensor(name, list(shape), dt))

    def consts(self, st):
        P = self.P
        self.idn_d = self.I["idn"]
        idf = self.sb(st, "idf", [128, 128], F32)
        self.idf = idf
        self.idb = self.sb(st, "idb", [128, 128], BF16)
        self.epsc = self.sb(st, "epsc", [128, 1], F32)
        P.op("sp", lambda e: e.dma_start(out=idf[:], in_=self.idn_d), writes=["idf"], ndma=1)
        P.op("dve", lambda e: e.tensor_copy(out=self.idb[:], in_=idf[:]), reads=["idf"], writes=["idb"])
        P.op("dve", lambda e: e.memset(self.epsc[:], EPS), writes=["epsc"])

    def rstd_of(self, src, width, stat, col, key_src, tag, junk):
        P = self.P
        c0 = stat[:, col:col + 1]
        c1 = stat[:, col + 1:col + 2]
        k0 = ("stat", tag, col)
        P.op("dve", lambda e: e.memset(c0, 0.0), writes=[k0])
        P.op("act", lambda e: e.activation(out=junk, in_=src, func=AF.Square, accum_out=c0),
             reads=[key_src, k0], writes=[k0, ("junk", tag)])
        P.op("act", lambda e: e.activation(out=c1, in_=c0, func=AF.Sqrt, bias=self.epsc[:, 0:1], scale=1.0 / width),
             reads=[k0, "epsc"], writes=[(k0, 1)])
        P.op("dve", lambda e: e.reciprocal(out=c0, in_=c1), reads=[(k0, 1)], writes=[k0])
        return c0, k0

    def ffn_phase(self, name, src_d, ntok, w_in_d, w_out_d, pre_d, post_d, dst_d):
        nc, P = self.nc, self.P
        with ExitStack() as st:
            wi = self.sb(st, name + "wi", [128, 8, 2 * DFF], BF16)
            wo = self.sb(st, name + "wo", [128, 22, D], BF16)
            gpre = self.sb(st, name + "gpre", [128, D], F32)
            gpost = self.sb(st, name + "gpost", [128, D], F32)
            xt = [self.sb(st, name + "xt%d" % i, [128, D], F32) for i in range(2)]
            xr = [self.sb(st, name + "xr%d" % i, [128, D], F32) for i in range(2)]
            xn = self.sb(st, name + "xn", [128, D], BF16)
            xT = self.sb(st, name + "xT", [128, 8, 512], BF16)
            h1T = self.sb(st, name + "h1T", [128, 22, 512], BF16)
            sa = [self.sb(st, name + "sa%d" % i, [128, 512], F32) for i in range(2)]
            tmp = self.sb(st, name + "tmp", [128, D], F32)
            junk = self.sb(st, name + "junk", [128, D], BF16)
            stat = self.sb(st, name + "stat", [128, 64], F32)
            psA = [self.ps(st, name + "psA%d" % i, [128, 512]) for i in range(2)]
            psB = [self.ps(st, name + "psB%d" % i, [128, 512]) for i in range(2)]
            psY = self.ps(st, name + "psY", [128, 1024])
            psT = [self.ps(st, name + "psT%d" % i, [128, 512], BF16) for i in range(2)]

            wi_src = w_in_d.rearrange("(k p) n -> p k n", p=128)
            for k in range(8):
                P.op("pool", lambda e, k=k: e.dma_start(out=wi[:, k, :], in_=wi_src[:, k, :]), writes=[("wi", k)], ndma=1)
            wo_src = w_out_d.rearrange("(f p) n -> p f n", p=128)
            for f0 in range(0, 22, 6):
                f1 = min(22, f0 + 6)
                P.op("pool", lambda e, f0=f0, f1=f1: e.dma_start(out=wo[:, f0:f1, :], in_=wo_src[:, f0:f1, :]),
                     writes=[("wo", f) for f in range(f0, f1)], ndma=1)
            P.op("sp", lambda e: e.dma_start(out=gpre[:], in_=pre_d.to_broadcast([128, D])), writes=["gpre"], ndma=1)
            P.op("sp", lambda e: e.dma_start(out=gpost[:], in_=post_d.to_broadcast([128, D])), writes=["gpost"], ndma=1)
            wi_keys = [("wi", k) for k in range(8)]
            wo_keys = [("wo", f) for f in range(22)]

            nblk = ntok // 512
            tcount = 0
            for blk in range(nblk):
                for tt in range(4):
                    r0 = blk * 512 + tt * 128
                    xb = xt[tcount % 2]
                    kx = ("xt", tcount % 2)
                    tcount += 1
                    P.op("sp", lambda e, xb=xb, r0=r0: e.dma_start(out=xb[:], in_=src_d[r0:r0 + 128, :]), writes=[kx], ndma=1)
                    c0, k0 = self.rstd_of(xb[:], D, stat, 2 * (tcount % 8), kx, name, junk[:])
                    P.op("dve", lambda e, xb=xb, c0=c0: e.scalar_tensor_tensor(out=xn[:], in0=xb[:], scalar=c0, in1=gpre[:], op0=ALU.mult, op1=ALU.mult),
                         reads=[kx, k0, "gpre"], writes=["xn"])
                    for g in range(2):
                        def tr(e, g=g):
                            r = None
                            for kk in range(4):
                                k = 4 * g + kk
                                r = e.transpose(out=psT[g][:, kk * 128:(kk + 1) * 128], in_=xn[:, k * 128:(k + 1) * 128], identity=self.idb[:])
                            return r
                        P.op("pe", tr, reads=["xn", "idb"], writes=[("psT", g)])
                        eng = "act" if g == 0 else "dve"
                        if eng == "act":
                            P.op("act", lambda e, g=g, tt=tt: e.copy(out=xT[:, 4 * g:4 * g + 4, tt * 128:(tt + 1) * 128],
                                                                     in_=psT[g][:].rearrange("p (c t) -> p c t", c=4)),
                                 reads=[("psT", g)], writes=[("xT", tt, g)])
                        else:
                            P.op("dve", lambda e, g=g, tt=tt: e.tensor_copy(out=xT[:, 4 * g:4 * g + 4, tt * 128:(tt + 1) * 128],
                                                                            in_=psT[g][:].rearrange("p (c t) -> p c t", c=4)),
                                 reads=[("psT", g)], writes=[("xT", tt, g)])
                xT_keys = [("xT", tt, g) for tt in range(4) for g in range(2)]
                for f in range(22):
                    pa, pb = psA[f % 2], psB[f % 2]

                    def mmab(e, f=f, pa=pa, pb=pb):
                        for k in range(8):
                            e.matmul(pa[:], lhsT=wi[:, k, f * 128:(f + 1) * 128], rhs=xT[:, k, :], start=(k == 0), stop=(k == 7))
                        r = None
                        for k in range(8):
                            r = e.matmul(pb[:], lhsT=wi[:, k, DFF + f * 128:DFF + (f + 1) * 128], rhs=xT[:, k, :], start=(k == 0), stop=(k == 7))
                        return r
                    P.op("pe", mmab, reads=wi_keys + xT_keys, writes=[("psA", f % 2), ("psB", f % 2)])
                    s = sa[f % 2]
                    P.op("act", lambda e, s=s, pa=pa: e.activation(out=s[:], in_=pa[:], func=AF.Silu), reads=[("psA", f % 2)], writes=[("sa", f % 2)])
                    P.op("dve", lambda e, s=s, pb=pb, f=f: e.tensor_tensor(out=h1T[:, f, :], in0=s[:], in1=pb[:], op=ALU.mult),
                         reads=[("sa", f % 2), ("psB", f % 2)], writes=[("h1T", f)])
                h1T_keys = [("h1T", f) for f in range(22)]
                for tt in range(4):
                    r0 = blk * 512 + tt * 128

                    def mmy(e, tt=tt):
                        r = None
                        for nh in range(2):
                            for f in range(22):
                                r = e.matmul(psY[:, nh * 512:(nh + 1) * 512], lhsT=h1T[:, f, tt * 128:(tt + 1) * 128],
                                             rhs=wo[:, f, nh * 512:(nh + 1) * 512], start=(f == 0), stop=(f == 21))
                        return r
                    P.op("pe", mmy, reads=h1T_keys + wo_keys, writes=["psY"])
                    xres = xr[tt % 2]
                    kr = ("xr", tt % 2)
                    P.op("sp", lambda e, xres=xres, r0=r0: e.dma_start(out=xres[:], in_=src_d[r0:r0 + 128, :]), writes=[kr], ndma=1)
                    c0, k0 = self.rstd_of(psY[:], D, stat, 16 + 2 * (tt % 4), "psY", name + "y", junk[:])
                    P.op("dve", lambda e, c0=c0: e.scalar_tensor_tensor(out=tmp[:], in0=psY[:], scalar=c0, in1=gpost[:], op0=ALU.mult, op1=ALU.mult),
                         reads=["psY", k0, "gpost"], writes=["tmp"])
                    P.op("dve", lambda e, xres=xres: e.scalar_tensor_tensor(out=xres[:], in0=tmp[:], scalar=0.5, in1=xres[:], op0=ALU.mult, op1=ALU.add),
                         reads=["tmp", kr], writes=[kr])
                    P.op("sp", lambda e, xres=xres, r0=r0: e.dma_start(out=dst_d[r0:r0 + 128, :], in_=xres[:]), reads=[kr], writes=[("dst", name, r0)], ndma=1)
            P.barrier()

    def transposes8(self, xn, psT, xT_out_fn, keys_in, key_out_fn, nchunks=8):
        P = self.P
        ng = (nchunks + 3) // 4
        for g in range(ng):
            n = min(4, nchunks - 4 * g)

            def tr(e, g=g, n=n):
                r = None
                for kk in range(n):
                    k = 4 * g + kk
                    r = e.transpose(out=psT[g % 2][:, kk * 128:(kk + 1) * 128], in_=xn[:, k * 128:(k + 1) * 128], identity=self.idb[:])
                return r
            P.op("pe", tr, reads=list(keys_in) + ["idb"], writes=[("psT", g % 2)])
            dst = xT_out_fn(g, n)
            src = psT[g % 2][:, 0:n * 128].rearrange("p (c t) -> p c t", c=n)
            if g % 2 == 0:
                P.op("act", lambda e, dst=dst, src=src: e.copy(out=dst, in_=src), reads=[("psT", g % 2)], writes=[key_out_fn(g)])
            else:
                P.op("dve", lambda e, dst=dst, src=src: e.tensor_copy(out=dst, in_=src), reads=[("psT", g % 2)], writes=[key_out_fn(g)])

    def load_w(self, dst, src_d, c0, c1, key, kchunks=8):
        src = src_d.rearrange("(k p) n -> p k n", p=128)
        self.P.op("pool", lambda e: e.dma_start(out=dst, in_=src[:, :, c0:c1]), writes=[key], ndma=1)

    def phase_a2(self, I, S, nblk):
        nc, P = self.nc, self.P
        w_in = I["w_in"]
        with ExitStack() as st:
            def dbl(name, shape, dt):
                return [self.sb(st, "a2%s%d" % (name, i), shape, dt) for i in range(2)]
            w_gk = self.sb(st, "a2w_gk", [128, 8, 512], BF16)
            w_gv = self.sb(st, "a2w_gv", [128, 8, 1024], BF16)
            w_dv = self.sb(st, "a2w_dv", [128, 8, 512], BF16)
            w_dk = self.sb(st, "a2w_dk", [128, 8, 512], BF16)
            w_ik = self.sb(st, "a2w_ik", [128, 8, 64], BF16)
            w_ga = self.sb(st, "a2w_ga", [128, 8, 16], BF16)
            w_au = self.sb(st, "a2w_au", [16, 512], BF16)
            balpha = self.sb(st, "a2balpha", [128, 512], F32)
            gpre = self.sb(st, "a2gpre", [128, D], F32)
            tri = self.sb(st, "a2tri", [128, 128], F32)
            negs = self.sb(st, "a2negs", [128, 1], F32)
            ej = self.sb(st, "a2ej", [128, 4], F32)
            ht = dbl("ht", [128, D], F32)
            hown = dbl("hown", [128, D], F32)
            xn = dbl("xn", [128, D], BF16)
            xT = dbl("xT", [128, 8, 512], BF16)
            xTown = dbl("xTown", [128, 8, 128], BF16)
            junk = self.sb(st, "a2junk", [128, D], BF16)
            stat = self.sb(st, "a2stat", [128, 64], F32)
            kTs = dbl("kTs", [128, 4, 512], BF16)
            ikTs = dbl("ikTs", [64, 512], BF16)
            gaT = dbl("gaT", [16, 512], BF16)
            zb = dbl("zb", [128, 512], F32)
            lt = dbl("lt", [128, 512], F32)
            enb = dbl("enb", [128, 512], F32)
            ktil = dbl("ktil", [128, 512], BF16)
            gvb = dbl("gvb", [128, 1024], BF16)
            dvb = dbl("dvb", [128, 512], BF16)
            decay = dbl("decay", [128, 4], F32)
            Sst = self.sb(st, "a2S", [128, 4, 256], F32)
            Stmp = dbl("Stmp", [128, 4, 256], F32)
            Ssel = dbl("Ssel", [128, 4, 256], F32)
            psT = [self.ps(st, "a2psT%d" % i, [128, 512], BF16) for i in range(2)]
            psW = self.ps(st, "a2psW", [128, 1024])
            psX = [self.ps(st, "a2psX%d" % i, [128, 512]) for i in range(2)]
            psZ = [self.ps(st, "a2psZ%d" % i, [128, 512]) for i in range(2)]

            self.load_w(w_gk[:], w_in, 512, 1024, "w_gk")
            self.load_w(w_gv[:], w_in, 1024, 2048, "w_gv")
            self.load_w(w_dv[:], w_in, 4112, 4624, "w_dv")
            self.load_w(w_dk[:], w_in, 3600, 4112, "w_dk")
            self.load_w(w_ik[:], w_in, 5136, 5200, "w_ik")
            self.load_w(w_ga[:], w_in, 3072, 3088, "w_ga")
            P.op("pool", lambda e: e.dma_start(out=w_au[:], in_=I["w_alpha_up"]), writes=["w_au"], ndma=1)
            P.op("sp", lambda e: e.dma_start(out=balpha[:], in_=I["b_alpha"].to_broadcast([128, 512])), writes=["balpha"], ndma=1)
            P.op("sp", lambda e: e.dma_start(out=gpre[:], in_=I["mix_pre"].to_broadcast([128, D])), writes=["gpre"], ndma=1)
            P.op("sp", lambda e: e.dma_start(out=tri[:], in_=I["tri"]), writes=["tri"], ndma=1)
            P.op("sp", lambda e: e.dma_start(out=ej[:], in_=I["ej"]), writes=["ej"], ndma=1)
            P.op("dve", lambda e: e.memset(negs[:], -1.0 / 16.0), writes=["negs"])
            P.op("dve", lambda e: e.memset(Sst[:], 0.0), writes=[("S", h) for h in range(4)])
            kab = self.sb(st, "a2kab", [128, 4], F32)
            KA = self.sb(st, "a2KA", [128, 4], F32)
            P.op("dve", lambda e: e.memset(KA[:], 0.0), writes=["KA"])

            tcount = 0
            for blk in range(nblk):
                bp = blk % 2
                xTb, hownb, xTownb, kTsb, ikTsb, gaTb, Sselb = xT[bp], hown[bp], xTown[bp], kTs[bp], ikTs[bp], gaT[bp], Ssel[bp]
                for u in range(4):
                    r0 = blk * 512 + u * 128
                    tp = tcount % 2
                    hb = ht[tp]
                    xnb = xn[tp]
                    kh = ("ht", tp)
                    kxn = ("xn", tp)
                    tcount += 1
                    P.op("sp", lambda e, hb=hb, r0=r0: e.dma_start(out=hb[:], in_=S["h1"][r0:r0 + 128, :]), writes=[kh], ndma=1)
                    c0, k0 = self.rstd_of(hb[:], D, stat, 2 * (tcount % 8), kh, "a2", junk[:])
                    P.op("dve", lambda e, hb=hb, c0=c0, xnb=xnb: e.scalar_tensor_tensor(out=xnb[:], in0=hb[:], scalar=c0, in1=gpre[:], op0=ALU.mult, op1=ALU.mult),
                         reads=[kh, k0, "gpre"], writes=[kxn], cost=1.2)
                    if u == 0:
                        P.op("pool", lambda e, hb=hb, hownb=hownb: e.tensor_scalar(out=hownb[:], in0=hb[:], scalar1=ej[:, 0:1], scalar2=None, op0=ALU.mult),
                             reads=[kh, "ej"], writes=[("hown", bp)], cost=2.0)
                    else:
                        P.op("dve", lambda e, hb=hb, u=u, hownb=hownb: e.scalar_tensor_tensor(out=hownb[:], in0=hb[:], scalar=ej[:, u:u + 1], in1=hownb[:], op0=ALU.mult, op1=ALU.add),
                             reads=[kh, "ej", ("hown", bp)], writes=[("hown", bp)], cost=1.2)
                    self.transposes8(xnb, psT, lambda g, n, u=u, xTb=xTb: xTb[:, 4 * g:4 * g + n, u * 128:(u + 1) * 128], [kxn], lambda g, u=u, bp=bp: ("xT", bp, u, g))
                P.op("sp", lambda e, blk=blk, hownb=hownb: e.dma_start(out=S["h1own"][blk * 128:(blk + 1) * 128, :], in_=hownb[:]), reads=[("hown", bp)], writes=[("h1own_d", blk)], ndma=1)
                xT_keys = [("xT", bp, u, g) for u in range(4) for g in range(2)]
                for u in range(4):
                    if u == 0:
                        P.op("pool", lambda e, xTb=xTb, xTownb=xTownb: e.tensor_scalar(out=xTownb[:], in0=xTb[:, :, 0:128], scalar1=ej[:, 0:1], scalar2=None, op0=ALU.mult),
                             reads=xT_keys + ["ej"], writes=[("xTown", bp)], cost=1.5)
                    else:
                        P.op("dve", lambda e, u=u, xTb=xTb, xTownb=xTownb: e.scalar_tensor_tensor(out=xTownb[:], in0=xTb[:, :, u * 128:(u + 1) * 128], scalar=ej[:, u:u + 1], in1=xTownb[:],
                                                                                                  op0=ALU.mult, op1=ALU.add),
                             reads=xT_keys + ["ej", ("xTown", bp)], writes=[("xTown", bp)], cost=1.5)
                P.op("sp", lambda e, blk=blk, xTownb=xTownb: e.dma_start(out=S["xTown"][blk], in_=xTownb[:]), reads=[("xTown", bp)], writes=[("xTown_d", blk)], ndma=1)
                for c in range(4):
                    pf = psX[c % 2]

                    def mmf(e, c=c, pf=pf, xTb=xTb):
                        r = None
                        for k in range(8):
                            r = e.matmul(pf[:], lhsT=w_dk[:, k, c * 128:(c + 1) * 128], rhs=xTb[:, k, :], start=(k == 0), stop=(k == 7))
                        return r
                    P.op("pe", mmf, reads=xT_keys + ["w_dk"], writes=[("psX", c % 2)], cost=1.9)
                    if c % 2 == 0:
                        P.op("act", lambda e, c=c, pf=pf, kTsb=kTsb: e.copy(out=kTsb[:, c, :], in_=pf[:]), reads=[("psX", c % 2)], writes=[("kTs", bp, c)])
                    else:
                        P.op("dve", lambda e, c=c, pf=pf, kTsb=kTsb: e.tensor_copy(out=kTsb[:, c, :], in_=pf[:]), reads=[("psX", c % 2)], writes=[("kTs", bp, c)])
                P.op("sp", lambda e, blk=blk, kTsb=kTsb: e.dma_start(out=S["kT"][:, :, blk * 512:(blk + 1) * 512], in_=kTsb[:]),
                     reads=[("kTs", bp, c) for c in range(4)], writes=[("kT_d", blk)], ndma=1)
                P.op("dve", lambda e, kTsb=kTsb: e.tensor_reduce(out=kab[:], in_=kTsb[:], axis=AX.X, op=ALU.max, apply_absolute_value=True),
                     reads=[("kTs", bp, c) for c in range(4)], writes=["kab"], cost=2.3)
                P.op("dve", lambda e: e.tensor_tensor(out=KA[:], in0=KA[:], in1=kab[:], op=ALU.max), reads=["kab", "KA"], writes=["KA"], cost=0.1)

                def mmik(e, xTb=xTb):
                    r = None
                    for k in range(8):
                        r = e.matmul(psZ[0][0:64, :], lhsT=w_ik[:, k, :], rhs=xTb[:, k, :], start=(k == 0), stop=(k == 7))
                    return r
                P.op("pe", mmik, reads=xT_keys + ["w_ik"], writes=[("psZ", 0)], cost=1.9)
                P.op("act", lambda e, ikTsb=ikTsb: e.copy(out=ikTsb[:], in_=psZ[0][0:64, :]), reads=[("psZ", 0)], writes=[("ikTs", bp)])
                P.op("sp", lambda e, blk=blk, ikTsb=ikTsb: e.dma_start(out=S["ikT"][:, blk * 512:(blk + 1) * 512], in_=ikTsb[:]), reads=[("ikTs", bp)], writes=[("ikT_d", blk)], ndma=1)

                def mmga(e, xTb=xTb):
                    r = None
                    for k in range(8):
                        r = e.matmul(psZ[1][0:16, :], lhsT=w_ga[:, k, :], rhs=xTb[:, k, :], start=(k == 0), stop=(k == 7))
                    return r
                P.op("pe", mmga, reads=xT_keys + ["w_ga"], writes=[("psZ", 1)], cost=1.9)
                P.op("dve", lambda e, gaTb=gaTb: e.tensor_copy(out=gaTb[:], in_=psZ[1][0:16, :]), reads=[("psZ", 1)], writes=[("gaT", bp)])
                for u in range(4):
                    tile = blk * 4 + u
                    tp = tile % 2
                    ucols = slice(u * 128, (u + 1) * 128)
                    pX, pZ = psX[tp], psZ[tp]
                    kX, kZ = ("psX", tp), ("psZ", tp)
                    zb_, lt_, enb_, ktil_, gvb_, dvb_, decay_, Stmp_ = zb[tp], lt[tp], enb[tp], ktil[tp], gvb[tp], dvb[tp], decay[tp], Stmp[tp]

                    def mmgk(e, ucols=ucols, pX=pX, xTb=xTb):
                        r = None
                        for k in range(8):
                            r = e.matmul(pX[:], lhsT=xTb[:, k, ucols], rhs=w_gk[:, k, :], start=(k == 0), stop=(k == 7))
                        return r
                    P.op("pe", mmgk, reads=xT_keys + ["w_gk"], writes=[kX], cost=1.9)
                    P.op("pe", lambda e, ucols=ucols, pZ=pZ, gaTb=gaTb: e.matmul(pZ[:], lhsT=gaTb[:, ucols], rhs=w_au[:], start=True, stop=True), reads=[("gaT", bp), "w_au"], writes=[kZ])
                    P.op("dve", lambda e, pZ=pZ, zb_=zb_: e.tensor_tensor(out=zb_[:], in0=pZ[:], in1=balpha[:], op=ALU.add), reads=[kZ, "balpha"], writes=[("zb", tp)])
                    P.op("act", lambda e, zb_=zb_, lt_=lt_: e.activation(out=lt_[:], in_=zb_[:], func=AF.Exp, scale=-1.0), reads=[("zb", tp)], writes=[("lt", tp)])
                    P.op("act", lambda e, lt_=lt_: e.activation(out=lt_[:], in_=lt_[:], func=AF.Ln, bias=1.0, scale=1.0), reads=[("lt", tp)], writes=[("lt", tp)])
                    P.op("pe", lambda e, pZ=pZ, lt_=lt_: e.matmul(pZ[:], lhsT=tri[:], rhs=lt_[:], start=True, stop=True), reads=["tri", ("lt", tp)], writes=[kZ], cost=1.0)
                    P.op("act", lambda e, pZ=pZ, enb_=enb_: e.activation(out=enb_[:], in_=pZ[:], func=AF.Exp, scale=-1.0), reads=[kZ], writes=[("enb", tp)])
                    P.op("dve", lambda e, pX=pX, enb_=enb_, ktil_=ktil_: e.tensor_tensor(out=ktil_[:], in0=pX[:], in1=enb_[:], op=ALU.mult), reads=[kX, ("enb", tp)], writes=[("ktil", tp)])

                    def mmbl(e, pZ=pZ, lt_=lt_):
                        r = None
                        for h in range(4):
                            r = e.matmul(pZ[:, h:h + 1], lhsT=lt_[:, h * 128:(h + 1) * 128], rhs=negs[:], start=True, stop=True)
                        return r
                    P.op("pe", mmbl, reads=[("lt", tp), "negs"], writes=[kZ], cost=0.8)
                    P.op("act", lambda e, pZ=pZ, decay_=decay_: e.activation(out=decay_[:], in_=pZ[:, 0:4], func=AF.Exp), reads=[kZ], writes=[("decay", tp)], cost=0.3)

                    def mmdv(e, ucols=ucols, pX=pX, xTb=xTb):
                        r = None
                        for k in range(8):
                            r = e.matmul(pX[:], lhsT=xTb[:, k, ucols], rhs=w_dv[:, k, :], start=(k == 0), stop=(k == 7))
                        return r
                    P.op("pe", mmdv, reads=xT_keys + ["w_dv"], writes=[kX], cost=1.9)
                    P.op("act", lambda e, pX=pX, dvb_=dvb_: e.copy(out=dvb_[:], in_=pX[:]), reads=[kX], writes=[("dvb", tp)])
                    P.op("sp", lambda e, tile=tile, dvb_=dvb_: e.dma_start(out=S["v2"][:, :, tile, :], in_=dvb_[:].rearrange("p (a c) -> p a c", a=4)),
                         reads=[("dvb", tp)], writes=[("v2_d", tile)], ndma=1)

                    def mmgv(e, ucols=ucols, xTb=xTb):
                        r = None
                        for nh in range(2):
                            for k in range(8):
                                r = e.matmul(psW[:, nh * 512:(nh + 1) * 512], lhsT=xTb[:, k, ucols], rhs=w_gv[:, k, nh * 512:(nh + 1) * 512], start=(k == 0), stop=(k == 7))
                        return r
                    P.op("pe", mmgv, reads=xT_keys + ["w_gv"], writes=["psW"], cost=3.8)
                    P.op("act", lambda e, gvb_=gvb_: e.copy(out=gvb_[:], in_=psW[:]), reads=["psW"], writes=[("gvb", tp)], cost=1.2)

                    def mmkv(e, ktil_=ktil_, gvb_=gvb_):
                        r = None
                        for h in range(4):
                            r = e.matmul(psW[:, h * 256:(h + 1) * 256], lhsT=ktil_[:, h * 128:(h + 1) * 128], rhs=gvb_[:, h * 256:(h + 1) * 256], start=True, stop=True)
                        return r
                    P.op("pe", mmkv, reads=[("ktil", tp), ("gvb", tp)], writes=["psW"], cost=0.8)
                    for h in range(4):
                        if u == 0:
                            P.op("pool", lambda e, h=h, Sselb=Sselb: e.tensor_scalar(out=Sselb[:, h, :], in0=Sst[:, h, :], scalar1=ej[:, 0:1], scalar2=None, op0=ALU.mult),
                                 reads=[("S", h), "ej"], writes=[("Ssel", bp, h)], cost=0.6)
                        else:
                            P.op("dve", lambda e, h=h, u=u, Sselb=Sselb: e.scalar_tensor_tensor(out=Sselb[:, h, :], in0=Sst[:, h, :], scalar=ej[:, u:u + 1], in1=Sselb[:, h, :],
                                                                                                 op0=ALU.mult, op1=ALU.add),
                                 reads=[("S", h), "ej", ("Ssel", bp, h)], writes=[("Ssel", bp, h)], cost=0.6)
                        P.op("dve", lambda e, h=h, Stmp_=Stmp_: e.tensor_tensor(out=Stmp_[:, h, :], in0=Sst[:, h, :], in1=psW[:, h * 256:(h + 1) * 256], op=ALU.add),
                             reads=[("S", h), "psW"], writes=[("Stmp", tp, h)], cost=0.4)
                        P.op("dve", lambda e, h=h, Stmp_=Stmp_, decay_=decay_: e.tensor_scalar(out=Sst[:, h, :], in0=Stmp_[:, h, :], scalar1=decay_[:, h:h + 1], scalar2=None, op0=ALU.mult),
                             reads=[("Stmp", tp, h), ("decay", tp)], writes=[("S", h)], cost=0.4)
                P.op("sp", lambda e, blk=blk, Sselb=Sselb: e.dma_start(out=S["Ssel"][blk], in_=Sselb[:]), reads=[("Ssel", bp, h) for h in range(4)], writes=[("Ssel_d", blk)], ndma=1)
            P.op("sp", lambda e: e.dma_start(out=S["ka"], in_=KA[:]), reads=["KA"], writes=["ka_d"], ndma=1)
            P.barrier()

    def phase_b1(self, I, S, nblk):
        nc, P = self.nc, self.P
        w_in = I["w_in"]
        with ExitStack() as st:
            W = {}
            specs = [("gq", 0, 512), ("gk", 512, 1024), ("gv", 1024, 2048), ("gr", 2048, 3072), ("ga", 3072, 3088), ("dq", 3088, 3600),
                     ("iq", 4624, 5136), ("iw", 5200, 5208), ("gta", 5208, 6232), ("gtb", 6232, 7256)]
            for nm, c0, c1 in specs:
                W[nm] = self.sb(st, "b1w_" + nm, [128, 8, c1 - c0], BF16)
                self.load_w(W[nm][:], w_in, c0, c1, "w_" + nm)
            w_au = self.sb(st, "b1w_au", [16, 512], BF16)
            balpha = self.sb(st, "b1balpha", [128, 512], F32)
            tri = self.sb(st, "b1tri", [128, 128], F32)
            gnorm = self.sb(st, "b1gnorm", [128, D], F32)
            maskT4 = self.sb(st, "b1maskT4", [128, 512], F32)
            xTo = [self.sb(st, "b1xTo%d" % i, [128, 8, 128], BF16) for i in range(2)]
            Sf = self.sb(st, "b1Sf", [128, 4, 256], F32)
            Sb = self.sb(st, "b1Sb", [128, 4, 256], BF16)
            gaTs = self.sb(st, "b1gaTs", [16, 128], BF16)
            zb = self.sb(st, "b1zb", [128, 512], F32)
            lt = self.sb(st, "b1lt", [128, 512], F32)
            eb = self.sb(st, "b1eb", [128, 512], F32)
            enb = self.sb(st, "b1enb", [128, 512], F32)
            qtil = self.sb(st, "b1qtil", [128, 512], BF16)
            ktil = self.sb(st, "b1ktil", [128, 512], BF16)
            gvb = self.sb(st, "b1gvb", [128, 1024], BF16)
            qT = self.sb(st, "b1qT", [128, 4, 128], BF16)
            kTl = self.sb(st, "b1kTl", [128, 4, 128], BF16)
            PT = self.sb(st, "b1PT", [128, 512], BF16)
            oaf = self.sb(st, "b1oaf", [128, D], F32)
            sgr = self.sb(st, "b1sgr", [128, D], F32)
            oanb = self.sb(st, "b1oanb", [128, D], BF16)
            sgab = self.sb(st, "b1sgab", [128, D], BF16)
            sgbb = self.sb(st, "b1sgbb", [128, D], BF16)
            dqTs = self.sb(st, "b1dqTs", [128, 4, 128], BF16)
            iqTs = self.sb(st, "b1iqTs", [128, 4, 128], BF16)
            iws = self.sb(st, "b1iws", [128, 8], F32)
            junk = self.sb(st, "b1junk", [128, D], BF16)
            stat = self.sb(st, "b1stat", [128, 64], F32)
            psT = [self.ps(st, "b1psT%d" % i, [128, 512], BF16) for i in range(2)]
            pA = self.ps(st, "b1pA", [128, 1024])
            pB = self.ps(st, "b1pB", [128, 1024])
            pX = self.ps(st, "b1pX", [128, 512])
            pZ = self.ps(st, "b1pZ", [128, 512])

            P.op("pool", lambda e: e.dma_start(out=w_au[:], in_=I["w_alpha_up"]), writes=["w_au"], ndma=1)
            P.op("sp", lambda e: e.dma_start(out=balpha[:], in_=I["b_alpha"].to_broadcast([128, 512])), writes=["balpha"], ndma=1)
            P.op("sp", lambda e: e.dma_start(out=tri[:], in_=I["tri"]), writes=["tri"], ndma=1)
            P.op("sp", lambda e: e.dma_start(out=gnorm[:], in_=I["gla_norm"].to_broadcast([128, D])), writes=["gnorm"], ndma=1)
            P.op("sp", lambda e: e.dma_start(out=maskT4[:], in_=I["maskT4"]), writes=["maskT4"], ndma=1)

            def tok_major(ps, wt, ncols, xk, xb, wkey):
                def mm(e):
                    r = None
                    for n0 in range(0, ncols, 512):
                        n1 = min(ncols, n0 + 512)
                        for k in range(8):
                            r = e.matmul(ps[:, n0:n1], lhsT=xb[:, k, :], rhs=wt[:, k, n0:n1], start=(k == 0), stop=(k == 7))
                    return r
                return mm

            for i in range(nblk):
                xb = xTo[i % 2]
                xk = ("xTo", i % 2)
                P.op("sp", lambda e, xb=xb, i=i: e.dma_start(out=xb[:], in_=S["xTown"][i]), writes=[xk], ndma=1)
                P.op("sp", lambda e, i=i: e.dma_start(out=Sf[:], in_=S["Ssel"][i]), writes=["Sf"], ndma=1)
                P.op("pool", lambda e: e.tensor_copy(out=Sb[:], in_=Sf[:]), reads=["Sf"], writes=["Sb"])
                def mmga(e, xb=xb):
                    r = None
                    for k in range(8):
                        r = e.matmul(pZ[0:16, 0:128], lhsT=W["ga"][:, k, :], rhs=xb[:, k, :], start=(k == 0), stop=(k == 7))
                    return r
                P.op("pe", mmga, reads=[xk, "w_ga"], writes=["pZ"])
                P.op("dve", lambda e: e.tensor_copy(out=gaTs[:], in_=pZ[0:16, 0:128]), reads=["pZ"], writes=["gaTs"])
                P.op("pe", lambda e: e.matmul(pZ[:], lhsT=gaTs[:], rhs=w_au[:], start=True, stop=True), reads=["gaTs", "w_au"], writes=["pZ"])
                P.op("dve", lambda e: e.tensor_tensor(out=zb[:], in0=pZ[:], in1=balpha[:], op=ALU.add), reads=["pZ", "balpha"], writes=["zb"])
                P.op("act", lambda e: e.activation(out=lt[:], in_=zb[:], func=AF.Exp, scale=-1.0), reads=["zb"], writes=["lt"])
                P.op("act", lambda e: e.activation(out=lt[:], in_=lt[:], func=AF.Ln, bias=1.0, scale=1.0), reads=["lt"], writes=["lt"])
                P.op("pe", lambda e: e.matmul(pZ[:], lhsT=tri[:], rhs=lt[:], start=True, stop=True), reads=["tri", "lt"], writes=["pZ"])
                P.op("act", lambda e: e.activation(out=eb[:], in_=pZ[:], func=AF.Exp), reads=["pZ"], writes=["eb"])
                P.op("act", lambda e: e.activation(out=enb[:], in_=pZ[:], func=AF.Exp, scale=-1.0), reads=["pZ"], writes=["enb"])
                P.op("pe", tok_major(pX, W["gq"], 512, xk, xb, "w_gq"), reads=[xk, "w_gq"], writes=["pX"])
                P.op("dve", lambda e: e.scalar_tensor_tensor(out=qtil[:], in0=pX[:], scalar=128.0 ** -0.5, in1=eb[:], op0=ALU.mult, op1=ALU.mult),
                     reads=["pX", "eb"], writes=["qtil"])
                P.op("pe", tok_major(pX, W["gk"], 512, xk, xb, "w_gk"), reads=[xk, "w_gk"], writes=["pX"])
                P.op("dve", lambda e: e.tensor_tensor(out=ktil[:], in0=pX[:], in1=enb[:], op=ALU.mult), reads=["pX", "enb"], writes=["ktil"])
                P.op("pe", tok_major(pA, W["gv"], 1024, xk, xb, "w_gv"), reads=[xk, "w_gv"], writes=["pA"])
                P.op("act", lambda e: e.copy(out=gvb[:], in_=pA[:]), reads=["pA"], writes=["gvb"])
                self.transposes8(qtil, psT, lambda g, n: qT[:, 0:n, :], ["qtil"], lambda g: "qT", nchunks=4)
                self.transposes8(ktil, [psT[1], psT[0]], lambda g, n: kTl[:, 0:n, :], ["ktil"], lambda g: "kTl", nchunks=4)

                def mmsc(e):
                    r = None
                    for h in range(4):
                        r = e.matmul(pX[:, h * 128:(h + 1) * 128], lhsT=kTl[:, h, :], rhs=qT[:, h, :], start=True, stop=True)
                    return r
                P.op("pe", mmsc, reads=["qT", "kTl"], writes=["pX"])
                P.op("dve", lambda e: e.tensor_tensor(out=PT[:], in0=pX[:], in1=maskT4[:], op=ALU.mult), reads=["pX", "maskT4"], writes=["PT"])

                def mmo(e):
                    r = None
                    for h in range(4):
                        e.matmul(pA[:, h * 256:(h + 1) * 256], lhsT=PT[:, h * 128:(h + 1) * 128], rhs=gvb[:, h * 256:(h + 1) * 256], start=True, stop=False)
                        r = e.matmul(pA[:, h * 256:(h + 1) * 256], lhsT=qT[:, h, :], rhs=Sb[:, h, :], start=False, stop=True)
                    return r
                P.op("pe", mmo, reads=["PT", "gvb", "qT", "Sb"], writes=["pA"])
                for h in range(4):
                    c0, k0 = self.rstd_of(pA[:, h * 256:(h + 1) * 256], 256, stat, 2 * h, "pA", "b1", junk[:, 0:256])
                    P.op("dve", lambda e, h=h, c0=c0: e.scalar_tensor_tensor(out=oaf[:, h * 256:(h + 1) * 256], in0=pA[:, h * 256:(h + 1) * 256], scalar=c0,
                                                                            in1=gnorm[:, h * 256:(h + 1) * 256], op0=ALU.mult, op1=ALU.mult),
                         reads=["pA", k0, "gnorm"], writes=[("oaf", h)])
                P.op("pe", tok_major(pB, W["gr"], 1024, xk, xb, "w_gr"), reads=[xk, "w_gr"], writes=["pB"])
                P.op("act", lambda e: e.activation(out=sgr[:], in_=pB[:], func=AF.Silu), reads=["pB"], writes=["sgr"])
                P.op("pool", lambda e: e.tensor_tensor(out=oanb[:], in0=oaf[:], in1=sgr[:], op=ALU.mult), reads=[("oaf", h) for h in range(4)] + ["sgr"], writes=["oanb"])
                P.op("sp", lambda e, i=i: e.dma_start(out=S["oan"][i * 128:(i + 1) * 128, :], in_=oanb[:]), reads=["oanb"], writes=[("oan_d", i)], ndma=1)
                P.op("pe", tok_major(pB, W["gta"], 1024, xk, xb, "w_gta"), reads=[xk, "w_gta"], writes=["pB"])
                P.op("act", lambda e: e.activation(out=sgab[:], in_=pB[:], func=AF.Sigmoid), reads=["pB"], writes=["sgab"])
                P.op("sp", lambda e, i=i: e.dma_start(out=S["sga"][i * 128:(i + 1) * 128, :], in_=sgab[:]), reads=["sgab"], writes=[("sga_d", i)], ndma=1)
                P.op("pe", tok_major(pA, W["gtb"], 1024, xk, xb, "w_gtb"), reads=[xk, "w_gtb"], writes=["pA"])
                P.op("act", lambda e: e.activation(out=sgbb[:], in_=pA[:], func=AF.Sigmoid), reads=["pA"], writes=["sgbb"])
                P.op("sp", lambda e, i=i: e.dma_start(out=S["sgb"][i * 128:(i + 1) * 128, :], in_=sgbb[:]), reads=["sgbb"], writes=[("sgb_d", i)], ndma=1)
                for nm, dstT, dkey in (("dq", dqTs, "dqT"), ("iq", iqTs, "iqT")):
                    def mmf(e, nm=nm, xb=xb):
                        r = None
                        for c in range(4):
                            for k in range(8):
                                r = e.matmul(pX[:, c * 128:(c + 1) * 128], lhsT=W[nm][:, k, c * 128:(c + 1) * 128], rhs=xb[:, k, :], start=(k == 0), stop=(k == 7))
                        return r
                    P.op("pe", mmf, reads=[xk, "w_" + nm], writes=["pX"])
                    P.op("dve", lambda e, dstT=dstT: e.tensor_scalar(out=dstT[:].rearrange("p c t -> p (c t)"), in0=pX[:], scalar1=0.125, scalar2=None, op0=ALU.mult),
                         reads=["pX"], writes=[dkey + "s"])
                    P.op("sp", lambda e, dstT=dstT, dkey=dkey, i=i: e.dma_start(out=S[dkey][i], in_=dstT[:]), reads=[dkey + "s"], writes=[(dkey + "_d", i)], ndma=1)

                def mmiw(e, xb=xb):
                    r = None
                    for k in range(8):
                        r = e.matmul(pZ[:, 0:8], lhsT=xb[:, k, :], rhs=W["iw"][:, k, :], start=(k == 0), stop=(k == 7))
                    return r
                P.op("pe", mmiw, reads=[xk, "w_iw"], writes=["pZ"])
                P.op("dve", lambda e: e.tensor_scalar(out=iws[:], in0=pZ[:, 0:8], scalar1=8.0 ** -0.5, scalar2=None, op0=ALU.mult), reads=["pZ"], writes=["iws"])
                P.op("sp", lambda e, i=i: e.dma_start(out=S["iw"][i * 128:(i + 1) * 128, :], in_=iws[:]), reads=["iws"], writes=[("iw_d", i)], ndma=1)
            P.barrier()

    def phase_b2(self, I, S, nblk, NIT=14):
        nc, P = self.nc, self.P
        SM = 512 * nblk
        with ExitStack() as st:
            tab = self.sb(st, "b2tab", [32, 8], F32)
            ohrev = self.sb(st, "b2ohrev", [32, 768], F32)
            Vs = self.sb(st, "b2Vs", [8, 768], F32)
            Biasf = self.sb(st, "b2Biasf", [128, 8, 640], F32)
            Biasb = self.sb(st, "b2Biasb", [128, 8, 640], BF16)
            cm = self.sb(st, "b2cm", [128, 512], F32)
            Dg = self.sb(st, "b2Dg", [128, 8, 128], BF16)
            dqT = self.sb(st, "b2dqT", [128, 4, 128], BF16)
            iqT = self.sb(st, "b2iqT", [128, 4, 128], BF16)
            iw = self.sb(st, "b2iw", [128, 8], F32)
            ik2 = [self.sb(st, "b2ik2_%d" % i, [128, 512], BF16) for i in range(2)]
            Rl = [self.sb(st, "b2R%d" % i, [128, 512], BF16) for i in range(2)]
            wk = self.sb(st, "b2wk", [128, SM], F32)
            madd = self.sb(st, "b2madd", [128, SM], BF16)
            madd_b = self.sb(st, "b2madd_b", [128, SM], BF16)
            dqT_b = self.sb(st, "b2dqT_b", [128, 4, 128], BF16)
            jk = self.sb(st, "b2jk", [128, SM], BF16)
            kTp = [self.sb(st, "b2kTp%d" % i, [128, SM], BF16) for i in range(2)]
            vp = [self.sb(st, "b2vp%d" % i, [128, SM // 128, 128], BF16) for i in range(2)]
            tmn = self.sb(st, "b2tmn", [128, 512], F32)
            Pg = [self.sb(st, "b2Pg%d" % i, [128, 512], BF16) for i in range(3)]
            PTs = [self.sb(st, "b2PT%d" % i, [128, 4, 128], BF16) for i in range(2)]
            ob = self.sb(st, "b2ob", [128, 512], BF16)
            sc = self.sb(st, "b2sc", [128, 16], F32)
            pw = self.sb(st, "b2pw", [128, NIT], F32)
            steps = self.sb(st, "b2steps", [128, NIT], F32)
            mids = self.sb(st, "b2mids", [128, NIT + 1], F32)
            cntd = self.sb(st, "b2cntd", [128, NIT], F32)
            cnta = self.sb(st, "b2cnta", [128, NIT], F32)
            mg = [self.sb(st, "b2mg%d" % i, [128, 16], F32) for i in range(2)]
            lcol = [self.sb(st, "b2lcol%d" % i, [128, 16], F32) for i in range(2)]
            psD = [self.ps(st, "b2psD%d" % i, [128, 512]) for i in range(2)]
            psI = self.ps(st, "b2psI", [128, 512])
            psQ = [self.ps(st, "b2psQ%d" % i, [128, 512]) for i in range(2)]
            psT = [self.ps(st, "b2psT%d" % i, [128, 512], BF16) for i in range(2)]
            po = self.ps(st, "b2po", [128, 512])
            psM = psD[0]

            P.op("sp", lambda e: e.dma_start(out=tab[:], in_=I["rel_bias_table"]), writes=["tab"], ndma=1)
            P.op("sp", lambda e: e.dma_start(out=ohrev[:], in_=I["ohrev"]), writes=["ohrev"], ndma=1)
            P.op("sp", lambda e: e.dma_start(out=cm[:], in_=I["cm"]), writes=["cm"], ndma=1)
            for k in range(NIT):
                P.op("pool", lambda e, k=k: e.memset(pw[:, k:k + 1], 2.0 ** -(k + 1)), writes=[("pw", k)])
            pw_keys = [("pw", k) for k in range(NIT)]

            def mmv(e):
                e.matmul(psI[0:8, 0:384], lhsT=tab[:], rhs=ohrev[:, 0:384], start=True, stop=True)
                return e.matmul(psQ[0][0:8, 0:384], lhsT=tab[:], rhs=ohrev[:, 384:768], start=True, stop=True)
            P.op("pe", mmv, reads=["tab", "ohrev"], writes=["psI", ("psQ", 0)])
            P.op("dve", lambda e: e.tensor_copy(out=Vs[:, 0:384], in_=psI[0:8, 0:384]), reads=["psI"], writes=["Vs0"])
            P.op("dve", lambda e: e.tensor_copy(out=Vs[:, 384:768], in_=psQ[0][0:8, 0:384]), reads=[("psQ", 0)], writes=["Vs1"])
            P.op("sp", lambda e: e.dma_start(out=S["vbias"], in_=Vs[:]), reads=["Vs0", "Vs1"], writes=["vbias_d"], ndma=1)
            for r0 in range(0, 128, 16):
                def ld(e, r0=r0):
                    out = []
                    for r in range(r0, r0 + 16):
                        out.append(e.dma_start(out=Biasf[r:r + 1, :, :], in_=S["vbias"][:, 127 - r:127 - r + 640].unsqueeze(0)))
                    return out
                P.op("sp" if (r0 // 16) % 2 == 0 else "pool", ld, reads=["vbias_d"], writes=[("Biasf", r0)], ndma=16)
            P.op("dve", lambda e: e.tensor_copy(out=Biasb[:], in_=Biasf[:]), reads=[("Biasf", r0) for r0 in range(0, 128, 16)], writes=["Biasb"])

            st_ = {"ikc": 0, "pgc": 0}
            KAf = self.sb(st, "b2KAf", [128, 4], F32)
            KAblk = self.sb(st, "b2KAblk", [128, 4, 2], BF16)
            absq = self.sb(st, "b2absq", [128, 4, 128], BF16)
            negb2 = [self.sb(st, "b2negb%d" % i_, [128, 8], F32) for i_ in range(2)]
            P.op("sp", lambda e: e.dma_start(out=KAf[:], in_=S["ka"]), writes=["KAf"], ndma=1)
            P.op("dve", lambda e: e.memset(KAblk[:], 0.0), writes=["KAblk"])
            P.op("dve", lambda e: e.tensor_copy(out=KAblk[0:64, :, 0], in_=KAf[0:64, :]), reads=["KAf", "KAblk"], writes=["KAblk"])
            P.op("dve", lambda e: e.tensor_copy(out=KAblk[64:128, :, 1], in_=KAf[64:128, :]), reads=["KAf", "KAblk"], writes=["KAblk"])
            madd2 = [madd, madd_b]
            dqT2 = [dqT, dqT_b]

            def stageX(i):
                Si = 512 * (i + 1)
                ng = i + 1
                par = i % 2
                maddc = madd2[par]
                dq_ = dqT2[par]
                P.op("sp", lambda e, i=i, dq_=dq_: e.dma_start(out=dq_[:], in_=S["dqT"][i]), writes=[("dqT", par)], ndma=1)
                P.op("sp", lambda e, i=i: e.dma_start(out=iqT[:], in_=S["iqT"][i]), writes=["iqT"], ndma=1)
                P.op("sp", lambda e, i=i: e.dma_start(out=iw[:], in_=S["iw"][i * 128:(i + 1) * 128, :]), writes=["iw"], ndma=1)
                for h in range(8):
                    P.op("pool", lambda e, h=h: e.tensor_scalar(out=Dg[:, h, :], in0=self.idf[:], scalar1=iw[:, h:h + 1], scalar2=None, op0=ALU.mult),
                         reads=["idf", "iw"], writes=[("Dg", h)], cost=0.4)
                P.op("act", lambda e, dq_=dq_: e.activation(out=absq[:].rearrange("p c t -> p (c t)"), in_=dq_[:].rearrange("p c t -> p (c t)"), func=AF.Abs),
                     reads=[("dqT", par)], writes=["absq"], cost=0.6)

                def mmb(e):
                    r = None
                    for p in range(4):
                        r = e.matmul(psI[:, 2 * p:2 * p + 2], lhsT=absq[:, p, :], rhs=KAblk[:, p, :], start=True, stop=True)
                    return r
                P.op("pe", mmb, reads=["absq", "KAblk"], writes=["psI"], cost=0.4)
                nb_ = negb2[par]
                P.op("dve", lambda e, nb_=nb_: e.tensor_scalar(out=nb_[:], in0=psI[:, 0:8], scalar1=-1.0, scalar2=None, op0=ALU.mult), reads=["psI"], writes=[("negb", par)], cost=0.1)
                yield
                for g in range(ng):
                    ikb = ik2[st_["ikc"] % 2]
                    kik = ("ik2", st_["ikc"] % 2)
                    st_["ikc"] += 1

                    def ldik(e, ikb=ikb, g=g):
                        a_ = e.dma_start(out=ikb[0:64, :], in_=S["ikT"][:, g * 512:(g + 1) * 512])
                        b_ = e.dma_start(out=ikb[64:128, :], in_=S["ikT"][:, g * 512:(g + 1) * 512])
                        return [a_, b_]
                    P.op("sp", ldik, writes=[kik], ndma=2)
                    for h in range(8):
                        hp = h % 2
                        pd = psD[h % 2]
                        P.op("pe", lambda e, h=h, hp=hp, pd=pd, ikb=ikb: e.matmul(pd[:], lhsT=iqT[hp * 64:(hp + 1) * 64, h // 2, :], rhs=ikb[hp * 64:(hp + 1) * 64, :],
                                                                                  start=True, stop=True),
                             reads=["iqT", kik], writes=[("psD", h % 2)], cost=0.25)
                        rl_ = Rl[h % 2]
                        if h % 2 == 0:
                            P.op("act", lambda e, rl_=rl_, pd=pd: e.activation(out=rl_[:], in_=pd[:], func=AF.Relu), reads=[("psD", h % 2)], writes=[("R", h % 2)], cost=0.6)
                        else:
                            P.op("dve", lambda e, rl_=rl_, pd=pd: e.tensor_scalar(out=rl_[:], in0=pd[:], scalar1=0.0, scalar2=None, op0=ALU.max),
                                 reads=[("psD", h % 2)], writes=[("R", h % 2)], cost=0.6)
                        P.op("pe", lambda e, h=h, rl_=rl_: e.matmul(psI[:], lhsT=Dg[:, h, :], rhs=rl_[:], start=(h == 0), stop=(h == 7)),
                             reads=[("Dg", h), ("R", h % 2)], writes=["psI"], cost=0.25)
                    gs = slice(g * 512, (g + 1) * 512)
                    if g < ng - 1:
                        P.op("act", lambda e, gs=gs: e.copy(out=wk[:, gs], in_=psI[:]), reads=["psI"], writes=[("wk", g)], cost=0.6)
                    else:
                        P.op("dve", lambda e, gs=gs: e.tensor_tensor(out=wk[:, gs], in0=psI[:], in1=cm[:], op=ALU.add), reads=["psI", "cm"], writes=[("wk", g)], cost=0.6)
                        P.op("dve", lambda e: e.tensor_tensor(out=tmn[:], in0=psI[:], in1=cm[:], op=ALU.subtract), reads=["psI", "cm"], writes=["tmn"], cost=0.6)
                    yield
                wk_keys = [("wk", g) for g in range(ng)]
                LO, W_, TT, SG, HI, MN1, MN2, THR = [sc[:, c:c + 1] for c in range(8)]
                big = Si / 960.0 + 0.1
                P.op("dve", lambda e, Si=Si: e.tensor_reduce(out=HI, in_=wk[:, 0:Si], axis=AX.X, op=ALU.max), reads=wk_keys, writes=["hi"], cost=big)
                P.op("dve", lambda e: e.tensor_reduce(out=MN1, in_=tmn[:], axis=AX.X, op=ALU.min), reads=["tmn"], writes=["mn1"], cost=0.6)
                if i > 0:
                    P.op("dve", lambda e, Si=Si: e.tensor_reduce(out=MN2, in_=wk[:, 0:Si - 512], axis=AX.X, op=ALU.min), reads=wk_keys, writes=["mn2"], cost=big)
                    P.op("dve", lambda e: e.tensor_tensor(out=MN1, in0=MN1, in1=MN2, op=ALU.min), reads=["mn1", "mn2"], writes=["mn1"], cost=0.1)
                P.op("dve", lambda e: e.tensor_scalar(out=LO, in0=MN1, scalar1=-1.0, scalar2=None, op0=ALU.add), reads=["mn1"], writes=["lo"], cost=0.1)
                P.op("dve", lambda e: e.scalar_tensor_tensor(out=W_, in0=HI, scalar=1.0, in1=LO, op0=ALU.add, op1=ALU.subtract), reads=["hi", "lo"], writes=["w"], cost=0.1)
                P.op("dve", lambda e: e.tensor_scalar(out=steps[:], in0=pw[:], scalar1=W_, scalar2=None, op0=ALU.mult), reads=pw_keys + ["w"], writes=["steps"], cost=0.1)
                P.op("dve", lambda e: e.tensor_tensor(out=mids[:, 0:1], in0=LO, in1=steps[:, 0:1], op=ALU.add), reads=["lo", "steps"], writes=[("mid", 0)], cost=0.1)
                P.op("pool", lambda e: e.memset(cntd[:], 0.0), writes=["cntd"] + [("cntd", it) for it in range(NIT)], cost=0.2)
                P.op("pool", lambda e: e.memset(cnta[:], 0.0), writes=["cnta"] + [("cnta", it) for it in range(NIT)], cost=0.2)
                yield
                Sh = (Si // 2 + 127) // 128 * 128
                n2 = Si - Sh
                half = Sh / 960.0 + 0.1
                for it in range(NIT):
                    MID = mids[:, it:it + 1]
                    P.op("act", lambda e, it=it, Sh=Sh, Si=Si, MID=MID: e.activation(out=jk[:, Sh:Si], in_=wk[:, Sh:Si], func=AF.Sign, bias=MID, scale=-1.0, accum_out=cnta[:, it:it + 1]),
                         reads=wk_keys + [("mid", it), "cnta"], writes=["jka", ("cnta", it)], cost=half)
                    P.op("dve", lambda e, it=it, Sh=Sh, MID=MID: e.tensor_scalar(out=jk[:, 0:Sh], in0=wk[:, 0:Sh], scalar1=MID, scalar2=0.0, op0=ALU.is_ge, op1=ALU.add,
                                                                                accum_out=cntd[:, it:it + 1]),
                         reads=wk_keys + [("mid", it), "cntd"], writes=["jkd", ("cntd", it)], cost=half)
                    P.op("dve", lambda e, it=it: e.scalar_tensor_tensor(out=TT, in0=cnta[:, it:it + 1], scalar=-0.5, in1=cntd[:, it:it + 1], op0=ALU.mult, op1=ALU.add),
                         reads=[("cnta", it), ("cntd", it)], writes=["tt"], cost=0.1)
                    P.op("dve", lambda e, n2=n2: e.tensor_scalar(out=SG, in0=TT, scalar1=255.5 - n2 / 2.0, scalar2=-0.5, op0=ALU.is_ge, op1=ALU.add), reads=["tt"], writes=["sg"], cost=0.1)
                    P.op("dve", lambda e, it=it, MID=MID: e.scalar_tensor_tensor(out=mids[:, it + 1:it + 2], in0=steps[:, it:it + 1], scalar=SG, in1=MID, op0=ALU.mult, op1=ALU.add),
                         reads=["steps", "sg", ("mid", it)], writes=[("mid", it + 1)], cost=0.1)
                    yield
                P.op("dve", lambda e: e.scalar_tensor_tensor(out=THR, in0=steps[:, NIT - 1:NIT], scalar=-0.5, in1=mids[:, NIT:NIT + 1], op0=ALU.mult, op1=ALU.add),
                     reads=["steps", ("mid", NIT)], writes=["thr"], cost=0.1)
                P.op("dve", lambda e, Si=Si, maddc=maddc: e.tensor_scalar(out=maddc[:, 0:Si], in0=wk[:, 0:Si], scalar1=THR, scalar2=NEG, op0=ALU.is_lt, op1=ALU.mult),
                     reads=wk_keys + ["thr"], writes=[("madd", par)], cost=big)
                yield

            def stageY(i):
                Si = 512 * (i + 1)
                ng = i + 1
                par = i % 2
                maddc = madd2[par]
                dq_ = dqT2[par]
                kdq = ("dqT", par)
                kmadd = ("madd", par)
                nkb = Si // 128

                def pass1(h, kb_, kk, p, rows):
                    mgb = mg[h % 2]
                    for g in range(ng):
                        pq = psM
                        gs = slice(g * 512, (g + 1) * 512)
                        P.op("pe", lambda e, pq=pq, rows=rows, p=p, kb_=kb_, gs=gs: e.matmul(pq[:], lhsT=dq_[rows, p, :], rhs=kb_[rows, gs], start=True, stop=True),
                             reads=[kdq, kk], writes=[("psD", 0)], cost=0.25)
                        P.op("dve", lambda e, pq=pq, g=g, mgb=mgb: e.tensor_reduce(out=mgb[:, g:g + 1], in_=pq[:], axis=AX.X, op=ALU.max),
                             reads=[("psD", 0)], writes=[("mg", h % 2, g)], cost=0.6)
                    if ng > 1:
                        P.op("dve", lambda e, mgb=mgb, ng=ng: e.tensor_reduce(out=mgb[:, 15:16], in_=mgb[:, 0:ng], axis=AX.X, op=ALU.max),
                             reads=[("mg", h % 2, g) for g in range(ng)], writes=[("m", h % 2)], cost=0.1)
                    else:
                        P.op("dve", lambda e, mgb=mgb: e.tensor_copy(out=mgb[:, 15:16], in_=mgb[:, 0:1]),
                             reads=[("mg", h % 2, 0)], writes=[("m", h % 2)], cost=0.1)
                    P.op("dve", lambda e, mgb=mgb: e.tensor_scalar(out=mgb[:, 14:15], in0=mgb[:, 15:16], scalar1=-1.0, scalar2=None, op0=ALU.mult),
                         reads=[("m", h % 2)], writes=[("negm", h % 2)], cost=0.1)

                loaded = {}

                def load_pair(p):
                    kb_ = kTp[p % 2]
                    vb_ = vp[p % 2]
                    kk = ("kTp", p % 2)
                    kv = ("vp", p % 2)
                    dc = Si * 256 * 128 / 150e3 + 2.0
                    P.op("sp", lambda e, kb_=kb_, p=p, Si=Si: e.dma_start(out=kb_[:, 0:Si], in_=S["kT"][:, p, 0:Si]), writes=[kk], ndma=1, cost=dc)
                    P.op("pool", lambda e, vb_=vb_, p=p, nkb=nkb: e.dma_start(out=vb_[:, 0:nkb, :], in_=S["v2"][:, p, 0:nkb, :]), writes=[kv], ndma=1, cost=dc)
                    loaded[p] = (kb_, vb_, kk, kv)

                load_pair(0)
                nb_ = negb2[par]
                for h in range(8):
                    p, hh = h // 2, h % 2
                    rows = slice(hh * 64, (hh + 1) * 64)
                    kb_, vb_, kk, kv = loaded[p]
                    if hh == 0 and p + 1 < 4:
                        load_pair(p + 1)
                    mgb = mg[h % 2]
                    lc = lcol[h % 2]
                    P.op("pool", lambda e, lc=lc: e.memset(lc[:], 0.0), writes=[("lcol", h % 2)] + [("lc", h % 2, g) for g in range(ng)], cost=0.2)
                    for g in range(ng):
                        pq = psQ[g % 2]
                        gs = slice(g * 512, (g + 1) * 512)

                        def qk2(e, pq=pq, rows=rows, p=p, kb_=kb_, gs=gs, g=g, h=h, ng=ng):
                            e.matmul(pq[:], lhsT=dq_[rows, p, :], rhs=kb_[rows, gs], start=True, stop=False)
                            if g == ng - 1:
                                e.matmul(pq[:], lhsT=self.idb[:], rhs=Biasb[:, h, 128:640], start=False, stop=False)
                            elif g == ng - 2:
                                e.matmul(pq[:, 384:512], lhsT=self.idb[:], rhs=Biasb[:, h, 0:128], start=False, stop=False)
                            return e.matmul(pq[:], lhsT=self.idb[:], rhs=maddc[:, gs], start=False, stop=True)
                        P.op("pe", qk2, reads=[kdq, kk, "idb", "Biasb", kmadd], writes=[("psQ", g % 2)], cost=0.5)
                        pgb = Pg[st_["pgc"] % 3]
                        kpg = ("Pg", st_["pgc"] % 3)
                        st_["pgc"] += 1
                        P.op("act", lambda e, pq=pq, pgb=pgb, nb_=nb_, lc=lc, g=g, h=h: e.activation(out=pgb[:], in_=pq[:], func=AF.Exp, bias=nb_[:, h:h + 1], scale=1.0,
                                                                                                     accum_out=lc[:, g:g + 1]),
                             reads=[("psQ", g % 2), ("negb", par), ("lcol", h % 2)], writes=[kpg, ("lc", h % 2, g)], cost=0.65)
                        qi = g % 2

                        def tr(e, pgb=pgb, qi=qi):
                            r = None
                            for c in range(4):
                                r = e.transpose(out=psT[qi][:, c * 128:(c + 1) * 128], in_=pgb[:, c * 128:(c + 1) * 128], identity=self.idb[:])
                            return r
                        P.op("pe", tr, reads=[kpg, "idb"], writes=[("psT", qi)], cost=0.3)
                        if qi == 0:
                            P.op("dve", lambda e, qi=qi: e.tensor_copy(out=PTs[qi][:].rearrange("p c t -> p (c t)"), in_=psT[qi][:]), reads=[("psT", qi)], writes=[("PT", qi)], cost=0.6)
                        else:
                            P.op("act", lambda e, qi=qi: e.copy(out=PTs[qi][:].rearrange("p c t -> p (c t)"), in_=psT[qi][:]), reads=[("psT", qi)], writes=[("PT", qi)], cost=0.6)

                        def pv(e, g=g, qi=qi, hh=hh, vb_=vb_, nkb=nkb):
                            r = None
                            for c in range(4):
                                kb = 4 * g + c
                                r = e.matmul(po[:, 0:64], lhsT=PTs[qi][:, c, :], rhs=vb_[:, kb, hh * 64:(hh + 1) * 64], start=(kb == 0), stop=(kb == nkb - 1))
                            return r
                        P.op("pe", pv, reads=[("PT", qi), kv], writes=["po"], cost=0.45)
                    LS, RL_ = sc[:, 8:9], sc[:, 9:10]
                    P.op("dve", lambda e, lc=lc: e.tensor_reduce(out=LS, in_=lc[:, 0:16], axis=AX.X, op=ALU.add), reads=[("lc", h % 2, g) for g in range(ng)] + [("lcol", h % 2)], writes=["ls"], cost=0.1)
                    P.op("dve", lambda e: e.reciprocal(out=RL_, in_=LS), reads=["ls"], writes=["rl"], cost=0.1)
                    P.op("dve", lambda e, h=h: e.tensor_scalar(out=ob[:, h * 64:(h + 1) * 64], in0=po[:, 0:64], scalar1=RL_, scalar2=None, op0=ALU.mult),
                         reads=["po", "rl"], writes=[("ob", h)], cost=0.15)
                    yield
                P.op("sp", lambda e, i=i: e.dma_start(out=S["ob"][i * 128:(i + 1) * 128, :], in_=ob[:]), reads=[("ob", h) for h in range(8)], writes=[("ob_d", i)], ndma=1)

            for _ in stageX(0):
                pass
            for i in range(nblk):
                gx = stageX(i + 1) if i + 1 < nblk else None
                nchunks = (2 + (i + 2) + 1 + NIT + 1) if gx is not None else 0
                per = (nchunks + 7) // 8
                for _ in stageY(i):
                    if gx is not None:
                        for _k in range(per):
                            try:
                                next(gx)
                            except StopIteration:
                                gx = None
                                break
                if gx is not None:
                    for _ in gx:
                        pass
            P.barrier()

    def phase_b3(self, I, S, nblk):
        nc, P = self.nc, self.P
        with ExitStack() as st:
            wgp = self.sb(st, "b3wgp", [128, 8, D], BF16)
            wdp = self.sb(st, "b3wdp", [128, 4, D], BF16)
            wout = self.sb(st, "b3wout", [128, 8, D], BF16)
            wcq = self.sb(st, "b3wcq", [128, 8, 512], BF16)
            wckv = self.sb(st, "b3wckv", [128, 8, D], BF16)
            wco = self.sb(st, "b3wco", [128, 4, D], BF16)
            G = {}
            for nm in ("mix_post", "cross_pre", "cross_post", "mem_norm"):
                G[nm] = self.sb(st, "b3g_" + nm, [128, D], F32)
                P.op("sp", lambda e, nm=nm: e.dma_start(out=G[nm][:], in_=I[nm].to_broadcast([128, D])), writes=["g_" + nm], ndma=1)
            self.load_w(wgp[:], I["w_gla_proj"], 0, D, "wgp")
            self.load_w(wdp[:], I["w_dsa_proj"], 0, D, "wdp", kchunks=4)
            self.load_w(wout[:], I["w_out"], 0, D, "wout")
            self.load_w(wcq[:], I["w_cq"], 0, 512, "wcq")
            self.load_w(wckv[:], I["w_ckv"], 0, D, "wckv")
            self.load_w(wco[:], I["w_co"], 0, D, "wco", kchunks=4)
            memf = self.sb(st, "b3memf", [128, D], F32)
            xnb = self.sb(st, "b3xnb", [128, D], BF16)
            xnb_2 = [xnb, self.sb(st, "b3xnb1", [128, D], BF16)]
            memT = self.sb(st, "b3memT", [128, 8, 256], BF16)
            kmT = self.sb(st, "b3kmT", [128, 4, 256], BF16)
            vm = self.sb(st, "b3vm", [128, 2, 512], BF16)
            oanb_2 = [self.sb(st, "b3oanb%d" % q_, [128, D], BF16) for q_ in range(2)]
            obb_2 = [self.sb(st, "b3obb%d" % q_, [128, 512], BF16) for q_ in range(2)]
            sga_2 = [self.sb(st, "b3sga%d" % q_, [128, D], BF16) for q_ in range(2)]
            sgb_2 = [self.sb(st, "b3sgb%d" % q_, [128, D], BF16) for q_ in range(2)]
            h1o_2 = [self.sb(st, "b3h1o%d" % q_, [128, D], F32) for q_ in range(2)]
            oT_2 = [self.sb(st, "b3oT%d" % q_, [128, 8, 128], BF16) for q_ in range(2)]
            obT_2 = [self.sb(st, "b3obT%d" % q_, [128, 4, 128], BF16) for q_ in range(2)]
            gay = self.sb(st, "b3gay", [128, D], F32)
            t1 = self.sb(st, "b3t1", [128, D], F32)
            mrg_2 = [self.sb(st, "b3mrg%d" % q_, [128, D], BF16) for q_ in range(2)]
            mT_2 = [self.sb(st, "b3mT%d" % q_, [128, 8, 128], BF16) for q_ in range(2)]
            tmp = self.sb(st, "b3tmp", [128, D], F32)
            h2t_2 = [self.sb(st, "b3h2t%d" % q_, [128, D], F32) for q_ in range(2)]
            h3t_2 = [self.sb(st, "b3h3t%d" % q_, [128, D], F32) for q_ in range(2)]
            xcT_2 = [self.sb(st, "b3xcT%d" % q_, [128, 8, 128], BF16) for q_ in range(2)]
            qcT_2 = [self.sb(st, "b3qcT%d" % q_, [128, 4, 128], BF16) for q_ in range(2)]
            Pc_2 = [self.sb(st, "b3Pc%d" % q_, [128, D], BF16) for q_ in range(2)]
            PcT_2 = [self.sb(st, "b3PcT%d" % q_, [128, 8, 128], BF16) for q_ in range(2)]
            ox_2 = [self.sb(st, "b3ox%d" % q_, [128, 512], BF16) for q_ in range(2)]
            oxT_2 = [self.sb(st, "b3oxT%d" % q_, [128, 4, 128], BF16) for q_ in range(2)]
            junk = self.sb(st, "b3junk", [128, D], BF16)
            stat = self.sb(st, "b3stat", [128, 64], F32)
            sc_2 = [self.sb(st, "b3sc%d" % q_, [128, 16], F32) for q_ in range(2)]
            psT = [self.ps(st, "b3psT%d" % i, [128, 512], BF16) for i in range(2)]
            pA = self.ps(st, "b3pA", [128, 1024])
            pB = self.ps(st, "b3pB", [128, 1024])
            pX = self.ps(st, "b3pX", [128, 512])

            def proj(ps, xT_, wt, nk, keys):
                def mm(e):
                    r = None
                    for nh in range(2):
                        for k in range(nk):
                            r = e.matmul(ps[:, nh * 512:(nh + 1) * 512], lhsT=xT_[:, k, :], rhs=wt[:, k, nh * 512:(nh + 1) * 512], start=(k == 0), stop=(k == nk - 1))
                    return r
                return mm

            for mc in range(2):
                P.op("sp", lambda e, mc=mc: e.dma_start(out=memf[:], in_=I["mem"][mc * 128:(mc + 1) * 128, :]), writes=["memf"], ndma=1)
                c0, k0 = self.rstd_of(memf[:], D, stat, 2 * mc, "memf", "b3m", junk[:])
                P.op("dve", lambda e, c0=c0: e.scalar_tensor_tensor(out=xnb[:], in0=memf[:], scalar=c0, in1=G["mem_norm"][:], op0=ALU.mult, op1=ALU.mult),
                     reads=["memf", k0, "g_mem_norm"], writes=["xnb"])
                self.transposes8(xnb, psT, lambda g, n, mc=mc: memT[:, 4 * g:4 * g + n, mc * 128:(mc + 1) * 128], ["xnb"], lambda g, mc=mc: ("memT", mc, g))
            memT_keys = [("memT", mc, g) for mc in range(2) for g in range(2)]

            def mmk(e):
                r = None
                for h in range(4):
                    for k in range(8):
                        r = e.matmul(pA[:, h * 256:(h + 1) * 256], lhsT=wckv[:, k, h * 128:(h + 1) * 128], rhs=memT[:, k, :], start=(k == 0), stop=(k == 7))
                return r
            P.op("pe", mmk, reads=memT_keys + ["wckv"], writes=["pA"])
            P.op("act", lambda e: e.copy(out=kmT[:].rearrange("p h m -> p (h m)"), in_=pA[:]), reads=["pA"], writes=["kmT"])
            for mc in range(2):
                def mmvm(e, mc=mc):
                    r = None
                    for k in range(8):
                        r = e.matmul(pB[:, 0:512], lhsT=memT[:, k, mc * 128:(mc + 1) * 128], rhs=wckv[:, k, 512:1024], start=(k == 0), stop=(k == 7))
                    return r
                P.op("pe", mmvm, reads=memT_keys + ["wckv"], writes=["pB"])
                P.op("act", lambda e, mc=mc: e.copy(out=vm[:, mc, :], in_=pB[:, 0:512]), reads=["pB"], writes=[("vm", mc)])
            vm_keys = [("vm", 0), ("vm", 1)]

            SH = {"pA", "pB", "pX", "idb", "idf", "kmT", "epsc", "tmp", "gay", "t1"}

            def shared(k):
                if isinstance(k, str):
                    return k in SH or k.startswith("g_") or k.startswith("w")
                if isinstance(k, tuple):
                    return isinstance(k[0], tuple) or k[0] in ("psT", "vm", "stat", "junk")
                return True
            orig_op = P.op

            def tile_body(i):
                rs = slice(i * 128, (i + 1) * 128)
                par = i % 2
                (oanb, obb, sga, sgb, h1o, oT, obT, mrg, mT, h2t, h3t, xcT, qcT, Pc, PcT, ox, oxT, sc, xnb) = [
                    x[par] for x in (oanb_2, obb_2, sga_2, sgb_2, h1o_2, oT_2, obT_2, mrg_2, mT_2, h2t_2, h3t_2, xcT_2, qcT_2, Pc_2, PcT_2, ox_2, oxT_2, sc_2, xnb_2)]

                def OPW(eng, fn, reads=(), writes=(), par=par, **kw):
                    mk = lambda k: k if shared(k) else ("par%d" % par, k)
                    return orig_op(eng, fn, reads=[mk(k) for k in reads], writes=[mk(k) for k in writes], **kw)
                P.op = OPW
                P.op("sp", lambda e, rs=rs: e.dma_start(out=oanb[:], in_=S["oan"][rs, :]), writes=["oanb"], ndma=1)
                P.op("sp", lambda e, rs=rs: e.dma_start(out=obb[:], in_=S["ob"][rs, :]), writes=["obb"], ndma=1)
                P.op("sp", lambda e, rs=rs: e.dma_start(out=sga[:], in_=S["sga"][rs, :]), writes=["sga"], ndma=1)
                P.op("sp", lambda e, rs=rs: e.dma_start(out=sgb[:], in_=S["sgb"][rs, :]), writes=["sgb"], ndma=1)
                P.op("sp", lambda e, rs=rs: e.dma_start(out=h1o[:], in_=S["h1own"][rs, :]), writes=["h1o"], ndma=1)
                self.transposes8(oanb, psT, lambda g, n: oT[:, 4 * g:4 * g + n, :], ["oanb"], lambda g: ("oT", g))
                P.op("pe", proj(pA, oT, wgp, 8, None), reads=[("oT", 0), ("oT", 1), "wgp"], writes=["pA"])
                P.op("dve", lambda e: e.tensor_tensor(out=gay[:], in0=pA[:], in1=sga[:], op=ALU.mult), reads=["pA", "sga"], writes=["gay"])
                self.transposes8(obb, psT, lambda g, n: obT[:, 0:n, :], ["obb"], lambda g: "obT", nchunks=4)
                P.op("pe", proj(pB, obT, wdp, 4, None), reads=["obT", "wdp"], writes=["pB"])
                P.op("dve", lambda e: e.tensor_tensor(out=t1[:], in0=pB[:], in1=sgb[:], op=ALU.mult), reads=["pB", "sgb"], writes=["t1"])
                P.op("pool", lambda e: e.tensor_tensor(out=mrg[:], in0=t1[:], in1=gay[:], op=ALU.add), reads=["t1", "gay"], writes=["mrg"])
                self.transposes8(mrg, psT, lambda g, n: mT[:, 4 * g:4 * g + n, :], ["mrg"], lambda g: ("mT", g))
                P.op("pe", proj(pA, mT, wout, 8, None), reads=[("mT", 0), ("mT", 1), "wout"], writes=["pA"])
                c0, k0 = self.rstd_of(pA[:], D, stat, 8, "pA", "b3a", junk[:])
                P.op("dve", lambda e, c0=c0: e.scalar_tensor_tensor(out=tmp[:], in0=pA[:], scalar=c0, in1=G["mix_post"][:], op0=ALU.mult, op1=ALU.mult),
                     reads=["pA", k0, "g_mix_post"], writes=["tmp"])
                P.op("pool", lambda e: e.tensor_tensor(out=h2t[:], in0=tmp[:], in1=h1o[:], op=ALU.add), reads=["tmp", "h1o"], writes=["h2t"])
                c0, k0 = self.rstd_of(h2t[:], D, stat, 10, "h2t", "b3b", junk[:])
                P.op("dve", lambda e, c0=c0: e.scalar_tensor_tensor(out=xnb[:], in0=h2t[:], scalar=c0, in1=G["cross_pre"][:], op0=ALU.mult, op1=ALU.mult),
                     reads=["h2t", k0, "g_cross_pre"], writes=["xnb"])
                self.transposes8(xnb, psT, lambda g, n: xcT[:, 4 * g:4 * g + n, :], ["xnb"], lambda g: ("xcT", g))

                def mmq(e):
                    r = None
                    for h in range(4):
                        for k in range(8):
                            r = e.matmul(pX[:, h * 128:(h + 1) * 128], lhsT=wcq[:, k, h * 128:(h + 1) * 128], rhs=xcT[:, k, :], start=(k == 0), stop=(k == 7))
                    return r
                P.op("pe", mmq, reads=[("xcT", 0), ("xcT", 1), "wcq"], writes=["pX"])
                P.op("dve", lambda e: e.tensor_scalar(out=qcT[:].rearrange("p h t -> p (h t)"), in0=pX[:], scalar1=128.0 ** -0.5, scalar2=None, op0=ALU.mult),
                     reads=["pX"], writes=["qcT"])

                def mml(e):
                    r = None
                    for h in range(4):
                        r = e.matmul(pB[:, h * 256:(h + 1) * 256], lhsT=qcT[:, h, :], rhs=kmT[:, h, :], start=True, stop=True)
                    return r
                P.op("pe", mml, reads=["qcT", "kmT"], writes=["pB"])
                MX, NMX, LC, RLC = sc[:, 0:4], sc[:, 4:8], sc[:, 8:12], sc[:, 12:16]
                P.op("dve", lambda e: e.tensor_reduce(out=MX, in_=pB[:].rearrange("p (h m) -> p h m", h=4), axis=AX.X, op=ALU.max), reads=["pB"], writes=["mx"])
                P.op("dve", lambda e: e.tensor_scalar(out=NMX, in0=MX, scalar1=-1.0, scalar2=None, op0=ALU.mult), reads=["mx"], writes=["nmx"])
                P.op("dve", lambda e: e.memset(LC, 0.0), writes=["lc"])
                for h in range(4):
                    P.op("act", lambda e, h=h: e.activation(out=Pc[:, h * 256:(h + 1) * 256], in_=pB[:, h * 256:(h + 1) * 256], func=AF.Exp, bias=sc[:, 4 + h:5 + h], scale=1.0,
                                                            accum_out=sc[:, 8 + h:9 + h]),
                         reads=["pB", "nmx", "lc"], writes=[("Pc", h), ("lc", h)])
                self.transposes8(Pc, psT, lambda g, n: PcT[:, 4 * g:4 * g + n, :], [("Pc", h) for h in range(4)], lambda g: ("PcT", g))

                def mmov(e):
                    r = None
                    for h in range(4):
                        for mc in range(2):
                            r = e.matmul(pX[:, h * 128:(h + 1) * 128], lhsT=PcT[:, h * 2 + mc, :], rhs=vm[:, mc, h * 128:(h + 1) * 128], start=(mc == 0), stop=(mc == 1))
                    return r
                P.op("pe", mmov, reads=[("PcT", 0), ("PcT", 1)] + vm_keys, writes=["pX"])
                P.op("dve", lambda e: e.reciprocal(out=RLC, in_=LC), reads=[("lc", h) for h in range(4)], writes=["rlc"])
                for h in range(4):
                    P.op("dve", lambda e, h=h: e.tensor_scalar(out=ox[:, h * 128:(h + 1) * 128], in0=pX[:, h * 128:(h + 1) * 128], scalar1=sc[:, 12 + h:13 + h], scalar2=None, op0=ALU.mult),
                         reads=["pX", "rlc"], writes=[("ox", h)])
                self.transposes8(ox, psT, lambda g, n: oxT[:, 0:n, :], [("ox", h) for h in range(4)], lambda g: "oxT", nchunks=4)
                P.op("pe", proj(pA, oxT, wco, 4, None), reads=["oxT", "wco"], writes=["pA"])
                c0, k0 = self.rstd_of(pA[:], D, stat, 12, "pA", "b3c", junk[:])
                P.op("dve", lambda e, c0=c0: e.scalar_tensor_tensor(out=tmp[:], in0=pA[:], scalar=c0, in1=G["cross_post"][:], op0=ALU.mult, op1=ALU.mult),
                     reads=["pA", k0, "g_cross_post"], writes=["tmp"])
                P.op("pool", lambda e: e.tensor_tensor(out=h3t[:], in0=tmp[:], in1=h2t[:], op=ALU.add), reads=["tmp", "h2t"], writes=["h3t"])
                P.op("sp", lambda e, rs=rs, h3t=h3t: e.dma_start(out=S["h3"][rs, :], in_=h3t[:]), reads=["h3t"], writes=[("h3_d", i)], ndma=1)
            for i in range(nblk):
                tile_body(i)
            P.op = orig_op
            P.barrier()

    def build(self):
        nc, P = self.nc, self.P
        upto = self.upto
        nblk = self.nblk
        I = {}
        for nm, shp in INPUT_SPECS:
            I[nm] = self.din(nm, shp)
        self.I = I
        dbg = self.debug
        S = {}

        def scr(name, shape, dt):
            if dbg:
                S[name] = self.dout(name, shape, dt)
            else:
                S[name] = self.dscr(name, shape, dt)
        scr("h1", [nblk * 512, D], F32)
        scr("h1own", [nblk * 128, D], F32)
        scr("xTown", [nblk, 128, 8, 128], BF16)
        scr("kT", [128, 4, nblk * 512], BF16)
        scr("ikT", [64, nblk * 512], BF16)
        scr("v2", [128, 4, nblk * 4, 128], BF16)
        scr("Ssel", [nblk, 128, 4, 256], F32)
        scr("oan", [nblk * 128, D], BF16)
        scr("sga", [nblk * 128, D], BF16)
        scr("sgb", [nblk * 128, D], BF16)
        scr("dqT", [nblk, 128, 4, 128], BF16)
        scr("iqT", [nblk, 128, 4, 128], BF16)
        scr("iw", [nblk * 128, 8], F32)
        scr("vbias", [8, 768], F32)
        scr("ka", [128, 4], F32)
        scr("ob", [nblk * 128, 512], BF16)
        scr("h3", [nblk * 128, D], F32)
        S["out"] = self.dout("out", [nblk * 128, D], F32)
        with ExitStack() as st:
            self.consts(st)
            self.ffn_phase("f1", I["xall"], nblk * 512, I["ffn1_w_in"], I["ffn1_w_out"], I["ffn1_pre"], I["ffn1_post"], S["h1"])
            if upto != "A1":
                self.phase_a2(I, S, nblk)
            if upto not in ("A1", "A2"):
                self.phase_b1(I, S, nblk)
            if upto not in ("A1", "A2", "B1"):
                self.phase_b2(I, S, nblk)
            if upto not in ("A1", "A2", "B1", "B2"):
                self.phase_b3(I, S, nblk)
            if upto not in ("A1", "A2", "B1", "B2", "B3"):
                self.ffn_phase("f2", S["h3"], nblk * 128, I["ffn2_w_in"], I["ffn2_w_out"], I["ffn2_pre"], I["ffn2_post"], S["out"])
            P.emit(st)
        return nc


INPUT_SPECS = [
    ("xall", [T, D]), ("idn", [128, 128]), ("tri", [128, 128]), ("ej", [128, 4]),
    ("ffn1_pre", [1, D]), ("ffn1_post", [1, D]), ("ffn1_w_in", [D, 2 * DFF]), ("ffn1_w_out", [DFF, D]),
    ("mix_pre", [1, D]), ("w_in", [D, 7256]), ("w_alpha_up", [16, 512]), ("b_alpha", [1, 512]),
    ("gla_norm", [1, D]), ("maskT4", [128, 512]),
    ("rel_bias_table", [32, 8]), ("ohrev", [32, 768]), ("cm", [128, 512]),
    ("mem", [256, D]), ("mix_post", [1, D]), ("cross_pre", [1, D]), ("cross_post", [1, D]), ("mem_norm", [1, D]),
    ("w_gla_proj", [D, D]), ("w_dsa_proj", [512, D]), ("w_out", [D, D]), ("w_cq", [D, 512]), ("w_ckv", [D, D]), ("w_co", [512, D]),
    ("ffn2_pre", [1, D]), ("ffn2_post", [1, D]), ("ffn2_w_in", [D, 2 * DFF]), ("ffn2_w_out", [DFF, D]),
]


def t5_bucket_np(rel):
    rel = np.asarray(rel, dtype=np.int64)
    relf = np.maximum(rel, 1).astype(np.float32)
    large = 16 + (np.log(relf / np.float32(16)) / np.float32(np.log(8.0)) * np.float32(16)).astype(np.int32)
    large = np.minimum(large, 31)
    return np.where(rel < 16, rel, large)


def make_in_maps(inputs, ncores=8):
    in_maps = []
    idn = np.eye(128, dtype=np.float32)
    tri = np.triu(np.ones((128, 128), dtype=np.float32)) * (-1.0 / 16.0)
    for c in range(ncores):
        b, j = c // 4, c % 4
        ej = np.zeros((128, 4), dtype=np.float32)
        ej[:, j] = 1.0
        maskT = np.triu(np.ones((128, 128), dtype=np.float32))
        m = {"xall": np.ascontiguousarray(inputs["x"][b]), "idn": idn, "tri": tri, "ej": ej, "maskT4": np.ascontiguousarray(np.tile(maskT, (1, 4)))}
        mp = np.arange(768)
        rel = 128 * j + 255 - mp
        oh = np.zeros((32, 768), dtype=np.float32)
        ok = rel >= 0
        bk = t5_bucket_np(np.maximum(rel, 0))
        oh[bk[ok], mp[ok]] += 1.0
        oh[31, mp[ok]] -= 1.0
        m["ohrev"] = oh
        cc = np.arange(512)[None, :]
        rr = np.arange(128)[:, None]
        m["cm"] = np.where(cc <= 128 * j + rr, 0.0, NEG).astype(np.float32)
        m["rel_bias_table"] = np.ascontiguousarray(inputs["rel_bias_table"]).astype(np.float32)
        m["mem"] = np.ascontiguousarray(inputs["mem"][b])
        for nm, shp in INPUT_SPECS:
            if nm in m:
                continue
            m[nm] = np.ascontiguousarray(inputs[nm][0]).reshape(shp)
        in_maps.append(m)
    return in_maps


def run(inputs, upto, nblk=16, debug=True, ncores=8):
    kb = KB(upto)
    kb.nblk = nblk
    kb.debug = debug
    nc = kb.build()
    in_maps = make_in_maps(inputs, ncores)
    res = run_bass_kernel_spmd(nc, in_maps, core_ids=list(range(ncores)))
    return res


def kernel(**inputs):
    inputs = {k: np.asarray(v) for k, v in inputs.items()}
    res = run(inputs, "ALL", nblk=16, debug=False)
    out = np.zeros((2, T, D), dtype=np.float32)
    for c in range(8):
        b, j = c // 4, c % 4
        o = np.asarray(res.results[c]["out"]).reshape(16, 128, D)
        ov = out[b].reshape(16, 4, 128, D)
        ov[:, j] = o
    return out
```

```python
import numpy as np
from contextlib import ExitStack
import concourse.bass as bass
import concourse.mybir as mybir
from concourse.bass_utils import run_bass_kernel_spmd

F32 = mybir.dt.float32
BF16 = mybir.dt.bfloat16
AF = mybir.ActivationFunctionType
ALU = mybir.AluOpType
AX = mybir.AxisListType

D = 1024
DFF = 2816
T = 8192
EPS = 1e-6
NEG = -30000.0

ENGS = ("pe", "act", "dve", "pool", "sp")


class Op:
    __slots__ = ("eng", "fn", "deps", "odeps", "needs_sig", "sig", "ndma", "idx", "dsem", "cost", "epoch", "start", "finish", "ready", "npend", "done")

    def __init__(self, eng, fn, ndma, cost):
        self.eng = eng
        self.fn = fn
        self.deps = set()
        self.odeps = set()
        self.needs_sig = False
        self.sig = None
        self.ndma = ndma
        self.dsem = None
        self.cost = cost
        self.epoch = 0
        self.start = 0.0
        self.finish = 0.0
        self.ready = 0.0
        self.npend = 0
        self.done = False


DEF_COST = {"pe": 0.35, "act": 0.7, "dve": 0.7, "pool": 0.9, "sp": 2.5}
SCHED = True
WINDOW = 24


class Prog:
    def __init__(self, nc, n_dma_sems=64):
        self.nc = nc
        self.ops = {e: [] for e in ENGS}
        self.lastw = {}
        self.readers = {}
        self.n_dma_sems = n_dma_sems
        self.all_ops = []
        self.epoch = 0

    def op(self, eng, fn, reads=(), writes=(), ndma=0, cost=None):
        if cost is None:
            cost = 2.5 if ndma > 0 else DEF_COST[eng]
        o = Op(eng, fn, ndma, cost)
        o.idx = len(self.all_ops)
        o.epoch = self.epoch
        self.all_ops.append(o)
        is_dma = ndma > 0
        for k in reads:
            w = self.lastw.get(k)
            if w is not None:
                self._dep(o, w, True, is_dma)
        for k in writes:
            w = self.lastw.get(k)
            if w is not None:
                self._dep(o, w, False, is_dma)
            for r in self.readers.get(k, ()):
                self._dep(o, r, False, is_dma)
        for k in reads:
            self.readers.setdefault(k, []).append(o)
        for k in writes:
            self.lastw[k] = o
            self.readers[k] = []
        self.ops[eng].append(o)
        return o

    def _dep(self, o, d, raw, is_dma):
        if d is o:
            return
        if d.eng == o.eng and d.ndma == 0 and not is_dma:
            if o.eng == "pe" or not raw:
                o.odeps.add(d)
                return
        o.deps.add(d)
        d.needs_sig = True

    def barrier(self):
        for e in ENGS:
            o = Op(e, None, 0, 0.0)
            o.idx = len(self.all_ops)
            o.epoch = self.epoch
            self.all_ops.append(o)
            self.ops[e].append(o)
        self.epoch += 1
        self.lastw = {}
        self.readers = {}

    def schedule(self):
        LAT = 0.2
        succs = {}
        for o in self.all_ops:
            o.npend = 0
            o.ready = 0.0
            o.done = False
        for o in self.all_ops:
            if o.fn is None:
                continue
            for d in (o.deps | o.odeps):
                succs.setdefault(d.idx, []).append(o)
                o.npend += 1
        nep = self.epoch + 1
        ep_remaining = [0] * (nep + 1)
        ep_finish = [0.0] * (nep + 1)
        for o in self.all_ops:
            if o.fn is not None:
                ep_remaining[o.epoch] += 1
        head = {e: 0 for e in ENGS}
        eng_free = {e: 0.0 for e in ENGS}
        order = {e: [] for e in ENGS}
        remaining = len(self.all_ops)
        if not SCHED:
            t = 0.0
            for o in self.all_ops:
                o.start = t
                t += 1.0
                order[o.eng].append(o)
            self.ops = order
            return
        while remaining > 0:
            best = None
            bstart = None
            for e in ENGS:
                q = self.ops[e]
                i = head[e]
                cnt = 0
                n = len(q)
                while i < n and cnt < WINDOW:
                    o = q[i]
                    if not o.done:
                        if o.fn is None:
                            if cnt == 0 and ep_remaining[o.epoch] == 0:
                                st_ = max(eng_free[e], ep_finish[o.epoch] + LAT)
                                if best is None or st_ < bstart or (st_ == bstart and o.idx < best.idx):
                                    best, bstart = o, st_
                            break
                        if o.npend == 0:
                            st_ = max(eng_free[e], o.ready)
                            if best is None or st_ < bstart or (st_ == bstart and o.idx < best.idx):
                                best, bstart = o, st_
                        cnt += 1
                    i += 1
            assert best is not None, "scheduler deadlock"
            o = best
            e = o.eng
            o.done = True
            o.start = bstart
            remaining -= 1
            if o.fn is None:
                o.finish = bstart
                eng_free[e] = max(eng_free[e], bstart)
            else:
                o.finish = bstart + o.cost
                eng_free[e] = bstart + (0.08 * o.ndma if o.ndma > 0 else o.cost)
                ep_remaining[o.epoch] -= 1
                if o.finish > ep_finish[o.epoch]:
                    ep_finish[o.epoch] = o.finish
                for s_ in succs.get(o.idx, ()):
                    s_.npend -= 1
                    if o.finish + LAT > s_.ready:
                        s_.ready = o.finish + LAT
            order[e].append(o)
            q = self.ops[e]
            while head[e] < len(q) and q[head[e]].done:
                head[e] += 1
        self.ops = order

    def emit(self, stack):
        nc = self.nc
        self.schedule()
        last_in_epoch = {}
        dmas_in_epoch = {}
        for e in ENGS:
            for o in self.ops[e]:
                if o.fn is None:
                    continue
                if o.ndma > 0:
                    dmas_in_epoch.setdefault(o.epoch, []).append(o)
                else:
                    last_in_epoch[(o.epoch, e)] = o
        for e in ENGS:
            for o in self.ops[e]:
                if o.fn is None:
                    for e2 in ENGS:
                        d = last_in_epoch.get((o.epoch, e2))
                        if d is not None and e2 != e:
                            o.deps.add(d)
                            d.needs_sig = True
                    for d in dmas_in_epoch.get(o.epoch, ()):
                        o.deps.add(d)
        esem = {e: stack.enter_context(nc.semaphore("s_" + e)) for e in ENGS}
        dsems = [stack.enter_context(nc.semaphore("d%d" % i)) for i in range(self.n_dma_sems)]
        dcount = [0] * self.n_dma_sems
        dlast = [None] * self.n_dma_sems
        ecount = {e: 0 for e in ENGS}
        rr = 0
        glob = sorted(self.all_ops, key=lambda x: (x.start, x.idx))
        pos = {}
        for e in ENGS:
            for n_, o in enumerate(self.ops[e]):
                pos[o.idx] = n_
        for o in glob:
            if o.ndma > 0:
                s = rr % self.n_dma_sems
                rr += 1
                if dlast[s] is not None:
                    o.deps.add(dlast[s])
                dcount[s] += 16 * o.ndma
                o.sig = (dsems[s], dcount[s])
                o.dsem = dsems[s]
                dlast[s] = o
        for e in ENGS:
            for o in self.ops[e]:
                if o.ndma == 0 and o.needs_sig and o.fn is not None:
                    ecount[e] += 1
                    o.sig = (esem[e], ecount[e])
        engobj = {"pe": nc.tensor, "act": nc.scalar, "dve": nc.vector, "pool": nc.gpsimd, "sp": nc.sync}
        block = stack.enter_context(nc.Block())

        def run(e):
            eng = engobj[e]
            waited = {}
            for o in self.ops[e]:
                for d in sorted(o.deps, key=lambda x: x.idx):
                    if d.sig is None:
                        continue
                    sem, val = d.sig
                    key = id(sem)
                    if waited.get(key, 0) < val:
                        eng.wait_ge(sem, val)
                        waited[key] = val
                if o.fn is None:
                    continue
                r = o.fn(eng)
                if o.ndma > 0:
                    rs = r if isinstance(r, (list, tuple)) else [r]
                    assert len(rs) == o.ndma, (len(rs), o.ndma)
                    for i in rs:
                        i.then_inc(o.dsem, 16)
                elif o.sig is not None:
                    last = r[-1] if isinstance(r, (list, tuple)) else r
                    last.then_inc(o.sig[0], 1)

        @block.tensor
        def _(e):
            run("pe")

        @block.scalar
        def _(e):
            run("act")

        @block.vector
        def _(e):
            run("dve")

        @block.gpsimd
        def _(e):
            run("pool")

        @block.sync
        def _(e):
            run("sp")


class KB:
    def __init__(self, upto):
        self.upto = upto
        self.nc = bass.Bass("TRN2", target_bir_lowering=False)
        self.P = Prog(self.nc)
        self.uid = 0

    def din(self, name, shape, dt=F32):
        return self.nc.dram_tensor(name, list(shape), dt, kind="ExternalInput").ap()

    def dout(self, name, shape, dt=F32):
        return self.nc.dram_tensor(name, list(shape), dt, kind="ExternalOutput").ap()

    def dscr(self, name, shape, dt):
        return self.nc.dram_tensor(name, list(shape), dt, kind="Internal").ap()

    def sb(self, st, name, shape, dt):
        return st.enter_context(self.nc.sbuf_tensor(name, list(shape), dt))

    def ps(self, st, name, shape, dt=F32):
        return st.enter_context(self.nc.psum_tensor(name, list(shape), dt))

    def consts(self, st):
        P = self.P
        self.idn_d = self.I["idn"]
        idf = self.sb(st, "idf", [128, 128], F32)
        self.idf = idf
        self.idb = self.sb(st, "idb", [128, 128], BF16)
        self.epsc = self.sb(st, "epsc", [128, 1], F32)
        P.op("sp", lambda e: e.dma_start(out=idf[:], in_=self.idn_d), writes=["idf"], ndma=1)
        P.op("dve", lambda e: e.tensor_copy(out=self.idb[:], in_=idf[:]), reads=["idf"], writes=["idb"])
        P.op("dve", lambda e: e.memset(self.epsc[:], EPS), writes=["epsc"])

    def rstd_of(self, src, width, stat, col, key_src, tag, junk):
        P = self.P
        c0 = stat[:, col:col + 1]
        c1 = stat[:, col + 1:col + 2]
        k0 = ("stat", tag, col)
        P.op("dve", lambda e: e.memset(c0, 0.0), writes=[k0])
        P.op("act", lambda e: e.activation(out=junk, in_=src, func=AF.Square, accum_out=c0),
             reads=[key_src, k0], writes=[k0, ("junk", tag)])
        P.op("act", lambda e: e.activation(out=c1, in_=c0, func=AF.Sqrt, bias=self.epsc[:, 0:1], scale=1.0 / width),
             reads=[k0, "epsc"], writes=[(k0, 1)])
        P.op("dve", lambda e: e.reciprocal(out=c0, in_=c1), reads=[(k0, 1)], writes=[k0])
        return c0, k0

    def ffn_phase(self, name, src_d, ntok, w_in_d, w_out_d, pre_d, post_d, dst_d):
        nc, P = self.nc, self.P
        with ExitStack() as st:
            wi = self.sb(st, name + "wi", [128, 8, 2 * DFF], BF16)
            wo = self.sb(st, name + "wo", [128, 22, D], BF16)
            gpre = self.sb(st, name + "gpre", [128, D], F32)
            gpost = self.sb(st, name + "gpost", [128, D], F32)
            xt = [self.sb(st, name + "xt%d" % i, [128, D], F32) for i in range(2)]
            xr = [self.sb(st, name + "xr%d" % i, [128, D], F32) for i in range(2)]
            xn = self.sb(st, name + "xn", [128, D], BF16)
            xT = self.sb(st, name + "xT", [128, 8, 512], BF16)
            h1T = self.sb(st, name + "h1T", [128, 22, 512], BF16)
            sa = [self.sb(st, name + "sa%d" % i, [128, 512], F32) for i in range(2)]
            tmp = self.sb(st, name + "tmp", [128, D], F32)
            junk = self.sb(st, name + "junk", [128, D], BF16)
            stat = self.sb(st, name + "stat", [128, 64], F32)
            psA = [self.ps(st, name + "psA%d" % i, [128, 512]) for i in range(2)]
            psB = [self.ps(st, name + "psB%d" % i, [128, 512]) for i in range(2)]
            psY = self.ps(st, name + "psY", [128, 1024])
            psT = [self.ps(st, name + "psT%d" % i, [128, 512], BF16) for i in range(2)]

            wi_src = w_in_d.rearrange("(k p) n -> p k n", p=128)
            for k in range(8):
                P.op("pool", lambda e, k=k: e.dma_start(out=wi[:, k, :], in_=wi_src[:, k, :]), writes=[("wi", k)], ndma=1)
            wo_src = w_out_d.rearrange("(f p) n -> p f n", p=128)
            for f0 in range(0, 22, 6):
                f1 = min(22, f0 + 6)
                P.op("pool", lambda e, f0=f0, f1=f1: e.dma_start(out=wo[:, f0:f1, :], in_=wo_src[:, f0:f1, :]),
                     writes=[("wo", f) for f in range(f0, f1)], ndma=1)
            P.op("sp", lambda e: e.dma_start(out=gpre[:], in_=pre_d.to_broadcast([128, D])), writes=["gpre"], ndma=1)
            P.op("sp", lambda e: e.dma_start(out=gpost[:], in_=post_d.to_broadcast([128, D])), writes=["gpost"], ndma=1)
            wi_keys = [("wi", k) for k in range(8)]
            wo_keys = [("wo", f) for f in range(22)]

            nblk = ntok // 512
            tcount = 0
            for blk in range(nblk):
                for tt in range(4):
                    r0 = blk * 512 + tt * 128
                    xb = xt[tcount % 2]
                    kx = ("xt", tcount % 2)
                    tcount += 1
                    P.op("sp", lambda e, xb=xb, r0=r0: e.dma_start(out=xb[:], in_=src_d[r0:r0 + 128, :]), writes=[kx], ndma=1)
                    c0, k0 = self.rstd_of(xb[:], D, stat, 2 * (tcount % 8), kx, name, junk[:])
                    P.op("dve", lambda e, xb=xb, c0=c0: e.scalar_tensor_tensor(out=xn[:], in0=xb[:], scalar=c0, in1=gpre[:], op0=ALU.mult, op1=ALU.mult),
                         reads=[kx, k0, "gpre"], writes=["xn"])
                    for g in range(2):
                        def tr(e, g=g):
                            r = None
                            for kk in range(4):
                                k = 4 * g + kk
                                r = e.transpose(out=psT[g][:, kk * 128:(kk + 1) * 128], in_=xn[:, k * 128:(k + 1) * 128], identity=self.idb[:])
                            return r
                        P.op("pe", tr, reads=["xn", "idb"], writes=[("psT", g)])
                        eng = "act" if g == 0 else "dve"
                        if eng == "act":
                            P.op("act", lambda e, g=g, tt=tt: e.copy(out=xT[:, 4 * g:4 * g + 4, tt * 128:(tt + 1) * 128],
                                                                     in_=psT[g][:].rearrange("p (c t) -> p c t", c=4)),
                                 reads=[("psT", g)], writes=[("xT", tt, g)])
                        else:
                            P.op("dve", lambda e, g=g, tt=tt: e.tensor_copy(out=xT[:, 4 * g:4 * g + 4, tt * 128:(tt + 1) * 128],
                                                                            in_=psT[g][:].rearrange("p (c t) -> p c t", c=4)),
                                 reads=[("psT", g)], writes=[("xT", tt, g)])
                xT_keys = [("xT", tt, g) for tt in range(4) for g in range(2)]
                for f in range(22):
                    pa, pb = psA[f % 2], psB[f % 2]

                    def mmab(e, f=f, pa=pa, pb=pb):
                        for k in range(8):
                            e.matmul(pa[:], lhsT=wi[:, k, f * 128:(f + 1) * 128], rhs=xT[:, k, :], start=(k == 0), stop=(k == 7))
                        r = None
                        for k in range(8):
                            r = e.matmul(pb[:], lhsT=wi[:, k, DFF + f * 128:DFF + (f + 1) * 128], rhs=xT[:, k, :], start=(k == 0), stop=(k == 7))
                        return r
                    P.op("pe", mmab, reads=wi_keys + xT_keys, writes=[("psA", f % 2), ("psB", f % 2)])
                    s = sa[f % 2]
                    P.op("act", lambda e, s=s, pa=pa: e.activation(out=s[:], in_=pa[:], func=AF.Silu), reads=[("psA", f % 2)], writes=[("sa", f % 2)])
                    P.op("dve", lambda e, s=s, pb=pb, f=f: e.tensor_tensor(out=h1T[:, f, :], in0=s[:], in1=pb[:], op=ALU.mult),
                         reads=[("sa", f % 2), ("psB", f % 2)], writes=[("h1T", f)])
                h1T_keys = [("h1T", f) for f in range(22)]
                for tt in range(4):
                    r0 = blk * 512 + tt * 128

                    def mmy(e, tt=tt):
                        r = None
                        for nh in range(2):
                            for f in range(22):
                                r = e.matmul(psY[:, nh * 512:(nh + 1) * 512], lhsT=h1T[:, f, tt * 128:(tt + 1) * 128],
                                             rhs=wo[:, f, nh * 512:(nh + 1) * 512], start=(f == 0), stop=(f == 21))
                        return r
                    P.op("pe", mmy, reads=h1T_keys + wo_keys, writes=["psY"])
                    xres = xr[tt % 2]
                    kr = ("xr", tt % 2)
                    P.op("sp", lambda e, xres=xres, r0=r0: e.dma_start(out=xres[:], in_=src_d[r0:r0 + 128, :]), writes=[kr], ndma=1)
                    c0, k0 = self.rstd_of(psY[:], D, stat, 16 + 2 * (tt % 4), "psY", name + "y", junk[:])
                    P.op("dve", lambda e, c0=c0: e.scalar_tensor_tensor(out=tmp[:], in0=psY[:], scalar=c0, in1=gpost[:], op0=ALU.mult, op1=ALU.mult),
                         reads=["psY", k0, "gpost"], writes=["tmp"])
                    P.op("dve", lambda e, xres=xres: e.scalar_tensor_tensor(out=xres[:], in0=tmp[:], scalar=0.5, in1=xres[:], op0=ALU.mult, op1=ALU.add),
                         reads=["tmp", kr], writes=[kr])
                    P.op("sp", lambda e, xres=xres, r0=r0: e.dma_start(out=dst_d[r0:r0 + 128, :], in_=xres[:]), reads=[kr], writes=[("dst", name, r0)], ndma=1)
            P.barrier()

    def transposes8(self, xn, psT, xT_out_fn, keys_in, key_out_fn, nchunks=8):
        P = self.P
        ng = (nchunks + 3) // 4
        for g in range(ng):
            n = min(4, nchunks - 4 * g)

            def tr(e, g=g, n=n):
                r = None
                for kk in range(n):
                    k = 4 * g + kk
                    r = e.transpose(out=psT[g % 2][:, kk * 128:(kk + 1) * 128], in_=xn[:, k * 128:(k + 1) * 128], identity=self.idb[:])
                return r
            P.op("pe", tr, reads=list(keys_in) + ["idb"], writes=[("psT", g % 2)])
            dst = xT_out_fn(g, n)
            src = psT[g % 2][:, 0:n * 128].rearrange("p (c t) -> p c t", c=n)
            if g % 2 == 0:
                P.op("act", lambda e, dst=dst, src=src: e.copy(out=dst, in_=src), reads=[("psT", g % 2)], writes=[key_out_fn(g)])
            else:
                P.op("dve", lambda e, dst=dst, src=src: e.tensor_copy(out=dst, in_=src), reads=[("psT", g % 2)], writes=[key_out_fn(g)])

    def load_w(self, dst, src_d, c0, c1, key, kchunks=8):
        src = src_d.rearrange("(k p) n -> p k n", p=128)
        self.P.op("pool", lambda e: e.dma_start(out=dst, in_=src[:, :, c0:c1]), writes=[key], ndma=1)

    def phase_a2(self, I, S, nblk):
        nc, P = self.nc, self.P
        w_in = I["w_in"]
        with ExitStack() as st:
            def dbl(name, shape, dt):
                return [self.sb(st, "a2%s%d" % (name, i), shape, dt) for i in range(2)]
            w_gk = self.sb(st, "a2w_gk", [128, 8, 512], BF16)
            w_gv = self.sb(st, "a2w_gv", [128, 8, 1024], BF16)
            w_dv = self.sb(st, "a2w_dv", [128, 8, 512], BF16)
            w_dk = self.sb(st, "a2w_dk", [128, 8, 512], BF16)
            w_ik = self.sb(st, "a2w_ik", [128, 8, 64], BF16)
            w_ga = self.sb(st, "a2w_ga", [128, 8, 16], BF16)
            w_au = self.sb(st, "a2w_au", [16, 512], BF16)
            balpha = self.sb(st, "a2balpha", [128, 512], F32)
            gpre = self.sb(st, "a2gpre", [128, D], F32)
            tri = self.sb(st, "a2tri", [128, 128], F32)
            negs = self.sb(st, "a2negs", [128, 1], F32)
            ej = self.sb(st, "a2ej", [128, 4], F32)
            ht = dbl("ht", [128, D], F32)
            hown = dbl("hown", [128, D], F32)
            xn = dbl("xn", [128, D], BF16)
            xT = dbl("xT", [128, 8, 512], BF16)
            xTown = dbl("xTown", [128, 8, 128], BF16)
            junk = self.sb(st, "a2junk", [128, D], BF16)
            stat = self.sb(st, "a2stat", [128, 64], F32)
            kTs = dbl("kTs", [128, 4, 512], BF16)
            ikTs = dbl("ikTs", [64, 512], BF16)
            gaT = dbl("gaT", [16, 512], BF16)
            zb = dbl("zb", [128, 512], F32)
            lt = dbl("lt", [128, 512], F32)
            enb = dbl("enb", [128, 512], F32)
            ktil = dbl("ktil", [128, 512], BF16)
            gvb = dbl("gvb", [128, 1024], BF16)
            dvb = dbl("dvb", [128, 512], BF16)
            decay = dbl("decay", [128, 4], F32)
            Sst = self.sb(st, "a2S", [128, 4, 256], F32)
            Stmp = dbl("Stmp", [128, 4, 256], F32)
            Ssel = dbl("Ssel", [128, 4, 256], F32)
            psT = [self.ps(st, "a2psT%d" % i, [128, 512], BF16) for i in range(2)]
            psW = self.ps(st, "a2psW", [128, 1024])
            psX = [self.ps(st, "a2psX%d" % i, [128, 512]) for i in range(2)]
            psZ = [self.ps(st, "a2psZ%d" % i, [128, 512]) for i in range(2)]

            self.load_w(w_gk[:], w_in, 512, 1024, "w_gk")
            self.load_w(w_gv[:], w_in, 1024, 2048, "w_gv")
            self.load_w(w_dv[:], w_in, 4112, 4624, "w_dv")
            self.load_w(w_dk[:], w_in, 3600, 4112, "w_dk")
            self.load_w(w_ik[:], w_in, 5136, 5200, "w_ik")
            self.load_w(w_ga[:], w_in, 3072, 3088, "w_ga")
            P.op("pool", lambda e: e.dma_start(out=w_au[:], in_=I["w_alpha_up"]), writes=["w_au"], ndma=1)
            P.op("sp", lambda e: e.dma_start(out=balpha[:], in_=I["b_alpha"].to_broadcast([128, 512])), writes=["balpha"], ndma=1)
            P.op("sp", lambda e: e.dma_start(out=gpre[:], in_=I["mix_pre"].to_broadcast([128, D])), writes=["gpre"], ndma=1)
            P.op("sp", lambda e: e.dma_start(out=tri[:], in_=I["tri"]), writes=["tri"], ndma=1)
            P.op("sp", lambda e: e.dma_start(out=ej[:], in_=I["ej"]), writes=["ej"], ndma=1)
            P.op("dve", lambda e: e.memset(negs[:], -1.0 / 16.0), writes=["negs"])
            P.op("dve", lambda e: e.memset(Sst[:], 0.0), writes=[("S", h) for h in range(4)])
            kab = self.sb(st, "a2kab", [128, 4], F32)
            KA = self.sb(st, "a2KA", [128, 4], F32)
            P.op("dve", lambda e: e.memset(KA[:], 0.0), writes=["KA"])

            tcount = 0
            for blk in range(nblk):
                bp = blk % 2
                xTb, hownb, xTownb, kTsb, ikTsb, gaTb, Sselb = xT[bp], hown[bp], xTown[bp], kTs[bp], ikTs[bp], gaT[bp], Ssel[bp]
                for u in range(4):
                    r0 = blk * 512 + u * 128
                    tp = tcount % 2
                    hb = ht[tp]
                    xnb = xn[tp]
                    kh = ("ht", tp)
                    kxn = ("xn", tp)
                    tcount += 1
                    P.op("sp", lambda e, hb=hb, r0=r0: e.dma_start(out=hb[:], in_=S["h1"][r0:r0 + 128, :]), writes=[kh], ndma=1)
                    c0, k0 = self.rstd_of(hb[:], D, stat, 2 * (tcount % 8), kh, "a2", junk[:])
                    P.op("dve", lambda e, hb=hb, c0=c0, xnb=xnb: e.scalar_tensor_tensor(out=xnb[:], in0=hb[:], scalar=c0, in1=gpre[:], op0=ALU.mult, op1=ALU.mult),
                         reads=[kh, k0, "gpre"], writes=[kxn], cost=1.2)
                    if u == 0:
                        P.op("pool", lambda e, hb=hb, hownb=hownb: e.tensor_scalar(out=hownb[:], in0=hb[:], scalar1=ej[:, 0:1], scalar2=None, op0=ALU.mult),
                             reads=[kh, "ej"], writes=[("hown", bp)], cost=2.0)
                    else:
                        P.op("dve", lambda e, hb=hb, u=u, hownb=hownb: e.scalar_tensor_tensor(out=hownb[:], in0=hb[:], scalar=ej[:, u:u + 1], in1=hownb[:], op0=ALU.mult, op1=ALU.add),
                             reads=[kh, "ej", ("hown", bp)], writes=[("hown", bp)], cost=1.2)
                    self.transposes8(xnb, psT, lambda g, n, u=u, xTb=xTb: xTb[:, 4 * g:4 * g + n, u * 128:(u + 1) * 128], [kxn], lambda g, u=u, bp=bp: ("xT", bp, u, g))
                P.op("sp", lambda e, blk=blk, hownb=hownb: e.dma_start(out=S["h1own"][blk * 128:(blk + 1) * 128, :], in_=hownb[:]), reads=[("hown", bp)], writes=[("h1own_d", blk)], ndma=1)
                xT_keys = [("xT", bp, u, g) for u in range(4) for g in range(2)]
                for u in range(4):
                    if u == 0:
                        P.op("pool", lambda e, xTb=xTb, xTownb=xTownb: e.tensor_scalar(out=xTownb[:], in0=xTb[:, :, 0:128], scalar1=ej[:, 0:1], scalar2=None, op0=ALU.mult),
                             reads=xT_keys + ["ej"], writes=[("xTown", bp)], cost=1.5)
                    else:
                        P.op("dve", lambda e, u=u, xTb=xTb, xTownb=xTownb: e.scalar_tensor_tensor(out=xTownb[:], in0=xTb[:, :, u * 128:(u + 1) * 128], scalar=ej[:, u:u + 1], in1=xTownb[:],
                                                                                                  op0=ALU.mult, op1=ALU.add),
                             reads=xT_keys + ["ej", ("xTown", bp)], writes=[("xTown", bp)], cost=1.5)
                P.op("sp", lambda e, blk=blk, xTownb=xTownb: e.dma_start(out=S["xTown"][blk], in_=xTownb[:]), reads=[("xTown", bp)], writes=[("xTown_d", blk)], ndma=1)
                for c in range(4):
                    pf = psX[c % 2]

                    def mmf(e, c=c, pf=pf, xTb=xTb):
                        r = None
                        for k in range(8):
                            r = e.matmul(pf[:], lhsT=w_dk[:, k, c * 128:(c + 1) * 128], rhs=xTb[:, k, :], start=(k == 0), stop=(k == 7))
                        return r
                    P.op("pe", mmf, reads=xT_keys + ["w_dk"], writes=[("psX", c % 2)], cost=1.9)
                    if c % 2 == 0:
                        P.op("act", lambda e, c=c, pf=pf, kTsb=kTsb: e.copy(out=kTsb[:, c, :], in_=pf[:]), reads=[("psX", c % 2)], writes=[("kTs", bp, c)])
                    else:
                        P.op("dve", lambda e, c=c, pf=pf, kTsb=kTsb: e.tensor_copy(out=kTsb[:, c, :], in_=pf[:]), reads=[("psX", c % 2)], writes=[("kTs", bp, c)])
                P.op("sp", lambda e, blk=blk, kTsb=kTsb: e.dma_start(out=S["kT"][:, :, blk * 512:(blk + 1) * 512], in_=kTsb[:]),
                     reads=[("kTs", bp, c) for c in range(4)], writes=[("kT_d", blk)], ndma=1)
                P.op("dve", lambda e, kTsb=kTsb: e.tensor_reduce(out=kab[:], in_=kTsb[:], axis=AX.X, op=ALU.max, apply_absolute_value=True),
                     reads=[("kTs", bp, c) for c in range(4)], writes=["kab"], cost=2.3)
                P.op("dve", lambda e: e.tensor_tensor(out=KA[:], in0=KA[:], in1=kab[:], op=ALU.max), reads=["kab", "KA"], writes=["KA"], cost=0.1)

                def mmik(e, xTb=xTb):
                    r = None
                    for k in range(8):
                        r = e.matmul(psZ[0][0:64, :], lhsT=w_ik[:, k, :], rhs=xTb[:, k, :], start=(k == 0), stop=(k == 7))
                    return r
                P.op("pe", mmik, reads=xT_keys + ["w_ik"], writes=[("psZ", 0)], cost=1.9)
                P.op("act", lambda e, ikTsb=ikTsb: e.copy(out=ikTsb[:], in_=psZ[0][0:64, :]), reads=[("psZ", 0)], writes=[("ikTs", bp)])
                P.op("sp", lambda e, blk=blk, ikTsb=ikTsb: e.dma_start(out=S["ikT"][:, blk * 512:(blk + 1) * 512], in_=ikTsb[:]), reads=[("ikTs", bp)], writes=[("ikT_d", blk)], ndma=1)

                def mmga(e, xTb=xTb):
                    r = None
                    for k in range(8):
                        r = e.matmul(psZ[1][0:16, :], lhsT=w_ga[:, k, :], rhs=xTb[:, k, :], start=(k == 0), stop=(k == 7))
                    return r
                P.op("pe", mmga, reads=xT_keys + ["w_ga"], writes=[("psZ", 1)], cost=1.9)
                P.op("dve", lambda e, gaTb=gaTb: e.tensor_copy(out=gaTb[:], in_=psZ[1][0:16, :]), reads=[("psZ", 1)], writes=[("gaT", bp)])
                for u in range(4):
                    tile = blk * 4 + u
                    tp = tile % 2
                    ucols = slice(u * 128, (u + 1) * 128)
                    pX, pZ = psX[tp], psZ[tp]
                    kX, kZ = ("psX", tp), ("psZ", tp)
                    zb_, lt_, enb_, ktil_, gvb_, dvb_, decay_, Stmp_ = zb[tp], lt[tp], enb[tp], ktil[tp], gvb[tp], dvb[tp], decay[tp], Stmp[tp]

                    def mmgk(e, ucols=ucols, pX=pX, xTb=xTb):
                        r = None
                        for k in range(8):
                            r = e.matmul(pX[:], lhsT=xTb[:, k, ucols], rhs=w_gk[:, k, :], start=(k == 0), stop=(k == 7))
                        return r
                    P.op("pe", mmgk, reads=xT_keys + ["w_gk"], writes=[kX], cost=1.9)
                    P.op("pe", lambda e, ucols=ucols, pZ=pZ, gaTb=gaTb: e.matmul(pZ[:], lhsT=gaTb[:, ucols], rhs=w_au[:], start=True, stop=True), reads=[("gaT", bp), "w_au"], writes=[kZ])
                    P.op("dve", lambda e, pZ=pZ, zb_=zb_: e.tensor_tensor(out=zb_[:], in0=pZ[:], in1=balpha[:], op=ALU.add), reads=[kZ, "balpha"], writes=[("zb", tp)])
                    P.op("act", lambda e, zb_=zb_, lt_=lt_: e.activation(out=lt_[:], in_=zb_[:], func=AF.Exp, scale=-1.0), reads=[("zb", tp)], writes=[("lt", tp)])
                    P.op("act", lambda e, lt_=lt_: e.activation(out=lt_[:], in_=lt_[:], func=AF.Ln, bias=1.0, scale=1.0), reads=[("lt", tp)], writes=[("lt", tp)])
                    P.op("pe", lambda e, pZ=pZ, lt_=lt_: e.matmul(pZ[:], lhsT=tri[:], rhs=lt_[:], start=True, stop=True), reads=["tri", ("lt", tp)], writes=[kZ], cost=1.0)
                    P.op("act", lambda e, pZ=pZ, enb_=enb_: e.activation(out=enb_[:], in_=pZ[:], func=AF.Exp, scale=-1.0), reads=[kZ], writes=[("enb", tp)])
                    P.op("dve", lambda e, pX=pX, enb_=enb_, ktil_=ktil_: e.tensor_tensor(out=ktil_[:], in0=pX[:], in1=enb_[:], op=ALU.mult), reads=[kX, ("enb", tp)], writes=[("ktil", tp)])

                    def mmbl(e, pZ=pZ, lt_=lt_):
                        r = None
                        for h in range(4):
                            r = e.matmul(pZ[:, h:h + 1], lhsT=lt_[:, h * 128:(h + 1) * 128], rhs=negs[:], start=True, stop=True)
                        return r
                    P.op("pe", mmbl, reads=[("lt", tp), "negs"], writes=[kZ], cost=0.8)
                    P.op("act", lambda e, pZ=pZ, decay_=decay_: e.activation(out=decay_[:], in_=pZ[:, 0:4], func=AF.Exp), reads=[kZ], writes=[("decay", tp)], cost=0.3)

                    def mmdv(e, ucols=ucols, pX=pX, xTb=xTb):
                        r = None
                        for k in range(8):
                            r = e.matmul(pX[:], lhsT=xTb[:, k, ucols], rhs=w_dv[:, k, :], start=(k == 0), stop=(k == 7))
                        return r
                    P.op("pe", mmdv, reads=xT_keys + ["w_dv"], writes=[kX], cost=1.9)
                    P.op("act", lambda e, pX=pX, dvb_=dvb_: e.copy(out=dvb_[:], in_=pX[:]), reads=[kX], writes=[("dvb", tp)])
                    P.op("sp", lambda e, tile=tile, dvb_=dvb_: e.dma_start(out=S["v2"][:, :, tile, :], in_=dvb_[:].rearrange("p (a c) -> p a c", a=4)),
                         reads=[("dvb", tp)], writes=[("v2_d", tile)], ndma=1)

                    def mmgv(e, ucols=ucols, xTb=xTb):
                        r = None
                        for nh in range(2):
                            for k in range(8):
                                r = e.matmul(psW[:, nh * 512:(nh + 1) * 512], lhsT=xTb[:, k, ucols], rhs=w_gv[:, k, nh * 512:(nh + 1) * 512], start=(k == 0), stop=(k == 7))
                        return r
                    P.op("pe", mmgv, reads=xT_keys + ["w_gv"], writes=["psW"], cost=3.8)
                    P.op("act", lambda e, gvb_=gvb_: e.copy(out=gvb_[:], in_=psW[:]), reads=["psW"], writes=[("gvb", tp)], cost=1.2)

                    def mmkv(e, ktil_=ktil_, gvb_=gvb_):
                        r = None
                        for h in range(4):
                            r = e.matmul(psW[:, h * 256:(h + 1) * 256], lhsT=ktil_[:, h * 128:(h + 1) * 128], rhs=gvb_[:, h * 256:(h + 1) * 256], start=True, stop=True)
                        return r
                    P.op("pe", mmkv, reads=[("ktil", tp), ("gvb", tp)], writes=["psW"], cost=0.8)
                    for h in range(4):
                        if u == 0:
                            P.op("pool", lambda e, h=h, Sselb=Sselb: e.tensor_scalar(out=Sselb[:, h, :], in0=Sst[:, h, :], scalar1=ej[:, 0:1], scalar2=None, op0=ALU.mult),
                                 reads=[("S", h), "ej"], writes=[("Ssel", bp, h)], cost=0.6)
                        else:
                            P.op("dve", lambda e, h=h, u=u, Sselb=Sselb: e.scalar_tensor_tensor(out=Sselb[:, h, :], in0=Sst[:, h, :], scalar=ej[:, u:u + 1], in1=Sselb[:, h, :],
                                                                                                 op0=ALU.mult, op1=ALU.add),
                                 reads=[("S", h), "ej", ("Ssel", bp, h)], writes=[("Ssel", bp, h)], cost=0.6)
                        P.op("dve", lambda e, h=h, Stmp_=Stmp_: e.tensor_tensor(out=Stmp_[:, h, :], in0=Sst[:, h, :], in1=psW[:, h * 256:(h + 1) * 256], op=ALU.add),
                             reads=[("S", h), "psW"], writes=[("Stmp", tp, h)], cost=0.4)
                        P.op("dve", lambda e, h=h, Stmp_=Stmp_, decay_=decay_: e.tensor_scalar(out=Sst[:, h, :], in0=Stmp_[:, h, :], scalar1=decay_[:, h:h + 1], scalar2=None, op0=ALU.mult),
                             reads=[("Stmp", tp, h), ("decay", tp)], writes=[("S", h)], cost=0.4)
                P.op("sp", lambda e, blk=blk, Sselb=Sselb: e.dma_start(out=S["Ssel"][blk], in_=Sselb[:]), reads=[("Ssel", bp, h) for h in range(4)], writes=[("Ssel_d", blk)], ndma=1)
            P.op("sp", lambda e: e.dma_start(out=S["ka"], in_=KA[:]), reads=["KA"], writes=["ka_d"], ndma=1)
            P.barrier()

    def phase_b1(self, I, S, nblk):
        nc, P = self.nc, self.P
        w_in = I["w_in"]
        with ExitStack() as st:
            W = {}
            specs = [("gq", 0, 512), ("gk", 512, 1024), ("gv", 1024, 2048), ("gr", 2048, 3072), ("ga", 3072, 3088), ("dq", 3088, 3600),
                     ("iq", 4624, 5136), ("iw", 5200, 5208), ("gta", 5208, 6232), ("gtb", 6232, 7256)]
            for nm, c0, c1 in specs:
                W[nm] = self.sb(st, "b1w_" + nm, [128, 8, c1 - c0], BF16)
                self.load_w(W[nm][:], w_in, c0, c1, "w_" + nm)
            w_au = self.sb(st, "b1w_au", [16, 512], BF16)
            balpha = self.sb(st, "b1balpha", [128, 512], F32)
            tri = self.sb(st, "b1tri", [128, 128], F32)
            gnorm = self.sb(st, "b1gnorm", [128, D], F32)
            maskT4 = self.sb(st, "b1maskT4", [128, 512], F32)
            xTo = [self.sb(st, "b1xTo%d" % i, [128, 8, 128], BF16) for i in range(2)]
            Sf = self.sb(st, "b1Sf", [128, 4, 256], F32)
            Sb = self.sb(st, "b1Sb", [128, 4, 256], BF16)
            gaTs = self.sb(st, "b1gaTs", [16, 128], BF16)
            zb = self.sb(st, "b1zb", [128, 512], F32)
            lt = self.sb(st, "b1lt", [128, 512], F32)
            eb = self.sb(st, "b1eb", [128, 512], F32)
            enb = self.sb(st, "b1enb", [128, 512], F32)
            qtil = self.sb(st, "b1qtil", [128, 512], BF16)
            ktil = self.sb(st, "b1ktil", [128, 512], BF16)
            gvb = self.sb(st, "b1gvb", [128, 1024], BF16)
            qT = self.sb(st, "b1qT", [128, 4, 128], BF16)
            kTl = self.sb(st, "b1kTl", [128, 4, 128], BF16)
            PT = self.sb(st, "b1PT", [128, 512], BF16)
            oaf = self.sb(st, "b1oaf", [128, D], F32)
            sgr = self.sb(st, "b1sgr", [128, D], F32)
            oanb = self.sb(st, "b1oanb", [128, D], BF16)
            sgab = self.sb(st, "b1sgab", [128, D], BF16)
            sgbb = self.sb(st, "b1sgbb", [128, D], BF16)
            dqTs = self.sb(st, "b1dqTs", [128, 4, 128], BF16)
            iqTs = self.sb(st, "b1iqTs", [128, 4, 128], BF16)
            iws = self.sb(st, "b1iws", [128, 8], F32)
            junk = self.sb(st, "b1junk", [128, D], BF16)
            stat = self.sb(st, "b1stat", [128, 64], F32)
            psT = [self.ps(st, "b1psT%d" % i, [128, 512], BF16) for i in range(2)]
            pA = self.ps(st, "b1pA", [128, 1024])
            pB = self.ps(st, "b1pB", [128, 1024])
            pX = self.ps(st, "b1pX", [128, 512])
            pZ = self.ps(st, "b1pZ", [128, 512])

            P.op("pool", lambda e: e.dma_start(out=w_au[:], in_=I["w_alpha_up"]), writes=["w_au"], ndma=1)
            P.op("sp", lambda e: e.dma_start(out=balpha[:], in_=I["b_alpha"].to_broadcast([128, 512])), writes=["balpha"], ndma=1)
            P.op("sp", lambda e: e.dma_start(out=tri[:], in_=I["tri"]), writes=["tri"], ndma=1)
            P.op("sp", lambda e: e.dma_start(out=gnorm[:], in_=I["gla_norm"].to_broadcast([128, D])), writes=["gnorm"], ndma=1)
            P.op("sp", lambda e: e.dma_start(out=maskT4[:], in_=I["maskT4"]), writes=["maskT4"], ndma=1)

            def tok_major(ps, wt, ncols, xk, xb, wkey):
                def mm(e):
                    r = None
                    for n0 in range(0, ncols, 512):
                        n1 = min(ncols, n0 + 512)
                        for k in range(8):
                            r = e.matmul(ps[:, n0:n1], lhsT=xb[:, k, :], rhs=wt[:, k, n0:n1], start=(k == 0), stop=(k == 7))
                    return r
                return mm

            for i in range(nblk):
                xb = xTo[i % 2]
                xk = ("xTo", i % 2)
                P.op("sp", lambda e, xb=xb, i=i: e.dma_start(out=xb[:], in_=S["xTown"][i]), writes=[xk], ndma=1)
                P.op("sp", lambda e, i=i: e.dma_start(out=Sf[:], in_=S["Ssel"][i]), writes=["Sf"], ndma=1)
                P.op("pool", lambda e: e.tensor_copy(out=Sb[:], in_=Sf[:]), reads=["Sf"], writes=["Sb"])
                def mmga(e, xb=xb):
                    r = None
                    for k in range(8):
                        r = e.matmul(pZ[0:16, 0:128], lhsT=W["ga"][:, k, :], rhs=xb[:, k, :], start=(k == 0), stop=(k == 7))
                    return r
                P.op("pe", mmga, reads=[xk, "w_ga"], writes=["pZ"])
                P.op("dve", lambda e: e.tensor_copy(out=gaTs[:], in_=pZ[0:16, 0:128]), reads=["pZ"], writes=["gaTs"])
                P.op("pe", lambda e: e.matmul(pZ[:], lhsT=gaTs[:], rhs=w_au[:], start=True, stop=True), reads=["gaTs", "w_au"], writes=["pZ"])
                P.op("dve", lambda e: e.tensor_tensor(out=zb[:], in0=pZ[:], in1=balpha[:], op=ALU.add), reads=["pZ", "balpha"], writes=["zb"])
                P.op("act", lambda e: e.activation(out=lt[:], in_=zb[:], func=AF.Exp, scale=-1.0), reads=["zb"], writes=["lt"])
                P.op("act", lambda e: e.activation(out=lt[:], in_=lt[:], func=AF.Ln, bias=1.0, scale=1.0), reads=["lt"], writes=["lt"])
                P.op("pe", lambda e: e.matmul(pZ[:], lhsT=tri[:], rhs=lt[:], start=True, stop=True), reads=["tri", "lt"], writes=["pZ"])
                P.op("act", lambda e: e.activation(out=eb[:], in_=pZ[:], func=AF.Exp), reads=["pZ"], writes=["eb"])
                P.op("act", lambda e: e.activation(out=enb[:], in_=pZ[:], func=AF.Exp, scale=-1.0), reads=["pZ"], writes=["enb"])
                P.op("pe", tok_major(pX, W["gq"], 512, xk, xb, "w_gq"), reads=[xk, "w_gq"], writes=["pX"])
                P.op("dve", lambda e: e.scalar_tensor_tensor(out=qtil[:], in0=pX[:], scalar=128.0 ** -0.5, in1=eb[:], op0=ALU.mult, op1=ALU.mult),
                     reads=["pX", "eb"], writes=["qtil"])
                P.op("pe", tok_major(pX, W["gk"], 512, xk, xb, "w_gk"), reads=[xk, "w_gk"], writes=["pX"])
                P.op("dve", lambda e: e.tensor_tensor(out=ktil[:], in0=pX[:], in1=enb[:], op=ALU.mult), reads=["pX", "enb"], writes=["ktil"])
                P.op("pe", tok_major(pA, W["gv"], 1024, xk, xb, "w_gv"), reads=[xk, "w_gv"], writes=["pA"])
                P.op("act", lambda e: e.copy(out=gvb[:], in_=pA[:]), reads=["pA"], writes=["gvb"])
                self.transposes8(qtil, psT, lambda g, n: qT[:, 0:n, :], ["qtil"], lambda g: "qT", nchunks=4)
                self.transposes8(ktil, [psT[1], psT[0]], lambda g, n: kTl[:, 0:n, :], ["ktil"], lambda g: "kTl", nchunks=4)

                def mmsc(e):
                    r = None
                    for h in range(4):
                        r = e.matmul(pX[:, h * 128:(h + 1) * 128], lhsT=kTl[:, h, :], rhs=qT[:, h, :], start=True, stop=True)
                    return r
                P.op("pe", mmsc, reads=["qT", "kTl"], writes=["pX"])
                P.op("dve", lambda e: e.tensor_tensor(out=PT[:], in0=pX[:], in1=maskT4[:], op=ALU.mult), reads=["pX", "maskT4"], writes=["PT"])

                def mmo(e):
                    r = None
                    for h in range(4):
                        e.matmul(pA[:, h * 256:(h + 1) * 256], lhsT=PT[:, h * 128:(h + 1) * 128], rhs=gvb[:, h * 256:(h + 1) * 256], start=True, stop=False)
                        r = e.matmul(pA[:, h * 256:(h + 1) * 256], lhsT=qT[:, h, :], rhs=Sb[:, h, :], start=False, stop=True)
                    return r
                P.op("pe", mmo, reads=["PT", "gvb", "qT", "Sb"], writes=["pA"])
                for h in range(4):
                    c0, k0 = self.rstd_of(pA[:, h * 256:(h + 1) * 256], 256, stat, 2 * h, "pA", "b1", junk[:, 0:256])
                    P.op("dve", lambda e, h=h, c0=c0: e.scalar_tensor_tensor(out=oaf[:, h * 256:(h + 1) * 256], in0=pA[:, h * 256:(h + 1) * 256], scalar=c0,
                                                                            in1=gnorm[:, h * 256:(h + 1) * 256], op0=ALU.mult, op1=ALU.mult),
                         reads=["pA", k0, "gnorm"], writes=[("oaf", h)])
                P.op("pe", tok_major(pB, W["gr"], 1024, xk, xb, "w_gr"), reads=[xk, "w_gr"], writes=["pB"])
                P.op("act", lambda e: e.activation(out=sgr[:], in_=pB[:], func=AF.Silu), reads=["pB"], writes=["sgr"])
                P.op("pool", lambda e: e.tensor_tensor(out=oanb[:], in0=oaf[:], in1=sgr[:], op=ALU.mult), reads=[("oaf", h) for h in range(4)] + ["sgr"], writes=["oanb"])
                P.op("sp", lambda e, i=i: e.dma_start(out=S["oan"][i * 128:(i + 1) * 128, :], in_=oanb[:]), reads=["oanb"], writes=[("oan_d", i)], ndma=1)
                P.op("pe", tok_major(pB, W["gta"], 1024, xk, xb, "w_gta"), reads=[xk, "w_gta"], writes=["pB"])
                P.op("act", lambda e: e.activation(out=sgab[:], in_=pB[:], func=AF.Sigmoid), reads=["pB"], writes=["sgab"])
                P.op("sp", lambda e, i=i: e.dma_start(out=S["sga"][i * 128:(i + 1) * 128, :], in_=sgab[:]), reads=["sgab"], writes=[("sga_d", i)], ndma=1)
                P.op("pe", tok_major(pA, W["gtb"], 1024, xk, xb, "w_gtb"), reads=[xk, "w_gtb"], writes=["pA"])
                P.op("act", lambda e: e.activation(out=sgbb[:], in_=pA[:], func=AF.Sigmoid), reads=["pA"], writes=["sgbb"])
                P.op("sp", lambda e, i=i: e.dma_start(out=S["sgb"][i * 128:(i + 1) * 128, :], in_=sgbb[:]), reads=["sgbb"], writes=[("sgb_d", i)], ndma=1)
                for nm, dstT, dkey in (("dq", dqTs, "dqT"), ("iq", iqTs, "iqT")):
                    def mmf(e, nm=nm, xb=xb):
                        r = None
                        for c in range(4):
                            for k in range(8):
                                r = e.matmul(pX[:, c * 128:(c + 1) * 128], lhsT=W[nm][:, k, c * 128:(c + 1) * 128], rhs=xb[:, k, :], start=(k == 0), stop=(k == 7))
                        return r
                    P.op("pe", mmf, reads=[xk, "w_" + nm], writes=["pX"])
                    P.op("dve", lambda e, dstT=dstT: e.tensor_scalar(out=dstT[:].rearrange("p c t -> p (c t)"), in0=pX[:], scalar1=0.125, scalar2=None, op0=ALU.mult),
                         reads=["pX"], writes=[dkey + "s"])
                    P.op("sp", lambda e, dstT=dstT, dkey=dkey, i=i: e.dma_start(out=S[dkey][i], in_=dstT[:]), reads=[dkey + "s"], writes=[(dkey + "_d", i)], ndma=1)

                def mmiw(e, xb=xb):
                    r = None
                    for k in range(8):
                        r = e.matmul(pZ[:, 0:8], lhsT=xb[:, k, :], rhs=W["iw"][:, k, :], start=(k == 0), stop=(k == 7))
                    return r
                P.op("pe", mmiw, reads=[xk, "w_iw"], writes=["pZ"])
                P.op("dve", lambda e: e.tensor_scalar(out=iws[:], in0=pZ[:, 0:8], scalar1=8.0 ** -0.5, scalar2=None, op0=ALU.mult), reads=["pZ"], writes=["iws"])
                P.op("sp", lambda e, i=i: e.dma_start(out=S["iw"][i * 128:(i + 1) * 128, :], in_=iws[:]), reads=["iws"], writes=[("iw_d", i)], ndma=1)
            P.barrier()

    def phase_b2(self, I, S, nblk, NIT=12):
        nc, P = self.nc, self.P
        SM = 512 * nblk
        with ExitStack() as st:
            tab = self.sb(st, "b2tab", [32, 8], F32)
            ohrev = self.sb(st, "b2ohrev", [32, 768], F32)
            Vs = self.sb(st, "b2Vs", [8, 768], F32)
            Biasf = self.sb(st, "b2Biasf", [128, 8, 640], F32)
            Biasb = self.sb(st, "b2Biasb", [128, 8, 640], BF16)
            cm = self.sb(st, "b2cm", [128, 512], F32)
            Dg = self.sb(st, "b2Dg", [128, 8, 128], BF16)
            dqT = self.sb(st, "b2dqT", [128, 4, 128], BF16)
            iqT = self.sb(st, "b2iqT", [128, 4, 128], BF16)
            iw = self.sb(st, "b2iw", [128, 8], F32)
            ik2 = [self.sb(st, "b2ik2_%d" % i, [128, 512], BF16) for i in range(2)]
            Rl = [self.sb(st, "b2R%d" % i, [128, 512], BF16) for i in range(2)]
            wk = self.sb(st, "b2wk", [128, SM], F32)
            madd = self.sb(st, "b2madd", [128, SM], BF16)
            madd_b = self.sb(st, "b2madd_b", [128, SM], BF16)
            dqT_b = self.sb(st, "b2dqT_b", [128, 4, 128], BF16)
            jk = self.sb(st, "b2jk", [128, SM], BF16)
            kTp = [self.sb(st, "b2kTp%d" % i, [128, SM], BF16) for i in range(2)]
            vp = [self.sb(st, "b2vp%d" % i, [128, SM // 128, 128], BF16) for i in range(2)]
            tmn = self.sb(st, "b2tmn", [128, 512], F32)
            Pg = [self.sb(st, "b2Pg%d" % i, [128, 512], BF16) for i in range(3)]
            PTs = [self.sb(st, "b2PT%d" % i, [128, 4, 128], BF16) for i in range(2)]
            ob = self.sb(st, "b2ob", [128, 512], BF16)
            sc = self.sb(st, "b2sc", [128, 16], F32)
            pw = self.sb(st, "b2pw", [128, NIT], F32)
            steps = self.sb(st, "b2steps", [128, NIT], F32)
            mids = self.sb(st, "b2mids", [128, NIT + 1], F32)
            cntd = self.sb(st, "b2cntd", [128, NIT], F32)
            cnta = self.sb(st, "b2cnta", [128, NIT], F32)
            mg = [self.sb(st, "b2mg%d" % i, [128, 16], F32) for i in range(2)]
            lcol = [self.sb(st, "b2lcol%d" % i, [128, 16], F32) for i in range(2)]
            psD = [self.ps(st, "b2psD%d" % i, [128, 512]) for i in range(2)]
            psI = self.ps(st, "b2psI", [128, 512])
            psQ = [self.ps(st, "b2psQ%d" % i, [128, 512]) for i in range(2)]
            psT = [self.ps(st, "b2psT%d" % i, [128, 512], BF16) for i in range(2)]
            po = self.ps(st, "b2po", [128, 512])
            psM = psD[0]

            P.op("sp", lambda e: e.dma_start(out=tab[:], in_=I["rel_bias_table"]), writes=["tab"], ndma=1)
            P.op("sp", lambda e: e.dma_start(out=ohrev[:], in_=I["ohrev"]), writes=["ohrev"], ndma=1)
            P.op("sp", lambda e: e.dma_start(out=cm[:], in_=I["cm"]), writes=["cm"], ndma=1)
            for k in range(NIT):
                P.op("pool", lambda e, k=k: e.memset(pw[:, k:k + 1], 2.0 ** -(k + 1)), writes=[("pw", k)])
            pw_keys = [("pw", k) for k in range(NIT)]

            def mmv(e):
                e.matmul(psI[0:8, 0:384], lhsT=tab[:], rhs=ohrev[:, 0:384], start=True, stop=True)
                return e.matmul(psQ[0][0:8, 0:384], lhsT=tab[:], rhs=ohrev[:, 384:768], start=True, stop=True)
            P.op("pe", mmv, reads=["tab", "ohrev"], writes=["psI", ("psQ", 0)])
            P.op("dve", lambda e: e.tensor_copy(out=Vs[:, 0:384], in_=psI[0:8, 0:384]), reads=["psI"], writes=["Vs0"])
            P.op("dve", lambda e: e.tensor_copy(out=Vs[:, 384:768], in_=psQ[0][0:8, 0:384]), reads=[("psQ", 0)], writes=["Vs1"])
            P.op("sp", lambda e: e.dma_start(out=S["vbias"], in_=Vs[:]), reads=["Vs0", "Vs1"], writes=["vbias_d"], ndma=1)
            for r0 in range(0, 128, 16):
                def ld(e, r0=r0):
                    out = []
                    for r in range(r0, r0 + 16):
                        out.append(e.dma_start(out=Biasf[r:r + 1, :, :], in_=S["vbias"][:, 127 - r:127 - r + 640].unsqueeze(0)))
                    return out
                P.op("sp" if (r0 // 16) % 2 == 0 else "pool", ld, reads=["vbias_d"], writes=[("Biasf", r0)], ndma=16)
            P.op("dve", lambda e: e.tensor_copy(out=Biasb[:], in_=Biasf[:]), reads=[("Biasf", r0) for r0 in range(0, 128, 16)], writes=["Biasb"])

            st_ = {"ikc": 0, "pgc": 0}
            KAf = self.sb(st, "b2KAf", [128, 4], F32)
            KAblk = self.sb(st, "b2KAblk", [128, 4, 2], BF16)
            absq = self.sb(st, "b2absq", [128, 4, 128], BF16)
            negb2 = [self.sb(st, "b2negb%d" % i_, [128, 8], F32) for i_ in range(2)]
            P.op("sp", lambda e: e.dma_start(out=KAf[:], in_=S["ka"]), writes=["KAf"], ndma=1)
            P.op("dve", lambda e: e.memset(KAblk[:], 0.0), writes=["KAblk"])
            P.op("dve", lambda e: e.tensor_copy(out=KAblk[0:64, :, 0], in_=KAf[0:64, :]), reads=["KAf", "KAblk"], writes=["KAblk"])
            P.op("dve", lambda e: e.tensor_copy(out=KAblk[64:128, :, 1], in_=KAf[64:128, :]), reads=["KAf", "KAblk"], writes=["KAblk"])
            madd2 = [madd, madd_b]
            dqT2 = [dqT, dqT_b]

            def stageX(i):
                Si = 512 * (i + 1)
                ng = i + 1
                par = i % 2
                maddc = madd2[par]
                dq_ = dqT2[par]
                P.op("sp", lambda e, i=i, dq_=dq_: e.dma_start(out=dq_[:], in_=S["dqT"][i]), writes=[("dqT", par)], ndma=1)
                P.op("sp", lambda e, i=i: e.dma_start(out=iqT[:], in_=S["iqT"][i]), writes=["iqT"], ndma=1)
                P.op("sp", lambda e, i=i: e.dma_start(out=iw[:], in_=S["iw"][i * 128:(i + 1) * 128, :]), writes=["iw"], ndma=1)
                for h in range(8):
                    P.op("pool", lambda e, h=h: e.tensor_scalar(out=Dg[:, h, :], in0=self.idf[:], scalar1=iw[:, h:h + 1], scalar2=None, op0=ALU.mult),
                         reads=["idf", "iw"], writes=[("Dg", h)], cost=0.4)
                P.op("act", lambda e, dq_=dq_: e.activation(out=absq[:].rearrange("p c t -> p (c t)"), in_=dq_[:].rearrange("p c t -> p (c t)"), func=AF.Abs),
                     reads=[("dqT", par)], writes=["absq"], cost=0.6)

                def mmb(e):
                    r = None
                    for p in range(4):
                        r = e.matmul(psI[:, 2 * p:2 * p + 2], lhsT=absq[:, p, :], rhs=KAblk[:, p, :], start=True, stop=True)
                    return r
                P.op("pe", mmb, reads=["absq", "KAblk"], writes=["psI"], cost=0.4)
                nb_ = negb2[par]
                P.op("dve", lambda e, nb_=nb_: e.tensor_scalar(out=nb_[:], in0=psI[:, 0:8], scalar1=-1.0, scalar2=None, op0=ALU.mult), reads=["psI"], writes=[("negb", par)], cost=0.1)
                yield
                for g in range(ng):
                    ikb = ik2[st_["ikc"] % 2]
                    kik = ("ik2", st_["ikc"] % 2)
                    st_["ikc"] += 1

                    def ldik(e, ikb=ikb, g=g):
                        a_ = e.dma_start(out=ikb[0:64, :], in_=S["ikT"][:, g * 512:(g + 1) * 512])
                        b_ = e.dma_start(out=ikb[64:128, :], in_=S["ikT"][:, g * 512:(g + 1) * 512])
                        return [a_, b_]
                    P.op("sp", ldik, writes=[kik], ndma=2)
                    for h in range(8):
                        hp = h % 2
                        pd = psD[h % 2]
                        P.op("pe", lambda e, h=h, hp=hp, pd=pd, ikb=ikb: e.matmul(pd[:], lhsT=iqT[hp * 64:(hp + 1) * 64, h // 2, :], rhs=ikb[hp * 64:(hp + 1) * 64, :],
                                                                                  start=True, stop=True),
                             reads=["iqT", kik], writes=[("psD", h % 2)], cost=0.25)
                        rl_ = Rl[h % 2]
                        if h % 2 == 0:
                            P.op("act", lambda e, rl_=rl_, pd=pd: e.activation(out=rl_[:], in_=pd[:], func=AF.Relu), reads=[("psD", h % 2)], writes=[("R", h % 2)], cost=0.6)
                        else:
                            P.op("dve", lambda e, rl_=rl_, pd=pd: e.tensor_scalar(out=rl_[:], in0=pd[:], scalar1=0.0, scalar2=None, op0=ALU.max),
                                 reads=[("psD", h % 2)], writes=[("R", h % 2)], cost=0.6)
                        P.op("pe", lambda e, h=h, rl_=rl_: e.matmul(psI[:], lhsT=Dg[:, h, :], rhs=rl_[:], start=(h == 0), stop=(h == 7)),
                             reads=[("Dg", h), ("R", h % 2)], writes=["psI"], cost=0.25)
                    gs = slice(g * 512, (g + 1) * 512)
                    if g < ng - 1:
                        P.op("act", lambda e, gs=gs: e.copy(out=wk[:, gs], in_=psI[:]), reads=["psI"], writes=[("wk", g)], cost=0.6)
                    else:
                        P.op("dve", lambda e, gs=gs: e.tensor_tensor(out=wk[:, gs], in0=psI[:], in1=cm[:], op=ALU.add), reads=["psI", "cm"], writes=[("wk", g)], cost=0.6)
                        P.op("dve", lambda e: e.tensor_tensor(out=tmn[:], in0=psI[:], in1=cm[:], op=ALU.subtract), reads=["psI", "cm"], writes=["tmn"], cost=0.6)
                    yield
                wk_keys = [("wk", g) for g in range(ng)]
                LO, W_, TT, SG, HI, MN1, MN2, THR = [sc[:, c:c + 1] for c in range(8)]
                big = Si / 960.0 + 0.1
                P.op("dve", lambda e, Si=Si: e.tensor_reduce(out=HI, in_=wk[:, 0:Si], axis=AX.X, op=ALU.max), reads=wk_keys, writes=["hi"], cost=big)
                P.op("dve", lambda e: e.tensor_reduce(out=MN1, in_=tmn[:], axis=AX.X, op=ALU.min), reads=["tmn"], writes=["mn1"], cost=0.6)
                if i > 0:
                    P.op("dve", lambda e, Si=Si: e.tensor_reduce(out=MN2, in_=wk[:, 0:Si - 512], axis=AX.X, op=ALU.min), reads=wk_keys, writes=["mn2"], cost=big)
                    P.op("dve", lambda e: e.tensor_tensor(out=MN1, in0=MN1, in1=MN2, op=ALU.min), reads=["mn1", "mn2"], writes=["mn1"], cost=0.1)
                P.op("dve", lambda e: e.tensor_scalar(out=LO, in0=MN1, scalar1=-1.0, scalar2=None, op0=ALU.add), reads=["mn1"], writes=["lo"], cost=0.1)
                P.op("dve", lambda e: e.scalar_tensor_tensor(out=W_, in0=HI, scalar=1.0, in1=LO, op0=ALU.add, op1=ALU.subtract), reads=["hi", "lo"], writes=["w"], cost=0.1)
                P.op("dve", lambda e: e.tensor_scalar(out=steps[:], in0=pw[:], scalar1=W_, scalar2=None, op0=ALU.mult), reads=pw_keys + ["w"], writes=["steps"], cost=0.1)
                P.op("dve", lambda e: e.tensor_tensor(out=mids[:, 0:1], in0=LO, in1=steps[:, 0:1], op=ALU.add), reads=["lo", "steps"], writes=[("mid", 0)], cost=0.1)
                P.op("pool", lambda e: e.memset(cntd[:], 0.0), writes=["cntd"] + [("cntd", it) for it in range(NIT)], cost=0.2)
                P.op("pool", lambda e: e.memset(cnta[:], 0.0), writes=["cnta"] + [("cnta", it) for it in range(NIT)], cost=0.2)
                yield
                Sh = (Si // 2 + 127) // 128 * 128
                n2 = Si - Sh
                half = Sh / 960.0 + 0.1
                for it in range(NIT):
                    MID = mids[:, it:it + 1]
                    P.op("act", lambda e, it=it, Sh=Sh, Si=Si, MID=MID: e.activation(out=jk[:, Sh:Si], in_=wk[:, Sh:Si], func=AF.Sign, bias=MID, scale=-1.0, accum_out=cnta[:, it:it + 1]),
                         reads=wk_keys + [("mid", it), "cnta"], writes=["jka", ("cnta", it)], cost=half)
                    P.op("dve", lambda e, it=it, Sh=Sh, MID=MID: e.tensor_scalar(out=jk[:, 0:Sh], in0=wk[:, 0:Sh], scalar1=MID, scalar2=0.0, op0=ALU.is_ge, op1=ALU.add,
                                                                                accum_out=cntd[:, it:it + 1]),
                         reads=wk_keys + [("mid", it), "cntd"], writes=["jkd", ("cntd", it)], cost=half)
                    P.op("dve", lambda e, it=it: e.scalar_tensor_tensor(out=TT, in0=cnta[:, it:it + 1], scalar=-0.5, in1=cntd[:, it:it + 1], op0=ALU.mult, op1=ALU.add),
                         reads=[("cnta", it), ("cntd", it)], writes=["tt"], cost=0.1)
                    P.op("dve", lambda e, n2=n2: e.tensor_scalar(out=SG, in0=TT, scalar1=255.5 - n2 / 2.0, scalar2=-0.5, op0=ALU.is_ge, op1=ALU.add), reads=["tt"], writes=["sg"], cost=0.1)
                    P.op("dve", lambda e, it=it, MID=MID: e.scalar_tensor_tensor(out=mids[:, it + 1:it + 2], in0=steps[:, it:it + 1], scalar=SG, in1=MID, op0=ALU.mult, op1=ALU.add),
                         reads=["steps", "sg", ("mid", it)], writes=[("mid", it + 1)], cost=0.1)
                    yield
                P.op("dve", lambda e: e.scalar_tensor_tensor(out=THR, in0=steps[:, NIT - 1:NIT], scalar=-0.5, in1=mids[:, NIT:NIT + 1], op0=ALU.mult, op1=ALU.add),
                     reads=["steps", ("mid", NIT)], writes=["thr"], cost=0.1)
                P.op("dve", lambda e, Si=Si, maddc=maddc: e.tensor_scalar(out=maddc[:, 0:Si], in0=wk[:, 0:Si], scalar1=THR, scalar2=NEG, op0=ALU.is_lt, op1=ALU.mult),
                     reads=wk_keys + ["thr"], writes=[("madd", par)], cost=big)
                yield

            def stageY(i):
                Si = 512 * (i + 1)
                ng = i + 1
                par = i % 2
                maddc = madd2[par]
                dq_ = dqT2[par]
                kdq = ("dqT", par)
                kmadd = ("madd", par)
                nkb = Si // 128

                def pass1(h, kb_, kk, p, rows):
                    mgb = mg[h % 2]
                    for g in range(ng):
                        pq = psM
                        gs = slice(g * 512, (g + 1) * 512)
                        P.op("pe", lambda e, pq=pq, rows=rows, p=p, kb_=kb_, gs=gs: e.matmul(pq[:], lhsT=dq_[rows, p, :], rhs=kb_[rows, gs], start=True, stop=True),
                             reads=[kdq, kk], writes=[("psD", 0)], cost=0.25)
                        P.op("dve", lambda e, pq=pq, g=g, mgb=mgb: e.tensor_reduce(out=mgb[:, g:g + 1], in_=pq[:], axis=AX.X, op=ALU.max),
                             reads=[("psD", 0)], writes=[("mg", h % 2, g)], cost=0.6)
                    if ng > 1:
                        P.op("dve", lambda e, mgb=mgb, ng=ng: e.tensor_reduce(out=mgb[:, 15:16], in_=mgb[:, 0:ng], axis=AX.X, op=ALU.max),
                             reads=[("mg", h % 2, g) for g in range(ng)], writes=[("m", h % 2)], cost=0.1)
                    else:
                        P.op("dve", lambda e, mgb=mgb: e.tensor_copy(out=mgb[:, 15:16], in_=mgb[:, 0:1]),
                             reads=[("mg", h % 2, 0)], writes=[("m", h % 2)], cost=0.1)
                    P.op("dve", lambda e, mgb=mgb: e.tensor_scalar(out=mgb[:, 14:15], in0=mgb[:, 15:16], scalar1=-1.0, scalar2=None, op0=ALU.mult),
                         reads=[("m", h % 2)], writes=[("negm", h % 2)], cost=0.1)

                loaded = {}

                def load_pair(p):
                    kb_ = kTp[p % 2]
                    vb_ = vp[p % 2]
                    kk = ("kTp", p % 2)
                    kv = ("vp", p % 2)
                    dc = Si * 256 * 128 / 150e3 + 2.0
                    P.op("sp", lambda e, kb_=kb_, p=p, Si=Si: e.dma_start(out=kb_[:, 0:Si], in_=S["kT"][:, p, 0:Si]), writes=[kk], ndma=1, cost=dc)
                    P.op("pool", lambda e, vb_=vb_, p=p, nkb=nkb: e.dma_start(out=vb_[:, 0:nkb, :], in_=S["v2"][:, p, 0:nkb, :]), writes=[kv], ndma=1, cost=dc)
                    loaded[p] = (kb_, vb_, kk, kv)

                load_pair(0)
                nb_ = negb2[par]
                for h in range(8):
                    p, hh = h // 2, h % 2
                    rows = slice(hh * 64, (hh + 1) * 64)
                    kb_, vb_, kk, kv = loaded[p]
                    if hh == 0 and p + 1 < 4:
                        load_pair(p + 1)
                    mgb = mg[h % 2]
                    lc = lcol[h % 2]
                    P.op("pool", lambda e, lc=lc: e.memset(lc[:], 0.0), writes=[("lcol", h % 2)] + [("lc", h % 2, g) for g in range(ng)], cost=0.2)
                    for g in range(ng):
                        pq = psQ[g % 2]
                        gs = slice(g * 512, (g + 1) * 512)

                        def qk2(e, pq=pq, rows=rows, p=p, kb_=kb_, gs=gs, g=g, h=h, ng=ng):
                            e.matmul(pq[:], lhsT=dq_[rows, p, :], rhs=kb_[rows, gs], start=True, stop=False)
                            if g == ng - 1:
                                e.matmul(pq[:], lhsT=self.idb[:], rhs=Biasb[:, h, 128:640], start=False, stop=False)
                            elif g == ng - 2:
                                e.matmul(pq[:, 384:512], lhsT=self.idb[:], rhs=Biasb[:, h, 0:128], start=False, stop=False)
                            return e.matmul(pq[:], lhsT=self.idb[:], rhs=maddc[:, gs], start=False, stop=True)
                        P.op("pe", qk2, reads=[kdq, kk, "idb", "Biasb", kmadd], writes=[("psQ", g % 2)], cost=0.5)
                        pgb = Pg[st_["pgc"] % 3]
                        kpg = ("Pg", st_["pgc"] % 3)
                        st_["pgc"] += 1
                        P.op("act", lambda e, pq=pq, pgb=pgb, nb_=nb_, lc=lc, g=g, h=h: e.activation(out=pgb[:], in_=pq[:], func=AF.Exp, bias=nb_[:, h:h + 1], scale=1.0,
                                                                                                     accum_out=lc[:, g:g + 1]),
                             reads=[("psQ", g % 2), ("negb", par), ("lcol", h % 2)], writes=[kpg, ("lc", h % 2, g)], cost=0.65)
                        qi = g % 2

                        def tr(e, pgb=pgb, qi=qi):
                            r = None
                            for c in range(4):
                                r = e.transpose(out=psT[qi][:, c * 128:(c + 1) * 128], in_=pgb[:, c * 128:(c + 1) * 128], identity=self.idb[:])
                            return r
                        P.op("pe", tr, reads=[kpg, "idb"], writes=[("psT", qi)], cost=0.3)
                        if qi == 0:
                            P.op("dve", lambda e, qi=qi: e.tensor_copy(out=PTs[qi][:].rearrange("p c t -> p (c t)"), in_=psT[qi][:]), reads=[("psT", qi)], writes=[("PT", qi)], cost=0.6)
                        else:
                            P.op("act", lambda e, qi=qi: e.copy(out=PTs[qi][:].rearrange("p c t -> p (c t)"), in_=psT[qi][:]), reads=[("psT", qi)], writes=[("PT", qi)], cost=0.6)

                        def pv(e, g=g, qi=qi, hh=hh, vb_=vb_, nkb=nkb):
                            r = None
                            for c in range(4):
                                kb = 4 * g + c
                                r = e.matmul(po[:, 0:64], lhsT=PTs[qi][:, c, :], rhs=vb_[:, kb, hh * 64:(hh + 1) * 64], start=(kb == 0), stop=(kb == nkb - 1))
                            return r
                        P.op("pe", pv, reads=[("PT", qi), kv], writes=["po"], cost=0.45)
                    LS, RL_ = sc[:, 8:9], sc[:, 9:10]
                    P.op("dve", lambda e, lc=lc: e.tensor_reduce(out=LS, in_=lc[:, 0:16], axis=AX.X, op=ALU.add), reads=[("lc", h % 2, g) for g in range(ng)] + [("lcol", h % 2)], writes=["ls"], cost=0.1)
                    P.op("dve", lambda e: e.reciprocal(out=RL_, in_=LS), reads=["ls"], writes=["rl"], cost=0.1)
                    P.op("dve", lambda e, h=h: e.tensor_scalar(out=ob[:, h * 64:(h + 1) * 64], in0=po[:, 0:64], scalar1=RL_, scalar2=None, op0=ALU.mult),
                         reads=["po", "rl"], writes=[("ob", h)], cost=0.15)
                    yield
                P.op("sp", lambda e, i=i: e.dma_start(out=S["ob"][i * 128:(i + 1) * 128, :], in_=ob[:]), reads=[("ob", h) for h in range(8)], writes=[("ob_d", i)], ndma=1)

            for _ in stageX(0):
                pass
            for i in range(nblk):
                gx = stageX(i + 1) if i + 1 < nblk else None
                nchunks = (2 + (i + 2) + 1 + NIT + 1) if gx is not None else 0
                per = (nchunks + 7) // 8
                for _ in stageY(i):
                    if gx is not None:
                        for _k in range(per):
                            try:
                                next(gx)
                            except StopIteration:
                                gx = None
                                break
                if gx is not None:
                    for _ in gx:
                        pass
            P.barrier()

    def phase_b3(self, I, S, nblk):
        nc, P = self.nc, self.P
        with ExitStack() as st:
            wgp = self.sb(st, "b3wgp", [128, 8, D], BF16)
            wdp = self.sb(st, "b3wdp", [128, 4, D], BF16)
            wout = self.sb(st, "b3wout", [128, 8, D], BF16)
            wcq = self.sb(st, "b3wcq", [128, 8, 512], BF16)
            wckv = self.sb(st, "b3wckv", [128, 8, D], BF16)
            wco = self.sb(st, "b3wco", [128, 4, D], BF16)
            G = {}
            for nm in ("mix_post", "cross_pre", "cross_post", "mem_norm"):
                G[nm] = self.sb(st, "b3g_" + nm, [128, D], F32)
                P.op("sp", lambda e, nm=nm: e.dma_start(out=G[nm][:], in_=I[nm].to_broadcast([128, D])), writes=["g_" + nm], ndma=1)
            self.load_w(wgp[:], I["w_gla_proj"], 0, D, "wgp")
            self.load_w(wdp[:], I["w_dsa_proj"], 0, D, "wdp", kchunks=4)
            self.load_w(wout[:], I["w_out"], 0, D, "wout")
            self.load_w(wcq[:], I["w_cq"], 0, 512, "wcq")
            self.load_w(wckv[:], I["w_ckv"], 0, D, "wckv")
            self.load_w(wco[:], I["w_co"], 0, D, "wco", kchunks=4)
            memf = self.sb(st, "b3memf", [128, D], F32)
            xnb = self.sb(st, "b3xnb", [128, D], BF16)
            memT = self.sb(st, "b3memT", [128, 8, 256], BF16)
            kmT = self.sb(st, "b3kmT", [128, 4, 256], BF16)
            vm = self.sb(st, "b3vm", [128, 2, 512], BF16)
            oanb = self.sb(st, "b3oanb", [128, D], BF16)
            obb = self.sb(st, "b3obb", [128, 512], BF16)
            sga = self.sb(st, "b3sga", [128, D], BF16)
            sgb = self.sb(st, "b3sgb", [128, D], BF16)
            h1o = self.sb(st, "b3h1o", [128, D], F32)
            oT = self.sb(st, "b3oT", [128, 8, 128], BF16)
            obT = self.sb(st, "b3obT", [128, 4, 128], BF16)
            gay = self.sb(st, "b3gay", [128, D], F32)
            t1 = self.sb(st, "b3t1", [128, D], F32)
            mrg = self.sb(st, "b3mrg", [128, D], BF16)
            mT = self.sb(st, "b3mT", [128, 8, 128], BF16)
            tmp = self.sb(st, "b3tmp", [128, D], F32)
            h2t = self.sb(st, "b3h2t", [128, D], F32)
            h3t = self.sb(st, "b3h3t", [128, D], F32)
            xcT = self.sb(st, "b3xcT", [128, 8, 128], BF16)
            qcT = self.sb(st, "b3qcT", [128, 4, 128], BF16)
            Pc = self.sb(st, "b3Pc", [128, D], BF16)
            PcT = self.sb(st, "b3PcT", [128, 8, 128], BF16)
            ox = self.sb(st, "b3ox", [128, 512], BF16)
            oxT = self.sb(st, "b3oxT", [128, 4, 128], BF16)
            junk = self.sb(st, "b3junk", [128, D], BF16)
            stat = self.sb(st, "b3stat", [128, 64], F32)
            sc = self.sb(st, "b3sc", [128, 16], F32)
            psT = [self.ps(st, "b3psT%d" % i, [128, 512], BF16) for i in range(2)]
            pA = self.ps(st, "b3pA", [128, 1024])
            pB = self.ps(st, "b3pB", [128, 1024])
            pX = self.ps(st, "b3pX", [128, 512])

            def proj(ps, xT_, wt, nk, keys):
                def mm(e):
                    r = None
                    for nh in range(2):
                        for k in range(nk):
                            r = e.matmul(ps[:, nh * 512:(nh + 1) * 512], lhsT=xT_[:, k, :], rhs=wt[:, k, nh * 512:(nh + 1) * 512], start=(k == 0), stop=(k == nk - 1))
                    return r
                return mm

            for mc in range(2):
                P.op("sp", lambda e, mc=mc: e.dma_start(out=memf[:], in_=I["mem"][mc * 128:(mc + 1) * 128, :]), writes=["memf"], ndma=1)
                c0, k0 = self.rstd_of(memf[:], D, stat, 2 * mc, "memf", "b3m", junk[:])
                P.op("dve", lambda e, c0=c0: e.scalar_tensor_tensor(out=xnb[:], in0=memf[:], scalar=c0, in1=G["mem_norm"][:], op0=ALU.mult, op1=ALU.mult),
                     reads=["memf", k0, "g_mem_norm"], writes=["xnb"])
                self.transposes8(xnb, psT, lambda g, n, mc=mc: memT[:, 4 * g:4 * g + n, mc * 128:(mc + 1) * 128], ["xnb"], lambda g, mc=mc: ("memT", mc, g))
            memT_keys = [("memT", mc, g) for mc in range(2) for g in range(2)]

            def mmk(e):
                r = None
                for h in range(4):
                    for k in range(8):
                        r = e.matmul(pA[:, h * 256:(h + 1) * 256], lhsT=wckv[:, k, h * 128:(h + 1) * 128], rhs=memT[:, k, :], start=(k == 0), stop=(k == 7))
                return r
            P.op("pe", mmk, reads=memT_keys + ["wckv"], writes=["pA"])
            P.op("act", lambda e: e.copy(out=kmT[:].rearrange("p h m -> p (h m)"), in_=pA[:]), reads=["pA"], writes=["kmT"])
            for mc in range(2):
                def mmvm(e, mc=mc):
                    r = None
                    for k in range(8):
                        r = e.matmul(pB[:, 0:512], lhsT=memT[:, k, mc * 128:(mc + 1) * 128], rhs=wckv[:, k, 512:1024], start=(k == 0), stop=(k == 7))
                    return r
                P.op("pe", mmvm, reads=memT_keys + ["wckv"], writes=["pB"])
                P.op("act", lambda e, mc=mc: e.copy(out=vm[:, mc, :], in_=pB[:, 0:512]), reads=["pB"], writes=[("vm", mc)])
            vm_keys = [("vm", 0), ("vm", 1)]

            for i in range(nblk):
                rs = slice(i * 128, (i + 1) * 128)
                P.op("sp", lambda e, rs=rs: e.dma_start(out=oanb[:], in_=S["oan"][rs, :]), writes=["oanb"], ndma=1)
                P.op("sp", lambda e, rs=rs: e.dma_start(out=obb[:], in_=S["ob"][rs, :]), writes=["obb"], ndma=1)
                P.op("sp", lambda e, rs=rs: e.dma_start(out=sga[:], in_=S["sga"][rs, :]), writes=["sga"], ndma=1)
                P.op("sp", lambda e, rs=rs: e.dma_start(out=sgb[:], in_=S["sgb"][rs, :]), writes=["sgb"], ndma=1)
                P.op("sp", lambda e, rs=rs: e.dma_start(out=h1o[:], in_=S["h1own"][rs, :]), writes=["h1o"], ndma=1)
                self.transposes8(oanb, psT, lambda g, n: oT[:, 4 * g:4 * g + n, :], ["oanb"], lambda g: ("oT", g))
                P.op("pe", proj(pA, oT, wgp, 8, None), reads=[("oT", 0), ("oT", 1), "wgp"], writes=["pA"])
                P.op("dve", lambda e: e.tensor_tensor(out=gay[:], in0=pA[:], in1=sga[:], op=ALU.mult), reads=["pA", "sga"], writes=["gay"])
                self.transposes8(obb, psT, lambda g, n: obT[:, 0:n, :], ["obb"], lambda g: "obT", nchunks=4)
                P.op("pe", proj(pB, obT, wdp, 4, None), reads=["obT", "wdp"], writes=["pB"])
                P.op("dve", lambda e: e.tensor_tensor(out=t1[:], in0=pB[:], in1=sgb[:], op=ALU.mult), reads=["pB", "sgb"], writes=["t1"])
                P.op("pool", lambda e: e.tensor_tensor(out=mrg[:], in0=t1[:], in1=gay[:], op=ALU.add), reads=["t1", "gay"], writes=["mrg"])
                self.transposes8(mrg, psT, lambda g, n: mT[:, 4 * g:4 * g + n, :], ["mrg"], lambda g: ("mT", g))
                P.op("pe", proj(pA, mT, wout, 8, None), reads=[("mT", 0), ("mT", 1), "wout"], writes=["pA"])
                c0, k0 = self.rstd_of(pA[:], D, stat, 8, "pA", "b3a", junk[:])
                P.op("dve", lambda e, c0=c0: e.scalar_tensor_tensor(out=tmp[:], in0=pA[:], scalar=c0, in1=G["mix_post"][:], op0=ALU.mult, op1=ALU.mult),
                     reads=["pA", k0, "g_mix_post"], writes=["tmp"])
                P.op("pool", lambda e: e.tensor_tensor(out=h2t[:], in0=tmp[:], in1=h1o[:], op=ALU.add), reads=["tmp", "h1o"], writes=["h2t"])
                c0, k0 = self.rstd_of(h2t[:], D, stat, 10, "h2t", "b3b", junk[:])
                P.op("dve", lambda e, c0=c0: e.scalar_tensor_tensor(out=xnb[:], in0=h2t[:], scalar=c0, in1=G["cross_pre"][:], op0=ALU.mult, op1=ALU.mult),
                     reads=["h2t", k0, "g_cross_pre"], writes=["xnb"])
                self.transposes8(xnb, psT, lambda g, n: xcT[:, 4 * g:4 * g + n, :], ["xnb"], lambda g: ("xcT", g))

                def mmq(e):
                    r = None
                    for h in range(4):
                        for k in range(8):
                            r = e.matmul(pX[:, h * 128:(h + 1) * 128], lhsT=wcq[:, k, h * 128:(h + 1) * 128], rhs=xcT[:, k, :], start=(k == 0), stop=(k == 7))
                    return r
                P.op("pe", mmq, reads=[("xcT", 0), ("xcT", 1), "wcq"], writes=["pX"])
                P.op("dve", lambda e: e.tensor_scalar(out=qcT[:].rearrange("p h t -> p (h t)"), in0=pX[:], scalar1=128.0 ** -0.5, scalar2=None, op0=ALU.mult),
                     reads=["pX"], writes=["qcT"])

                def mml(e):
                    r = None
                    for h in range(4):
                        r = e.matmul(pB[:, h * 256:(h + 1) * 256], lhsT=qcT[:, h, :], rhs=kmT[:, h, :], start=True, stop=True)
                    return r
                P.op("pe", mml, reads=["qcT", "kmT"], writes=["pB"])
                MX, NMX, LC, RLC = sc[:, 0:4], sc[:, 4:8], sc[:, 8:12], sc[:, 12:16]
                P.op("dve", lambda e: e.tensor_reduce(out=MX, in_=pB[:].rearrange("p (h m) -> p h m", h=4), axis=AX.X, op=ALU.max), reads=["pB"], writes=["mx"])
                P.op("dve", lambda e: e.tensor_scalar(out=NMX, in0=MX, scalar1=-1.0, scalar2=None, op0=ALU.mult), reads=["mx"], writes=["nmx"])
                P.op("dve", lambda e: e.memset(LC, 0.0), writes=["lc"])
                for h in range(4):
                    P.op("act", lambda e, h=h: e.activation(out=Pc[:, h * 256:(h + 1) * 256], in_=pB[:, h * 256:(h + 1) * 256], func=AF.Exp, bias=sc[:, 4 + h:5 + h], scale=1.0,
                                                            accum_out=sc[:, 8 + h:9 + h]),
                         reads=["pB", "nmx", "lc"], writes=[("Pc", h), ("lc", h)])
                self.transposes8(Pc, psT, lambda g, n: PcT[:, 4 * g:4 * g + n, :], [("Pc", h) for h in range(4)], lambda g: ("PcT", g))

                def mmov(e):
                    r = None
                    for h in range(4):
                        for mc in range(2):
                            r = e.matmul(pX[:, h * 128:(h + 1) * 128], lhsT=PcT[:, h * 2 + mc, :], rhs=vm[:, mc, h * 128:(h + 1) * 128], start=(mc == 0), stop=(mc == 1))
                    return r
                P.op("pe", mmov, reads=[("PcT", 0), ("PcT", 1)] + vm_keys, writes=["pX"])
                P.op("dve", lambda e: e.reciprocal(out=RLC, in_=LC), reads=[("lc", h) for h in range(4)], writes=["rlc"])
                for h in range(4):
                    P.op("dve", lambda e, h=h: e.tensor_scalar(out=ox[:, h * 128:(h + 1) * 128], in0=pX[:, h * 128:(h + 1) * 128], scalar1=sc[:, 12 + h:13 + h], scalar2=None, op0=ALU.mult),
                         reads=["pX", "rlc"], writes=[("ox", h)])
                self.transposes8(ox, psT, lambda g, n: oxT[:, 0:n, :], [("ox", h) for h in range(4)], lambda g: "oxT", nchunks=4)
                P.op("pe", proj(pA, oxT, wco, 4, None), reads=["oxT", "wco"], writes=["pA"])
                c0, k0 = self.rstd_of(pA[:], D, stat, 12, "pA", "b3c", junk[:])
                P.op("dve", lambda e, c0=c0: e.scalar_tensor_tensor(out=tmp[:], in0=pA[:], scalar=c0, in1=G["cross_post"][:], op0=ALU.mult, op1=ALU.mult),
                     reads=["pA", k0, "g_cross_post"], writes=["tmp"])
                P.op("pool", lambda e: e.tensor_tensor(out=h3t[:], in0=tmp[:], in1=h2t[:], op=ALU.add), reads=["tmp", "h2t"], writes=["h3t"])
                P.op("sp", lambda e, rs=rs: e.dma_start(out=S["h3"][rs, :], in_=h3t[:]), reads=["h3t"], writes=[("h3_d", i)], ndma=1)
            P.barrier()

    def build(self):
        nc, P = self.nc, self.P
        upto = self.upto
        nblk = self.nblk
        I = {}
        for nm, shp in INPUT_SPECS:
            I[nm] = self.din(nm, shp)
        self.I = I
        dbg = self.debug
        S = {}

        def scr(name, shape, dt):
            if dbg:
                S[name] = self.dout(name, shape, dt)
            else:
                S[name] = self.dscr(name, shape, dt)
        scr("h1", [nblk * 512, D], F32)
        scr("h1own", [nblk * 128, D], F32)
        scr("xTown", [nblk, 128, 8, 128], BF16)
        scr("kT", [128, 4, nblk * 512], BF16)
        scr("ikT", [64, nblk * 512], BF16)
        scr("v2", [128, 4, nblk * 4, 128], BF16)
        scr("Ssel", [nblk, 128, 4, 256], F32)
        scr("oan", [nblk * 128, D], BF16)
        scr("sga", [nblk * 128, D], BF16)
        scr("sgb", [nblk * 128, D], BF16)
        scr("dqT", [nblk, 128, 4, 128], BF16)
        scr("iqT", [nblk, 128, 4, 128], BF16)
        scr("iw", [nblk * 128, 8], F32)
        scr("vbias", [8, 768], F32)
        scr("ka", [128, 4], F32)
        scr("ob", [nblk * 128, 512], BF16)
        scr("h3", [nblk * 128, D], F32)
        S["out"] = self.dout("out", [nblk * 128, D], F32)
        with ExitStack() as st:
            self.consts(st)
            self.ffn_phase("f1", I["xall"], nblk * 512, I["ffn1_w_in"], I["ffn1_w_out"], I["ffn1_pre"], I["ffn1_post"], S["h1"])
            if upto != "A1":
                self.phase_a2(I, S, nblk)
            if upto not in ("A1", "A2"):
                self.phase_b1(I, S, nblk)
            if upto not in ("A1", "A2", "B1"):
                self.phase_b2(I, S, nblk)
            if upto not in ("A1", "A2", "B1", "B2"):
                self.phase_b3(I, S, nblk)
            if upto not in ("A1", "A2", "B1", "B2", "B3"):
                self.ffn_phase("f2", S["h3"], nblk * 128, I["ffn2_w_in"], I["ffn2_w_out"], I["ffn2_pre"], I["ffn2_post"], S["out"])
            P.emit(st)
        return nc


INPUT_SPECS = [
    ("xall", [T, D]), ("idn", [128, 128]), ("tri", [128, 128]), ("ej", [128, 4]),
    ("ffn1_pre", [1, D]), ("ffn1_post", [1, D]), ("ffn1_w_in", [D, 2 * DFF]), ("ffn1_w_out", [DFF, D]),
    ("mix_pre", [1, D]), ("w_in", [D, 7256]), ("w_alpha_up", [16, 512]), ("b_alpha", [1, 512]),
    ("gla_norm", [1, D]), ("maskT4", [128, 512]),
    ("rel_bias_table", [32, 8]), ("ohrev", [32, 768]), ("cm", [128, 512]),
    ("mem", [256, D]), ("mix_post", [1, D]), ("cross_pre", [1, D]), ("cross_post", [1, D]), ("mem_norm", [1, D]),
    ("w_gla_proj", [D, D]), ("w_dsa_proj", [512, D]), ("w_out", [D, D]), ("w_cq", [D, 512]), ("w_ckv", [D, D]), ("w_co", [512, D]),
    ("ffn2_pre", [1, D]), ("ffn2_post", [1, D]), ("ffn2_w_in", [D, 2 * DFF]), ("ffn2_w_out", [DFF, D]),
]


def t5_bucket_np(rel):
    rel = np.asarray(rel, dtype=np.int64)
    relf = np.maximum(rel, 1).astype(np.float32)
    large = 16 + (np.log(relf / np.float32(16)) / np.float32(np.log(8.0)) * np.float32(16)).astype(np.int32)
    large = np.minimum(large, 31)
    return np.where(rel < 16, rel, large)


def make_in_maps(inputs, ncores=8):
    in_maps = []
    idn = np.eye(128, dtype=np.float32)
    tri = np.triu(np.ones((128, 128), dtype=np.float32)) * (-1.0 / 16.0)
    for c in range(ncores):
        b, j = c // 4, c % 4
        ej = np.zeros((128, 4), dtype=np.float32)
        ej[:, j] = 1.0
        maskT = np.triu(np.ones((128, 128), dtype=np.float32))
        m = {"xall": np.ascontiguousarray(inputs["x"][b]), "idn": idn, "tri": tri, "ej": ej, "maskT4": np.ascontiguousarray(np.tile(maskT, (1, 4)))}
        mp = np.arange(768)
        rel = 128 * j + 255 - mp
        oh = np.zeros((32, 768), dtype=np.float32)
        ok = rel >= 0
        bk = t5_bucket_np(np.maximum(rel, 0))
        oh[bk[ok], mp[ok]] += 1.0
        oh[31, mp[ok]] -= 1.0
        m["ohrev"] = oh
        cc = np.arange(512)[None, :]
        rr = np.arange(128)[:, None]
        m["cm"] = np.where(cc <= 128 * j + rr, 0.0, NEG).astype(np.float32)
        m["rel_bias_table"] = np.ascontiguousarray(inputs["rel_bias_table"]).astype(np.float32)
        m["mem"] = np.ascontiguousarray(inputs["mem"][b])
        for nm, shp in INPUT_SPECS:
            if nm in m:
                continue
            m[nm] = np.ascontiguousarray(inputs[nm][0]).reshape(shp)
        in_maps.append(m)
    return in_maps


def run(inputs, upto, nblk=16, debug=True, ncores=8):
    kb = KB(upto)
    kb.nblk = nblk
    kb.debug = debug
    nc = kb.build()
    in_maps = make_in_maps(inputs, ncores)
    res = run_bass_kernel_spmd(nc, in_maps, core_ids=list(range(ncores)))
    return res


def kernel(**inputs):
    inputs = {k: np.asarray(v) for k, v in inputs.items()}
    res = run(inputs, "ALL", nblk=16, debug=False)
    out = np.zeros((2, T, D), dtype=np.float32)
    for c in range(8):
        b, j = c // 4, c % 4
        o = np.asarray(res.results[c]["out"]).reshape(16, 128, D)
        ov = out[b].reshape(16, 4, 128, D)
        ov[:, j] = o
    return out
```
